# Optimizing a Trainium2 kernel written in Bass

```python
import jax, jax.numpy as jnp
from jax import lax
import numpy as np

D_MODEL = 2048
BATCH = 1
SEQ = 8192
DEPTH = 1

POOL_GROUPS = 4
POOL_WINDOWS = (2, 4, 8, 16)
POOL_GROUP_W = 192
POOL_W = POOL_GROUPS * POOL_GROUP_W
ATTN_HEADS = 6
HEAD_DIM = 128
ATTN_W = ATTN_HEADS * HEAD_DIM
IDX_HEADS = 4
IDX_DIM = 64
TOPK_MAX = 256
Q_BLOCK = 128
MEM_LEN = 256
XATTN_HEADS = 4
XATTN_W = XATTN_HEADS * HEAD_DIM
N_BRANCH = 3
N_GROUPS = 4
EXPERTS_PER_GROUP = 4
N_EXPERTS = N_GROUPS * EXPERTS_PER_GROUP
EXPERT_FF = 512
TOPK_EXPERT = 2
ROPE_THETA = 10000.0
NORM_EPS = 1e-6
IN_COLS = POOL_W + 3 * ATTN_W + IDX_HEADS * IDX_DIM + IDX_DIM + IDX_HEADS + XATTN_W + N_BRANCH * D_MODEL

kernel_name = "hybrid_pool_dsa_memxattn_hmoe_block"


def rms_norm(x, g):
    xf = x.astype(jnp.float32)
    y = xf * lax.rsqrt(jnp.mean(xf * xf, axis=-1, keepdims=True) + NORM_EPS)
    return (y * g.astype(jnp.float32)).astype(x.dtype)


def rotary(x, positions):
    d = x.shape[-1]
    inv_freq = ROPE_THETA ** (-jnp.arange(0, d, 2, dtype=jnp.float32) / d)
    ang = positions.astype(jnp.float32)[..., None] * inv_freq
    cos = jnp.cos(ang)[:, :, None, :]
    sin = jnp.sin(ang)[:, :, None, :]
    xf = x.astype(jnp.float32)
    x1, x2 = xf[..., : d // 2], xf[..., d // 2:]
    out = jnp.concatenate([x1 * cos - x2 * sin, x2 * cos + x1 * sin], axis=-1)
    return out.astype(x.dtype)


def causal_multiscale_pool(u):
    B, S, G, C = u.shape
    uf = u.astype(jnp.float32)
    cs = jnp.concatenate([jnp.zeros((B, 1, G, C), jnp.float32), jnp.cumsum(uf, axis=1)], axis=1)
    t = jnp.arange(S)
    outs = []
    for g, w in enumerate(POOL_WINDOWS):
        lo = jnp.maximum(t + 1 - w, 0)
        total = cs[:, 1:, g] - cs[:, lo, g]
        cnt = jnp.minimum(t + 1, w).astype(jnp.float32)
        outs.append(total / cnt[None, :, None])
    pooled = jnp.stack(outs, axis=2)
    return (pooled - uf).astype(u.dtype)


def dsa_attention(q, k, v, qi, ki, wi, k_sel):
    B, S, H, dh = q.shape
    n_blocks = S // Q_BLOCK
    key_pos = jnp.arange(S)
    scale = HEAD_DIM ** -0.5

    def one_block(i):
        start = i * Q_BLOCK
        q_b = lax.dynamic_slice_in_dim(q, start, Q_BLOCK, axis=1)
        qi_b = lax.dynamic_slice_in_dim(qi, start, Q_BLOCK, axis=1)
        wi_b = lax.dynamic_slice_in_dim(wi, start, Q_BLOCK, axis=1)
        t = start + jnp.arange(Q_BLOCK)
        dots = jnp.einsum('bqhd,bsd->bqhs', qi_b, ki).astype(jnp.float32)
        score = jnp.einsum('bqhs,bqh->bqs', jax.nn.relu(dots), wi_b.astype(jnp.float32))
        causal = key_pos[None, None, :] <= t[None, :, None]
        score = jnp.where(causal, score, -jnp.inf)
        _, idx = lax.top_k(score, k_sel)
        k_g = jax.vmap(lambda kb, ib: kb[ib])(k, idx)
        v_g = jax.vmap(lambda vb, ib: vb[ib])(v, idx)
        logits = jnp.einsum('bqhd,bqkhd->bhqk', q_b, k_g).astype(jnp.float32) * scale
        valid = idx <= t[None, :, None]
        logits = jnp.where(valid[:, None], logits, -jnp.inf)
        p = jax.nn.softmax(logits, axis=-1).astype(v.dtype)
        return jnp.einsum('bhqk,bqkhd->bqhd', p, v_g)

    outs = lax.map(one_block, jnp.arange(n_blocks))
    return jnp.transpose(outs, (1, 0, 2, 3, 4)).reshape(B, S, H * dh)


def hierarchical_moe(h, w_router_group, w_router_expert, w_e_gate, w_e_up, w_e_down):
    B, S, _ = h.shape
    lg = jnp.einsum('bsd,dg->bsg', h, w_router_group).astype(jnp.float32)
    pg = jax.nn.softmax(lg, axis=-1)
    g_top = jnp.argmax(lg, axis=-1)
    pg_top = jnp.take_along_axis(pg, g_top[..., None], axis=-1)
    le = jnp.einsum('bsd,de->bse', h, w_router_expert).astype(jnp.float32)
    le = le.reshape(B, S, N_GROUPS, EXPERTS_PER_GROUP)
    le_sel = jnp.take_along_axis(le, g_top[..., None, None], axis=2)[:, :, 0]
    pe = jax.nn.softmax(le_sel, axis=-1)
    pe_top, e_local = lax.top_k(pe, TOPK_EXPERT)
    w = pg_top * pe_top / jnp.sum(pe_top, axis=-1, keepdims=True)
    e_id = g_top[..., None] * EXPERTS_PER_GROUP + e_local
    combine = jnp.einsum('bsk,bske->bse', w, jax.nn.one_hot(e_id, N_EXPERTS, dtype=jnp.float32))
    a = jnp.einsum('bsd,edf->bsef', h, w_e_gate)
    b = jnp.einsum('bsd,edf->bsef', h, w_e_up)
    act = jax.nn.silu(a) * b * combine[..., None].astype(h.dtype)
    return jnp.einsum('bsef,efd->bsd', act, w_e_down)


def hybrid_layer(x, mem, positions, g_mix, w_in, b_gate, w_pool_grp, pool_scale,
                 q_norm_g, k_norm_g, g_mem, w_mem_kv, xq_norm_g, xk_norm_g,
                 w_pool_out, w_attn_out, w_cross_out, w_o, g_ffn,
                 w_router_group, w_router_expert, w_e_gate, w_e_up, w_e_down):
    B, S, D = x.shape
    k_sel = min(TOPK_MAX, S // 4)
    h = rms_norm(x, g_mix)
    proj = h @ w_in
    sizes = (POOL_W, ATTN_W, ATTN_W, ATTN_W, IDX_HEADS * IDX_DIM, IDX_DIM, IDX_HEADS, XATTN_W)
    split_pts = [int(p) for p in np.cumsum(sizes)]
    u_pool, q, k, v, qi, ki, wi, xq, gate_logits = jnp.split(proj, split_pts, axis=-1)

    u = u_pool.reshape(B, S, POOL_GROUPS, POOL_GROUP_W)
    p = causal_multiscale_pool(u)
    p = jnp.einsum('bsgc,gcd->bsgd', p, w_pool_grp).reshape(B, S, POOL_W) * pool_scale
    pool_out = p @ w_pool_out

    q = rotary(rms_norm(q.reshape(B, S, ATTN_HEADS, HEAD_DIM), q_norm_g), positions)
    k = rotary(rms_norm(k.reshape(B, S, ATTN_HEADS, HEAD_DIM), k_norm_g), positions)
    v = v.reshape(B, S, ATTN_HEADS, HEAD_DIM)
    qi = rotary(qi.reshape(B, S, IDX_HEADS, IDX_DIM), positions)
    ki = rotary(ki[:, :, None, :], positions)[:, :, 0]
    wi = wi * (IDX_HEADS ** -0.5) * (IDX_DIM ** -0.5)
    attn = dsa_attention(q, k, v, qi, ki, wi, k_sel)
    attn_out = attn @ w_attn_out

    mem_h = rms_norm(mem, g_mem)
    kv_m = mem_h @ w_mem_kv
    M = mem.shape[1]
    k_m = rms_norm(kv_m[..., :XATTN_W].reshape(B, M, XATTN_HEADS, HEAD_DIM), xk_norm_g)
    v_m = kv_m[..., XATTN_W:].reshape(B, M, XATTN_HEADS, HEAD_DIM)
    xq = rms_norm(xq.reshape(B, S, XATTN_HEADS, HEAD_DIM), xq_norm_g)
    xl = jnp.einsum('bshd,bmhd->bhsm', xq, k_m).astype(jnp.float32) * (HEAD_DIM ** -0.5)
    xp = jax.nn.softmax(xl, axis=-1).astype(v_m.dtype)
    cross = jnp.einsum('bhsm,bmhd->bshd', xp, v_m).reshape(B, S, XATTN_W)
    cross_out = cross @ w_cross_out

    gates = jax.nn.sigmoid((gate_logits + b_gate).astype(jnp.float32)).astype(x.dtype)
    gates = gates.reshape(B, S, N_BRANCH, D)
    merged = gates[:, :, 0] * pool_out + gates[:, :, 1] * attn_out + gates[:, :, 2] * cross_out
    x = x + merged @ w_o

    x = x + hierarchical_moe(rms_norm(x, g_ffn), w_router_group, w_router_expert, w_e_gate, w_e_up, w_e_down)
    return x


def setup_inputs(seed: int = 0) -> dict:
    key = jax.random.key(seed)
    ks = jax.random.split(key, 24)
    f32 = jnp.float32
    L = DEPTH

    def nrm(k, shape, fan_in):
        return jax.random.normal(k, shape, f32) * (fan_in ** -0.5)

    def gain(k, shape):
        return 1.0 + 0.05 * jax.random.normal(k, shape, f32)

    return {
        "x": jax.random.normal(ks[0], (BATCH, SEQ, D_MODEL), f32),
        "mem": jax.random.normal(ks[1], (BATCH, MEM_LEN, D_MODEL), f32),
        "positions": jnp.broadcast_to(jnp.arange(SEQ, dtype=jnp.int32), (BATCH, SEQ)),
        "g_mix": gain(ks[2], (L, D_MODEL)),
        "w_in": nrm(ks[3], (L, D_MODEL, IN_COLS), D_MODEL),
        "b_gate": 0.02 * jax.random.normal(ks[4], (L, N_BRANCH * D_MODEL), f32),
        "w_pool_grp": nrm(ks[5], (L, POOL_GROUPS, POOL_GROUP_W, POOL_GROUP_W), POOL_GROUP_W),
        "pool_scale": gain(ks[6], (L, POOL_W)),
        "q_norm_g": gain(ks[7], (L, HEAD_DIM)),
        "k_norm_g": gain(ks[8], (L, HEAD_DIM)),
        "g_mem": gain(ks[9], (L, D_MODEL)),
        "w_mem_kv": nrm(ks[10], (L, D_MODEL, 2 * XATTN_W), D_MODEL),
        "xq_norm_g": gain(ks[11], (L, HEAD_DIM)),
        "xk_norm_g": gain(ks[12], (L, HEAD_DIM)),
        "w_pool_out": nrm(ks[13], (L, POOL_W, D_MODEL), POOL_W),
        "w_attn_out": nrm(ks[14], (L, ATTN_W, D_MODEL), ATTN_W),
        "w_cross_out": nrm(ks[15], (L, XATTN_W, D_MODEL), XATTN_W),
        "w_o": nrm(ks[16], (L, D_MODEL, D_MODEL), D_MODEL),
        "g_ffn": gain(ks[17], (L, D_MODEL)),
        "w_router_group": nrm(ks[18], (L, D_MODEL, N_GROUPS), D_MODEL),
        "w_router_expert": nrm(ks[19], (L, D_MODEL, N_EXPERTS), D_MODEL),
        "w_e_gate": nrm(ks[20], (L, N_EXPERTS, D_MODEL, EXPERT_FF), D_MODEL),
        "w_e_up": nrm(ks[21], (L, N_EXPERTS, D_MODEL, EXPERT_FF), D_MODEL),
        "w_e_down": nrm(ks[22], (L, N_EXPERTS, EXPERT_FF, D_MODEL), EXPERT_FF),
    }


def reference(x, mem, positions, g_mix, w_in, b_gate, w_pool_grp, pool_scale,
              q_norm_g, k_norm_g, g_mem, w_mem_kv, xq_norm_g, xk_norm_g,
              w_pool_out, w_attn_out, w_cross_out, w_o, g_ffn,
              w_router_group, w_router_expert, w_e_gate, w_e_up, w_e_down):
    for l in range(DEPTH):
        x = hybrid_layer(x, mem, positions, g_mix[l], w_in[l], b_gate[l], w_pool_grp[l], pool_scale[l],
                         q_norm_g[l], k_norm_g[l], g_mem[l], w_mem_kv[l], xq_norm_g[l], xk_norm_g[l],
                         w_pool_out[l], w_attn_out[l], w_cross_out[l], w_o[l], g_ffn[l],
                         w_router_group[l], w_router_expert[l], w_e_gate[l], w_e_up[l], w_e_down[l])
    return x
```

```python
import numpy as np
from contextlib import ExitStack
import concourse.bass as bass
import concourse.mybir as mybir
from concourse.bass_utils import run_bass_kernel_spmd

F32 = mybir.dt.float32
BF16 = mybir.dt.bfloat16
I32 = mybir.dt.int32
AF = mybir.ActivationFunctionType
ALU = mybir.AluOpType
AX = mybir.AxisListType

NCORES = 8
D = 2048
KC = 16
C_UP, C_Q, C_K, C_V, C_QI, C_KI, C_WI, C_XQ, C_G = 0, 768, 1536, 2304, 3072, 3328, 3392, 3396, 3908
IN_COLS = 10052
EPS = 1e-6
TWO_PI = 2.0 * np.pi
CW1 = 6.28125
CW2 = TWO_PI - 6.28125
NEG = -1.0e30
NITER = 18
VW = 132
NE = 16
FF = 512
DEBUG_NAMES = None
DVE_GAP = 2
PI_LO = 3.1415925


class Buf:
    __slots__ = ("name", "lw", "rd", "psum")

    def __init__(self, name, psum=False):
        self.name = name
        self.lw = None
        self.rd = []
        self.psum = psum


class _Rec:
    def __init__(self, K, eng, R, W, sem):
        self.K, self.eng, self.R, self.W, self.sem = K, eng, R, W, sem

    def __getattr__(self, name):
        def f(*a, **kw):
            self.K._record(self.eng, name, a, kw, self.R, self.W, self.sem)
        return f


class Kern:
    ENGS = ("pe", "act", "dve", "pool", "sp")

    def __init__(self, nc, es):
        self.nc, self.es = nc, es
        self.ops = {e: [] for e in self.ENGS}
        self.cnt = {e: 0 for e in self.ENGS}
        self.pending = {e: [] for e in self.ENGS}
        self.dsem = {}
        self.pad_ap = None
        self.csem = {e: es.enter_context(nc.semaphore("cs_" + e)) for e in ("pe", "act", "dve", "pool")}

    def pe(self, R=(), W=()): return _Rec(self, "pe", R, W, None)
    def act(self, R=(), W=()): return _Rec(self, "act", R, W, None)
    def dve(self, R=(), W=()): return _Rec(self, "dve", R, W, None)
    def pool(self, R=(), W=()): return _Rec(self, "pool", R, W, None)
    def dma(self, q, sem, R=(), W=()): return _Rec(self, q, R, W, sem)

    def _record(self, eng, name, a, kw, R, W, sem):
        toks = list(self.pending[eng])
        self.pending[eng] = []
        for b in R:
            if b.lw is not None:
                toks.append(b.lw)
            if b.psum:
                toks.extend(t for t in b.rd if not (t[0] == "c" and t[1] == eng))
        for b in W:
            if b.lw is not None:
                toks.append(b.lw)
            toks.extend(b.rd)
        if sem is None:
            if eng == "pe":
                toks = [t for t in toks if not (t[0] == "c" and t[1] == "pe")]
            if eng == "dve" and self.pad_ap is not None:
                own = [t[2] for t in toks if t[0] == "c" and t[1] == "dve"]
                toks = [t for t in toks if not (t[0] == "c" and t[1] == "dve")]
                if own:
                    between = self.cnt["dve"] - max(own)
                    for _ in range(max(0, DVE_GAP - between)):
                        self.cnt["dve"] += 1
                        self.ops["dve"].append(([], "memset", (self.pad_ap, 0.0), {}, ("c", "dve", self.cnt["dve"])))
            self.cnt[eng] += 1
            tok = ("c", eng, self.cnt[eng])
        else:
            if sem not in self.dsem:
                self.dsem[sem] = [self.es.enter_context(self.nc.semaphore("ds_%d" % len(self.dsem))), 0]
            self.dsem[sem][1] += 1
            tok = ("d", sem, 16 * self.dsem[sem][1])
        self.ops[eng].append((toks, name, a, kw, tok))
        for b in R:
            b.rd.append(tok)
        for b in W:
            b.lw = tok
            b.rd = []

    def barrier(self):
        toks = [("c", e, self.cnt[e]) for e in ("pe", "act", "dve", "pool") if self.cnt[e] > 0]
        toks += [("d", s, 16 * v[1]) for s, v in self.dsem.items()]
        for e in self.ENGS:
            self.pending[e].extend(toks)

    def finish(self):
        self.barrier()
        for e in self.ENGS:
            self.ops[e].append((self.pending[e], None, (), {}, None))
            self.pending[e] = []

    def emit(self):
        nc = self.nc
        miles = {e: set() for e in self.ENGS}
        for e in self.ENGS:
            for toks, _, _, _, _ in self.ops[e]:
                for t in toks:
                    if t[0] == "c":
                        miles[t[1]].add(t[2])
        rank = {}
        for e in self.ENGS:
            rank[e] = {idx: r + 1 for r, idx in enumerate(sorted(miles[e]))}
        with nc.Block() as block:
            for ename, attr in (("pe", "tensor"), ("act", "scalar"), ("dve", "vector"), ("pool", "gpsimd"), ("sp", "sync")):
                ops = self.ops[ename]

                def body(e, ops=ops, ename=ename):
                    seen = {}
                    for toks, name, a, kw, tok in ops:
                        need = {}
                        for t in toks:
                            if t[0] == "c":
                                key, val = ("c", t[1]), rank[t[1]][t[2]]
                            else:
                                key, val = ("d", t[1]), t[2]
                            if seen.get(key, 0) >= val:
                                continue
                            need[key] = max(need.get(key, 0), val)
                        for key, val in need.items():
                            seen[key] = val
                            sem = self.csem[key[1]] if key[0] == "c" else self.dsem[key[1]][0]
                            e.wait_ge(sem, val)
                        if name is None:
                            continue
                        ins = getattr(e, name)(*a, **kw)
                        if DEBUG_NAMES is not None:
                            DEBUG_NAMES[ins.ins.name] = (ename, name, {k: str(v) for k, v in kw.items() if k.startswith("op") or k == "func"})
                        if tok[0] == "c":
                            if tok[2] in rank[ename]:
                                ins.then_inc(self.csem[ename], 1)
                        else:
                            ins.then_inc(self.dsem[tok[1]][0], 16)

                getattr(block, attr)(body)


def build(NS):
    S = 1024 * NS
    NT = S // 128
    NOWN = NS * 128
    NTOK = NOWN + 128
    nc = bass.Bass("TRN2", target_bir_lowering=False)
    es = ExitStack()
    K = Kern(nc, es)

    def din(name, shape, dt=F32):
        return nc.dram_tensor(name, list(shape), dt, kind="ExternalInput").ap()

    x_all = din("x_all", [S, D])
    x_own = din("x_own", [NTOK, D])
    posT_all = din("posT_all", [128, NT], I32)
    posT_own = din("posT_own", [128, NS], I32)
    mem = din("mem", [256, D])
    w_in = din("w_in", [D, IN_COLS])
    gmix_d = din("gmix_rep", [128, D])
    gffn_d = din("gffn_rep", [128, D])
    gmem_d = din("gmem_rep", [128, D])
    bgT_d = din("bgT", [128, 48])
    wpg_d = din("w_pool_grp", [4, 192, 192])
    pscale_d = din("pscaleT", [96, 8])
    gq_d = din("gq_rep", [128, 128])
    gk_d = din("gk_rep", [128, 128])
    gxq_d = din("gxq_rep", [128, 128])
    gxk_d = din("gxk_rep", [128, 128])
    wmkv_d = din("w_mem_kv", [D, 1024])
    wpo_d = din("w_pool_out", [768, D])
    wao_d = din("w_attn_out", [768, D])
    wco_d = din("w_cross_out", [512, D])
    wo_d = din("w_o", [D, D])
    wr_d = din("w_router", [D, 20])
    weg_d = din("w_e_gate", [NE, D, FF])
    weu_d = din("w_e_up", [NE, D, FF])
    wed_d = din("w_e_down", [NE, FF, D])
    ident_d = din("ident", [128, 128])
    invf_d = din("invfT", [128, 192])
    offs_d = din("offsT", [128, 192])
    tailmask_d = din("tailmask", [128, 1024])
    invcnt_d = din("invcnt", [96, 4, NOWN])
    y_out = nc.dram_tensor("y_own", [NOWN, D], F32, kind="ExternalOutput").ap()
    KT_scr = nc.dram_tensor("KT_scr", [6, 128, S], BF16, kind="Internal").ap()
    V_scr = nc.dram_tensor("V_scr", [6, 128, NT, VW], BF16, kind="Internal").ap()
    hT_scr = nc.dram_tensor("hT_scr", [128, KC, NTOK], BF16, kind="Internal").ap()
    aT_scr = nc.dram_tensor("aT_scr", [128, 6, NOWN], BF16, kind="Internal").ap()
    mT_scr = nc.dram_tensor("mT_scr", [128, KC, NOWN], BF16, kind="Internal").ap()

    def sb(st, name, shape, dt=F32):
        t = st.enter_context(nc.sbuf_tensor("s_" + name, list(shape), dt))
        return t, Buf(name)

    def ps(st, name, shape, dt=F32):
        t = st.enter_context(nc.psum_tensor("p_" + name, list(shape), dt))
        return t, Buf(name, psum=True)

    Bx_all, Bx_own, Bw = Buf("x_all"), Buf("x_own"), Buf("weights")
    BKT, BV, BhT, By = Buf("KT_scr"), Buf("V_scr"), Buf("hT_scr"), Buf("y")
    BaT, BmT = Buf("aT_scr"), Buf("mT_scr")

    identf, Bidf = sb(es, "identf", [128, 128])
    identb, Bidb = sb(es, "identb", [128, 128], BF16)
    invf, Binvf = sb(es, "invf", [128, 192])
    offs, Boffs = sb(es, "offs", [128, 192])
    K.dma("sp", Bidf, W=[Bidf]).dma_start(out=identf[:], in_=ident_d[:, :])
    K.dma("sp", Binvf, W=[Binvf]).dma_start(out=invf[:], in_=invf_d[:, :])
    K.dma("sp", Boffs, W=[Boffs]).dma_start(out=offs[:], in_=offs_d[:, :])
    K.dve(R=[Bidf], W=[Bidb]).tensor_copy(identb[:], identf[:])

    def rms_stats(xt_ap, Bxt, junk_ap, Bjunk, ss, Bss, sd, Bsd, rstd, Brstd, width):
        K.dve(W=[Bss]).memset(ss, 0.0)
        K.act(R=[Bxt, Bss], W=[Bjunk, Bss]).activation(out=junk_ap, in_=xt_ap, func=AF.Square, accum_out=ss)
        K.act(R=[Bss], W=[Bsd]).activation(out=sd, in_=ss, func=AF.Sqrt, bias=EPS, scale=1.0 / width)
        K.dve(R=[Bsd], W=[Brstd]).reciprocal(rstd, sd)

    def trig_dve(posf_col, Bposf, ang, Bang, angi, Bangi):
        K.dve(R=[Binvf, Boffs, Bposf], W=[Bang]).scalar_tensor_tensor(
            out=ang[:, 0:192], in0=invf[:], scalar=posf_col, in1=offs[:], op0=ALU.mult, op1=ALU.add)
        K.dve(R=[Bang], W=[Bang]).tensor_scalar(ang[:, 192:384], ang[:, 0:192], 1.0 / TWO_PI, None, op0=ALU.mult)
        K.dve(R=[Bang], W=[Bangi]).tensor_copy(angi[:], ang[:, 192:384])
        K.dve(R=[Bangi], W=[Bang]).tensor_copy(ang[:, 192:384], angi[:])
        K.dve(R=[Bang], W=[Bang]).scalar_tensor_tensor(
            out=ang[:, 0:192], in0=ang[:, 192:384], scalar=-CW1, in1=ang[:, 0:192], op0=ALU.mult, op1=ALU.add)
        K.dve(R=[Bang], W=[Bang]).scalar_tensor_tensor(
            out=ang[:, 0:192], in0=ang[:, 192:384], scalar=-CW2, in1=ang[:, 0:192], op0=ALU.mult, op1=ALU.add)
        K.dve(R=[Bang], W=[Bang]).tensor_scalar(ang[:, 0:192], ang[:, 0:192], -PI_LO, PI_LO, op0=ALU.max, op1=ALU.min)

    def rotary(eng, src, Bsrc, dst, Bdst, cosv, sinv, Bcs, tmp, Btmp, nh, hd):
        x1, x2 = src[:, :, 0:hd], src[:, :, hd:2 * hd]
        cb = cosv.unsqueeze(1).to_broadcast([128, nh, hd])
        sbb = sinv.unsqueeze(1).to_broadcast([128, nh, hd])
        t1, t2 = tmp[:, 0:nh, 0:hd], tmp[:, 0:nh, hd:2 * hd]
        eng(R=[Bsrc, Bcs], W=[Btmp]).tensor_tensor(out=t1, in0=x1, in1=cb, op=ALU.mult)
        eng(R=[Bsrc, Bcs], W=[Btmp]).tensor_tensor(out=t2, in0=x2, in1=sbb, op=ALU.mult)
        eng(R=[Btmp], W=[Bdst]).tensor_tensor(out=dst[:, :, 0:hd], in0=t1, in1=t2, op=ALU.subtract)
        eng(R=[Bsrc, Bcs], W=[Btmp]).tensor_tensor(out=t1, in0=x2, in1=cb, op=ALU.mult)
        eng(R=[Bsrc, Bcs], W=[Btmp]).tensor_tensor(out=t2, in0=x1, in1=sbb, op=ALU.mult)
        eng(R=[Btmp], W=[Bdst]).tensor_tensor(out=dst[:, :, hd:2 * hd], in0=t1, in1=t2, op=ALU.add)

    def head_fac(raw, Braw, nh, sq, Bsq, ssq, Bssq, fac, Bfac):
        K.act(R=[Braw], W=[Bsq]).activation(out=sq[:, 0:nh * 128], in_=raw, func=AF.Square)
        K.dve(R=[Bsq], W=[Bssq]).tensor_reduce(
            out=ssq[:, 0:nh], in_=sq[:, 0:nh * 128].rearrange("p (h d) -> p h d", h=nh), axis=AX.X, op=ALU.add)
        K.act(R=[Bssq], W=[Bssq]).activation(out=ssq[:, 0:nh], in_=ssq[:, 0:nh], func=AF.Sqrt, bias=EPS, scale=1.0 / 128)
        K.dve(R=[Bssq], W=[Bfac]).reciprocal(fac[:, 0:nh], ssq[:, 0:nh])

    stAC = ExitStack()
    kiT, BkiT = sb(stAC, "kiT", [64, S], BF16)
    qT, BqT = sb(stAC, "qT", [128, 6, NOWN], BF16)
    qiT, BqiT = sb(stAC, "qiT", [64, 4, NOWN], BF16)
    sgn, Bsgn = sb(stAC, "sgn", [128, NS, 4])

    stAB = ExitStack()
    gmix, Bgmix = sb(stAB, "gmix", [128, D])
    gk, Bgk = sb(stAB, "gk", [128, 128])
    gq, Bgq = sb(stAB, "gq", [128, 128])
    posA_i, BposAi = sb(stAB, "posA_i", [128, NT], I32)
    posA, BposA = sb(stAB, "posA", [128, NT])
    posO_i, BposOi = sb(stAB, "posO_i", [128, NS], I32)
    posO, BposO = sb(stAB, "posO", [128, NS])
    WA, BWA = sb(stAB, "WA", [128, KC, 1600], BF16)
    xt = [sb(stAB, "xt%d" % i, [128, D]) for i in range(2)]
    xb = [sb(stAB, "xb%d" % i, [128, D], BF16) for i in range(2)]
    xT = [sb(stAB, "xT%d" % i, [128, KC, 128], BF16) for i in range(2)]
    st_ss = [sb(stAB, "ss%d" % i, [128, 1]) for i in range(2)]
    st_sd = [sb(stAB, "sd%d" % i, [128, 1]) for i in range(2)]
    st_rs = [sb(stAB, "rs%d" % i, [128, 1]) for i in range(2)]
    ang = [sb(stAB, "ang%d" % i, [128, 384]) for i in range(2)]
    angi = [sb(stAB, "angi%d" % i, [128, 192], I32) for i in range(2)]
    cs = [sb(stAB, "cs%d" % i, [128, 192]) for i in range(2)]
    Ksb = [sb(stAB, "Ksb%d" % i, [128, 6, 128]) for i in range(2)]
    sq, Bsq = sb(stAB, "sq", [128, 768])
    ssq, Bssq = sb(stAB, "ssq", [128, 6])
    fac, Bfac = sb(stAB, "fac", [128, 6])
    kn, Bkn = sb(stAB, "kn", [128, 6, 128])
    rtmp, Brtmp = sb(stAB, "rtmp", [128, 6, 128])
    kr = [sb(stAB, "kr%d" % i, [128, 6, 128], BF16) for i in range(3)]
    kif = [sb(stAB, "kif%d" % i, [128, 4, 64]) for i in range(2)]
    itmp, Bitmp = sb(stAB, "itmp", [128, 4, 64])
    kir = [sb(stAB, "kir%d" % i, [128, 4, 64], BF16) for i in range(3)]
    KTst = [sb(stAB, "KTst%d" % i, [128, 6, 512], BF16) for i in range(2)]
    Vst = [sb(stAB, "Vst%d" % i, [128, 6, 4, VW], BF16) for i in range(2)]
    wisb = [sb(stAB, "wis%d" % i, [128, 4]) for i in range(2)]
    aw, Baw = sb(stAB, "aw", [128, 4])
    psT = [ps(stAB, "psT%d" % i, [128, 1024], BF16) for i in range(2)]
    psA = [ps(stAB, "psA%d" % i, [128, 512]) for i in range(4)]
    psK, BpsK = ps(stAB, "psK", [128, 1024], BF16)
    psK2 = ps(stAB, "psK2", [128, 1024], BF16)

    K.dma("sp", Bgmix, W=[Bgmix]).dma_start(out=gmix[:], in_=gmix_d[:, :])
    K.dma("sp", Bgk, W=[Bgk]).dma_start(out=gk[:], in_=gk_d[:, :])
    K.dma("sp", Bgq, W=[Bgq]).dma_start(out=gq[:], in_=gq_d[:, :])
    K.dma("sp", BposAi, W=[BposAi]).dma_start(out=posA_i[:], in_=posT_all[:, :])
    K.dma("sp", BposOi, W=[BposOi]).dma_start(out=posO_i[:], in_=posT_own[:, :])
    K.dve(R=[BposAi], W=[BposA]).tensor_copy(posA[:], posA_i[:])
    K.dve(R=[BposOi], W=[BposO]).tensor_copy(posO[:], posO_i[:])
    for c0 in range(0, 1536, 512):
        K.dma("pool", BWA, R=[Bw], W=[BWA]).dma_start(
            out=WA[:, :, c0:c0 + 512], in_=w_in[:, C_K + c0:C_K + c0 + 512].rearrange("(k p) c -> p k c", p=128))
    K.dma("pool", BWA, R=[Bw], W=[BWA]).dma_start(
        out=WA[:, :, 1536:1600], in_=w_in[:, C_KI:C_KI + 64].rearrange("(k p) c -> p k c", p=128))
    for i in range(2):
        K.pool(W=[Vst[i][1]]).memset(Vst[i][0][:], 1.0)

    colsA = [(0, 512), (512, 512), (1024, 512), (1536, 64)]
    colsB = [(0, 512), (512, 256), (768, 324)]
    items = [("A", j) for j in range(NT)] + [("B", i) for i in range(NS + 1)]

    def stL(g):
        kind, t = items[g]
        src_rows, Bsrc = (x_all[t * 128:(t + 1) * 128, :], Bx_all) if kind == "A" else (x_own[t * 128:(t + 1) * 128, :], Bx_own)
        x_t, Bx_t = xt[g % 2]
        K.dma("sp", Bx_t, R=[Bsrc], W=[Bx_t]).dma_start(out=x_t[:], in_=src_rows)

    def stN(g):
        b = g % 2
        (x_t, Bx_t), (xb_t, Bxb_t) = xt[b], xb[b]
        (ss, Bss), (sd, Bsd), (rs, Brs) = st_ss[b], st_sd[b], st_rs[b]
        rms_stats(x_t[:], Bx_t, xb_t[:], Bxb_t, ss[:], Bss, sd[:], Bsd, rs[:], Brs, D)
        K.dve(R=[Bx_t, Brs, Bgmix], W=[Bxb_t]).scalar_tensor_tensor(
            out=xb_t[:], in0=x_t[:], scalar=rs[:, 0:1], in1=gmix[:], op0=ALU.mult, op1=ALU.mult)

    def stT(g):
        kind, t = items[g]
        b = g % 2
        (xb_t, Bxb_t), (xT_t, BxT_t) = xb[b], xT[b]
        for half in range(2):
            pT, BpT = psT[half]
            for kk in range(8):
                kc = half * 8 + kk
                K.pe(R=[Bxb_t, Bidb], W=[BpT]).transpose(pT[:, kk * 128:(kk + 1) * 128], xb_t[:, kc * 128:(kc + 1) * 128], identb[:])
            dst = xT_t[:, half * 8:(half + 1) * 8, :].rearrange("p k t -> p (k t)")
            if half == 0:
                K.act(R=[BpT], W=[BxT_t]).copy(dst, pT[:, :])
            else:
                K.dve(R=[BpT], W=[BxT_t]).tensor_copy(dst, pT[:, :])
        if kind == "B":
            K.dma("sp", BxT_t, R=[BxT_t], W=[BhT]).dma_start(out=hT_scr[:, :, t * 128:(t + 1) * 128], in_=xT_t[:])

    def stM(g):
        kind, t = items[g]
        b = g % 2
        xT_t, BxT_t = xT[b]
        Ks, BKs = Ksb[b]
        Ksf = Ks[:].rearrange("p h d -> p (h d)")
        ki_f, Bki_f = kif[b]
        if kind == "B" and t == 0:
            K.dma("pool", BWA, R=[Bw], W=[BWA]).dma_start(
                out=WA[:, :, 0:512], in_=w_in[:, C_Q:C_Q + 512].rearrange("(k p) c -> p k c", p=128))
            K.dma("pool", BWA, R=[Bw], W=[BWA]).dma_start(
                out=WA[:, :, 512:768], in_=w_in[:, C_Q + 512:C_Q + 768].rearrange("(k p) c -> p k c", p=128))
            K.dma("pool", BWA, R=[Bw], W=[BWA]).dma_start(
                out=WA[:, :, 768:1092], in_=w_in[:, C_QI:C_QI + 324].rearrange("(k p) c -> p k c", p=128))
        if kind == "B" and t == NS:
            return
        pos_col, Bpos = (posA[:, t:t + 1], BposA) if kind == "A" else (posO[:, t:t + 1], BposO)
        trig_dve(pos_col, Bpos, ang[b][0], ang[b][1], angi[b][0], angi[b][1])
        cols = colsA if kind == "A" else colsB
        for kc in range(KC):
            for cg, (c0, w) in enumerate(cols):
                K.pe(R=[BxT_t, BWA], W=[psA[cg][1]]).matmul(
                    psA[cg][0][:, 0:w], lhsT=xT_t[:, kc, :], rhs=WA[:, kc, c0:c0 + w], start=(kc == 0), stop=(kc == KC - 1))
        K.act(R=[psA[0][1]], W=[BKs]).copy(Ksf[:, 0:512], psA[0][0][:, 0:512])
        K.act(R=[psA[1][1]], W=[BKs]).copy(Ksf[:, 512:768], psA[1][0][:, 0:256])
        if kind == "A":
            Vs, BVs = Vst[(t // 4) % 2]
            jj = t % 4
            K.act(R=[psA[1][1]], W=[BVs]).copy(Vs[:, 0:2, jj, 0:128], psA[1][0][:, 256:512].rearrange("p (h d) -> p h d", h=2))
            K.act(R=[psA[2][1]], W=[BVs]).copy(Vs[:, 2:6, jj, 0:128], psA[2][0][:, 0:512].rearrange("p (h d) -> p h d", h=4))
            K.act(R=[psA[3][1]], W=[Bki_f]).copy(ki_f[:, 0, :], psA[3][0][:, 0:64])
        else:
            K.act(R=[psA[2][1]], W=[Bki_f]).copy(ki_f[:], psA[2][0][:, 0:256].rearrange("p (h d) -> p h d", h=4))
            K.act(R=[psA[2][1]], W=[wisb[b][1]]).copy(wisb[b][0][:], psA[2][0][:, 320:324])
        K.act(R=[ang[b][1]], W=[cs[b][1]]).activation(out=cs[b][0][:], in_=ang[b][0][:, 0:192], func=AF.Sin)

    def stR(g):
        kind, t = items[g]
        if kind == "B" and t == NS:
            return
        b = g % 2
        b3 = g % 3
        Ks, BKs = Ksb[b]
        cs_t, Bcs_t = cs[b]
        ki_f, Bki_f = kif[b]
        ki_r, Bki_r = kir[b3]
        kr_t, Bkr_t = kr[b3]
        gain, Bgain = (gk, Bgk) if kind == "A" else (gq, Bgq)
        Ksf = Ks[:].rearrange("p h d -> p (h d)")
        head_fac(Ksf, BKs, 6, sq, Bsq, ssq, Bssq, fac, Bfac)
        K.dve(R=[BKs, Bfac], W=[Bkn]).tensor_tensor(out=kn[:], in0=Ks[:], in1=fac[:, 0:6].unsqueeze(2).to_broadcast([128, 6, 128]), op=ALU.mult)
        K.pool(R=[Bkn, Bgain], W=[Bkn]).tensor_tensor(out=kn[:], in0=kn[:], in1=gain[:].unsqueeze(1).to_broadcast([128, 6, 128]), op=ALU.mult)
        rotary(K.pool, kn, Bkn, kr_t, Bkr_t, cs_t[:, 64:128], cs_t[:, 0:64], Bcs_t, rtmp, Brtmp, 6, 64)
        if kind == "A":
            rotary(K.pool, ki_f[:, 0:1, :], Bki_f, ki_r[:, 0:1, :], Bki_r, cs_t[:, 160:192], cs_t[:, 128:160], Bcs_t, itmp, Bitmp, 1, 32)
        else:
            wis, Bwis = wisb[b]
            K.dve(R=[Bwis], W=[Bsgn]).tensor_scalar(sgn[:, t, :], wis[:], 0.0, 2.0, op0=ALU.is_ge, op1=ALU.mult)
            K.dve(R=[Bsgn], W=[Bsgn]).tensor_scalar(sgn[:, t, :], sgn[:, t, :], -1.0, None, op0=ALU.add)
            K.dve(R=[Bwis, Bsgn], W=[Baw]).scalar_tensor_tensor(out=aw[:], in0=wis[:], scalar=1.0 / 16, in1=sgn[:, t, :], op0=ALU.mult, op1=ALU.mult)
            rotary(K.pool, ki_f, Bki_f, itmp, Bitmp, cs_t[:, 160:192], cs_t[:, 128:160], Bcs_t, rtmp, Brtmp, 4, 32)
            K.pool(R=[Bitmp, Baw], W=[Bki_r]).tensor_tensor(out=ki_r[:], in0=itmp[:], in1=aw[:].unsqueeze(2).to_broadcast([128, 4, 64]), op=ALU.mult)

    def stO(g):
        kind, t = items[g]
        if kind == "B" and t == NS:
            return
        b3 = g % 3
        ki_r, Bki_r = kir[b3]
        kr_t, Bkr_t = kr[b3]
        for h in range(6):
            K.pe(R=[Bkr_t, Bidb], W=[BpsK]).transpose(psK[:, h * 128:(h + 1) * 128], kr_t[:, h, :], identb[:])
        if kind == "A":
            sbi = (t // 4) % 2
            jj = t % 4
            Vs, BVs = Vst[sbi]
            KTs, BKTs = KTst[sbi]
            K.pe(R=[Bki_r, Bidb], W=[psK2[1]]).transpose(psK2[0][0:64, 0:128], ki_r[:, 0, :], identb[:])
            K.act(R=[BpsK], W=[BKTs]).copy(KTs[:, :, jj * 128:(jj + 1) * 128], psK[:, 0:768].rearrange("p (h t) -> p h t", h=6))
            K.dve(R=[psK2[1]], W=[BkiT]).tensor_copy(kiT[:, t * 128:(t + 1) * 128], psK2[0][0:64, 0:128])
            if jj == 3:
                t0 = (t - 3) * 128
                K.dma("sp", BKTs, R=[BKTs], W=[BKT]).dma_start(
                    out=KT_scr[:, :, t0:t0 + 512].rearrange("h d t -> d h t"), in_=KTs[:])
                K.dma("sp", BVs, R=[BVs], W=[BV]).dma_start(
                    out=V_scr[:, :, t - 3:t + 1, :].rearrange("h p j c -> p h j c"), in_=Vs[:])
        else:
            K.act(R=[BpsK], W=[BqT]).copy(qT[:, :, t * 128:(t + 1) * 128], psK[:, 0:768].rearrange("p (h t) -> p h t", h=6))
            for h in range(4):
                K.pe(R=[Bki_r, Bidb], W=[psK2[1]]).transpose(psK2[0][0:64, h * 128:(h + 1) * 128], ki_r[:, h, :], identb[:])
            K.dve(R=[psK2[1]], W=[BqiT]).tensor_copy(qiT[:, :, t * 128:(t + 1) * 128], psK2[0][0:64, 0:512].rearrange("p (h t) -> p h t", h=4))

    NI = len(items)
    stL(0)
    stL(1)
    stN(0)
    stL(2)
    stN(1)
    stT(0)
    for n in range(NI + 3):
        if n + 3 < NI:
            stL(n + 3)
        if n + 2 < NI:
            stN(n + 2)
        if n + 1 < NI:
            stT(n + 1)
        if 0 <= n - 3 < NI:
            stO(n - 3)
        if 0 <= n - 1 < NI:
            stR(n - 1)
        if n < NI:
            stM(n)
    K.barrier()
    stAB.close()

    stC = ExitStack()
    LMAX = S
    PIECE = 1024
    U8 = mybir.dt.uint8
    sm2 = [sb(stC, "sm%d" % i, [128, LMAX]) for i in range(2)]
    cjunk, Bcjunk = sb(stC, "cjunk", [128, LMAX], U8)
    tmask, Btmask = sb(stC, "tmask", [128, 1024])
    ttmp, Bttmp = sb(stC, "ttmp", [128, 1024])
    rh = [sb(stC, "rh%d" % i, [128, 512]) for i in range(4)]
    mb = [sb(stC, "mb%d" % i, [128, 1024], BF16) for i in range(2)]
    maskT2 = [sb(stC, "maskT%d" % i, [128, LMAX // 128, 128], BF16) for i in range(2)]
    KTp = [sb(stC, "KTp%d" % i, [128, PIECE], BF16) for i in range(3)]
    Vp = [sb(stC, "Vp%d" % i, [128, PIECE // 128, VW], BF16) for i in range(3)]
    pT_ = [sb(stC, "pT%d" % i, [128, 512], BF16) for i in range(3)]
    hi0, Bhi0 = sb(stC, "hi0", [128, 1])
    m1, Bm1 = sb(stC, "m1", [128, 1])
    m2, Bm2 = sb(stC, "m2", [128, 1])
    lo, Blo = sb(stC, "lo", [128, 1])
    w0, Bw0 = sb(stC, "w0", [128, 1])
    mid, Bmid = sb(stC, "mid", [128, 1])
    gew, Bgew = sb(stC, "gew", [128, 1])
    cnt, Bcnt = sb(stC, "cnt", [128, 32])
    pw, Bpw = sb(stC, "pw", [128, NITER])
    wt, Bwt = sb(stC, "wt", [128, NITER])
    wt2, Bwt2 = sb(stC, "wt2", [128, NITER])
    zcol, Bzcol = sb(stC, "zcol", [128, 1])
    bb = [sb(stC, "bb%d" % i, [128, 1]) for i in range(2)]
    aa = [sb(stC, "aa%d" % i, [128, 1]) for i in range(2)]
    osb2 = [sb(stC, "osb%d" % i, [128, 6, VW]) for i in range(2)]
    rden, Brden = sb(stC, "rden", [128, 6])
    attn_b, Battn_b = sb(stC, "attn_b", [128, 6, 128], BF16)
    aTst = [sb(stC, "aTst%d" % i, [128, 6, 128], BF16) for i in range(2)]
    psI = [ps(stC, "psI%d" % i, [128, 512]) for i in range(2)]
    psM, BpsM = ps(stC, "psM", [128, 1024], BF16)
    psL = [ps(stC, "psL%d" % i, [128, 512]) for i in range(2)]
    psO = [ps(stC, "psO%d" % i, [128, 512]) for i in range(2)]
    psX, BpsX = ps(stC, "psX", [128, 1024], BF16)

    K.dma("sp", Btmask, W=[Btmask]).dma_start(out=tmask[:], in_=tailmask_d[:, :])
    cstate = {"rh": 0, "pt": 0, "kv": 0}
    K.pool(W=[Bzcol]).memset(zcol[:], 0.0)
    for it in range(NITER):
        K.pool(W=[Bpw]).memset(pw[:, it:it + 1], float(2.0 ** -(it + 2)))

    def c_indexer(s):
        L = 1024 * (s + 1)
        NG = L // 512
        tcol = slice(s * 128, (s + 1) * 128)
        sm, Bsm = sm2[s % 2]
        for g in range(NG):
            for h in range(4):
                pI, BpI = psI[(g * 4 + h) % 2]
                K.pe(R=[BqiT, BkiT], W=[BpI]).matmul(pI[:, :], lhsT=qiT[:, h, tcol], rhs=kiT[:, g * 512:(g + 1) * 512], start=True, stop=True)
                r_t, Br_t = rh[cstate["rh"] % 4]
                cstate["rh"] += 1
                K.act(R=[BpI], W=[Br_t]).activation(out=r_t[:], in_=pI[:, :], func=AF.Relu)
                dst = sm[:, g * 512:(g + 1) * 512]
                if h == 0:
                    if g >= NG - 2:
                        tg = g - (NG - 2)
                        K.dve(R=[Br_t, Bsgn, Btmask], W=[Bsm]).scalar_tensor_tensor(
                            out=dst, in0=r_t[:], scalar=sgn[:, s, 0:1], in1=tmask[:, tg * 512:(tg + 1) * 512], op0=ALU.mult, op1=ALU.add)
                    else:
                        K.dve(R=[Br_t, Bsgn], W=[Bsm]).tensor_scalar(dst, r_t[:], sgn[:, s, 0:1], None, op0=ALU.mult)
                else:
                    K.dve(R=[Br_t, Bsgn, Bsm], W=[Bsm]).scalar_tensor_tensor(
                        out=dst, in0=r_t[:], scalar=sgn[:, s, h:h + 1], in1=dst, op0=ALU.mult, op1=ALU.add)

    def c_threshold(s):
        L = 1024 * (s + 1)
        sm, Bsm = sm2[s % 2]
        maskT, BmaskT = maskT2[s % 2]
        K.dve(R=[Bsm], W=[Bhi0]).tensor_reduce(out=hi0[:], in_=sm[:, 0:L], axis=AX.X, op=ALU.max)
        K.dve(R=[Bsm, Btmask], W=[Bttmp]).scalar_tensor_tensor(
            out=ttmp[:], in0=tmask[:], scalar=-2.0, in1=sm[:, L - 1024:L], op0=ALU.mult, op1=ALU.add)
        K.dve(R=[Bttmp], W=[Bm1]).tensor_reduce(out=m1[:], in_=ttmp[:], axis=AX.X, op=ALU.min)
        if L > 1024:
            K.dve(R=[Bsm], W=[Bm2]).tensor_reduce(out=m2[:], in_=sm[:, 0:L - 1024], axis=AX.X, op=ALU.min)
            K.dve(R=[Bm1, Bm2], W=[Blo]).tensor_tensor(out=lo[:], in0=m1[:], in1=m2[:], op=ALU.min)
        else:
            K.dve(R=[Bm1], W=[Blo]).tensor_copy(lo[:], m1[:])
        K.dve(R=[Bhi0, Blo], W=[Bw0]).tensor_tensor(out=w0[:], in0=hi0[:], in1=lo[:], op=ALU.subtract)
        K.dve(R=[Bw0], W=[Bgew]).tensor_scalar(gew[:], w0[:], 0.01, 1e-6, op0=ALU.mult, op1=ALU.add)
        K.dve(R=[Blo, Bgew], W=[Blo]).tensor_tensor(out=lo[:], in0=lo[:], in1=gew[:], op=ALU.subtract)
        K.dve(R=[Bw0], W=[Bw0]).tensor_scalar(w0[:], w0[:], 1.011, 2e-6, op0=ALU.mult, op1=ALU.add)
        K.dve(R=[Bpw, Bw0], W=[Bwt]).tensor_scalar(wt[:], pw[:], w0[:, 0:1], None, op0=ALU.mult)
        K.dve(R=[Bwt], W=[Bwt2]).tensor_scalar(wt2[:], wt[:], 2.0, None, op0=ALU.mult)
        K.dve(R=[Blo, Bwt2], W=[bb[0][1]]).tensor_tensor(out=bb[0][0][:], in0=lo[:], in1=wt2[:, 0:1], op=ALU.add)
        a_prev, Ba_prev = zcol, Bzcol
        for it in range(NITER):
            b_t, Bb_t = bb[it % 2]
            b_n, Bb_n = bb[(it + 1) % 2]
            a_n, Ba_n = aa[it % 2]
            K.dve(R=[Bsm, Ba_prev, Bb_t], W=[Bcjunk, Bcnt]).scalar_tensor_tensor(
                out=cjunk[:, 0:L], in0=sm[:, 0:L], scalar=a_prev[:, 0:1], in1=b_t[:, 0:1].to_broadcast([128, L]),
                op0=ALU.subtract, op1=ALU.is_ge, accum_out=cnt[:, it:it + 1])
            K.dve(R=[Ba_prev, Bb_t, Bwt], W=[Bb_n]).scalar_tensor_tensor(
                out=b_n[:], in0=a_prev[:], scalar=wt[:, it:it + 1], in1=b_t[:], op0=ALU.subtract, op1=ALU.add)
            K.dve(R=[Bcnt, Bwt2], W=[Ba_n]).tensor_scalar(a_n[:], cnt[:, it:it + 1], 255.5, wt2[:, it:it + 1], op0=ALU.is_ge, op1=ALU.mult)
            a_prev, Ba_prev = a_n, Ba_n
        b_f, Bb_f = bb[NITER % 2]
        K.dve(R=[Ba_prev, Bb_f, Bwt2], W=[Blo]).scalar_tensor_tensor(
            out=lo[:], in0=a_prev[:], scalar=wt2[:, NITER - 1:NITER], in1=b_f[:], op0=ALU.subtract, op1=ALU.add)
        for pc in range(L // 1024):
            m_t, Bm_t = mb[pc % 2]
            K.dve(R=[Bsm, Blo], W=[Bm_t]).tensor_scalar(
                m_t[:], sm[:, pc * 1024:(pc + 1) * 1024], lo[:, 0:1], -30000.0, op0=ALU.is_lt, op1=ALU.mult)
            for c in range(8):
                K.pe(R=[Bm_t, Bidb], W=[BpsM]).transpose(psM[:, c * 128:(c + 1) * 128], m_t[:, c * 128:(c + 1) * 128], identb[:])
            K.act(R=[BpsM], W=[BmaskT]).copy(maskT[:, pc * 8:(pc + 1) * 8, :].rearrange("p c t -> p (c t)"), psM[:, :])

    def c_attention(s):
        L = 1024 * (s + 1)
        NCH = L // 128
        tcol = slice(s * 128, (s + 1) * 128)
        maskT, BmaskT = maskT2[s % 2]
        osb, Bosb = osb2[s % 2]
        for h in range(6):
            pO, BpO = psO[h // 3]
            ocol = (h % 3) * VW
            for p0 in range(0, L, PIECE):
                pw = min(PIECE, L - p0)
                KT_t, BKT_t = KTp[cstate["kv"] % 3]
                V_t, BV_t = Vp[cstate["kv"] % 3]
                cstate["kv"] += 1
                K.dma("sp", BKT_t, R=[BKT], W=[BKT_t]).dma_start(out=KT_t[:, 0:pw], in_=KT_scr[h, :, p0:p0 + pw])
                K.dma("sp", BV_t, R=[BV], W=[BV_t]).dma_start(out=V_t[:, 0:pw // 128, :], in_=V_scr[h, :, p0 // 128:(p0 + pw) // 128, :])
                for gl in range(pw // 512):
                    g = p0 // 512 + gl
                    pL, BpL = psL[g % 2]
                    K.pe(R=[BmaskT, Bidb], W=[BpL]).matmul(
                        pL[:, :], lhsT=identb[:], rhs=maskT[:, g * 4:(g + 1) * 4, :].rearrange("p c t -> p (c t)"),
                        start=True, stop=False, skip_group_check=True)
                    for c in range(4):
                        cl = gl * 4 + c
                        K.pe(R=[BKT_t, BqT], W=[BpL]).matmul(
                            pL[:, c * 128:(c + 1) * 128], lhsT=KT_t[:, cl * 128:(cl + 1) * 128], rhs=qT[:, h, tcol],
                            start=False, stop=(c == 3), skip_group_check=True)
                    p_t, Bp_t = pT_[cstate["pt"] % 3]
                    cstate["pt"] += 1
                    K.act(R=[BpL], W=[Bp_t]).activation(out=p_t[:], in_=pL[:, :], func=AF.Exp, scale=float(128 ** -0.5))
                    for c in range(4):
                        cl = gl * 4 + c
                        ch = g * 4 + c
                        K.pe(R=[Bp_t, BV_t], W=[BpO]).matmul(
                            pO[:, ocol:ocol + VW], lhsT=p_t[:, c * 128:(c + 1) * 128], rhs=V_t[:, cl, :],
                            start=(ch == 0), stop=(ch == NCH - 1), skip_group_check=True)
            K.act(R=[BpO], W=[Bosb]).copy(osb[:, h, :], pO[:, ocol:ocol + VW])

    def c_finalize(s):
        osb, Bosb = osb2[s % 2]
        a_st, Ba_st = aTst[s % 2]
        K.dve(R=[Bosb], W=[Brden]).reciprocal(rden[:], osb[:, :, 128])
        K.dve(R=[Bosb, Brden], W=[Battn_b]).tensor_tensor(
            out=attn_b[:], in0=osb[:, :, 0:128], in1=rden[:].unsqueeze(2).to_broadcast([128, 6, 128]), op=ALU.mult)
        for h in range(6):
            K.pe(R=[Battn_b, Bidb], W=[BpsX]).transpose(psX[:, h * 128:(h + 1) * 128], attn_b[:, h, :], identb[:])
        K.act(R=[BpsX], W=[Ba_st]).copy(a_st[:], psX[:, 0:768].rearrange("p (h t) -> p h t", h=6))
        K.dma("sp", Ba_st, R=[Ba_st], W=[BaT]).dma_start(out=aT_scr[:, :, s * 128:(s + 1) * 128], in_=a_st[:])

    c_indexer(0)
    c_threshold(0)
    for s in range(NS):
        if s + 1 < NS:
            c_indexer(s + 1)
        c_attention(s)
        if s + 1 < NS:
            c_threshold(s + 1)
        c_finalize(s)
    K.barrier()
    stC.close()
    stAC.close()

    stD = ExitStack()
    hT, BhT_s = sb(stD, "hT", [128, KC, NTOK], BF16)
    K.dma("sp", BhT_s, R=[BhT], W=[BhT_s]).dma_start(out=hT[:], in_=hT_scr[:, :, :])
    attnT, BattnT = sb(stD, "attnT2", [128, 6, NOWN], BF16)
    K.dma("sp", BattnT, R=[BaT], W=[BattnT]).dma_start(out=attnT[:], in_=aT_scr[:, :, :])
    crossT, BcrossT = sb(stD, "crossT", [128, 4, NOWN], BF16)
    p2T, Bp2T = sb(stD, "p2T", [96, 8, NOWN], BF16)
    NTC = [(o, min(512, NOWN - o)) for o in range(0, NOWN, 512)]

    stX = ExitStack()
    gmem, Bgmem = sb(stX, "gmem", [128, D])
    gxq, Bgxq = sb(stX, "gxq", [128, 128])
    gxk, Bgxk = sb(stX, "gxk", [128, 128])
    Wkv, BWkv = sb(stX, "Wkv", [128, KC, 1024], BF16)
    Wxq, BWxq = sb(stX, "Wxq", [128, KC, 512], BF16)
    mt = [sb(stX, "mt%d" % i, [128, D]) for i in range(2)]
    mbf, Bmbf = sb(stX, "mbf", [128, D], BF16)
    mjunk, Bmjunk = sb(stX, "mjunk", [128, D], BF16)
    memT, BmemT = sb(stX, "memT", [128, KC, 256], BF16)
    xs_ss = [sb(stX, "xss%d" % i, [128, 1]) for i in range(2)]
    xs_sd = [sb(stX, "xsd%d" % i, [128, 1]) for i in range(2)]
    xs_rs = [sb(stX, "xrs%d" % i, [128, 1]) for i in range(2)]
    kraw, Bkraw = sb(stX, "kraw", [128, 4, 128])
    xsq, Bxsq = sb(stX, "xsq", [128, 512])
    xssq, Bxssq = sb(stX, "xssq", [128, 4])
    xfac, Bxfac = sb(stX, "xfac", [128, 4])
    knb, Bknb = sb(stX, "knb", [128, 4, 128], BF16)
    kmT, BkmT = sb(stX, "kmT", [128, 4, 256], BF16)
    vm, Bvm = sb(stX, "vm", [128, 2, 4, VW], BF16)
    xqT, BxqT = sb(stX, "xqT", [128, 4, NOWN], BF16)
    xp = [sb(stX, "xp%d" % i, [128, 2, 128], BF16) for i in range(2)]
    xo, Bxo = sb(stX, "xo", [128, 4, VW])
    xrd, Bxrd = sb(stX, "xrd", [128, 4])
    cross_b, Bcross_b = sb(stX, "cross_b", [128, 4, 128], BF16)
    psXT = [ps(stX, "psXT%d" % i, [128, 1024], BF16) for i in range(2)]
    psXA = [ps(stX, "psXA%d" % i, [128, 512]) for i in range(2)]
    psXL, BpsXL = ps(stX, "psXL", [128, 512])
    psXO, BpsXO = ps(stX, "psXO", [128, 512])
    psXK, BpsXK = ps(stX, "psXK", [128, 1024], BF16)

    K.dma("sp", Bgmem, W=[Bgmem]).dma_start(out=gmem[:], in_=gmem_d[:, :])
    K.dma("sp", Bgxq, W=[Bgxq]).dma_start(out=gxq[:], in_=gxq_d[:, :])
    K.dma("sp", Bgxk, W=[Bgxk]).dma_start(out=gxk[:], in_=gxk_d[:, :])
    for c0 in range(0, 1024, 512):
        K.dma("pool", BWkv, R=[Bw], W=[BWkv]).dma_start(
            out=Wkv[:, :, c0:c0 + 512], in_=wmkv_d[:, c0:c0 + 512].rearrange("(k p) c -> p k c", p=128))
    K.dma("pool", BWxq, R=[Bw], W=[BWxq]).dma_start(
        out=Wxq[:], in_=w_in[:, C_XQ:C_XQ + 512].rearrange("(k p) c -> p k c", p=128))
    K.pool(W=[Bvm]).memset(vm[:], 1.0)
    for mtile in range(2):
        (m_t, Bm_t) = mt[mtile]
        (ss, Bss), (sd, Bsd), (rs, Brs) = xs_ss[mtile], xs_sd[mtile], xs_rs[mtile]
        K.dma("sp", Bm_t, W=[Bm_t]).dma_start(out=m_t[:], in_=mem[mtile * 128:(mtile + 1) * 128, :])
        rms_stats(m_t[:], Bm_t, mjunk[:], Bmjunk, ss[:], Bss, sd[:], Bsd, rs[:], Brs, D)
        K.dve(R=[Bm_t, Brs, Bgmem], W=[Bmbf]).scalar_tensor_tensor(
            out=mbf[:], in0=m_t[:], scalar=rs[:, 0:1], in1=gmem[:], op0=ALU.mult, op1=ALU.mult)
        for half in range(2):
            pT, BpT = psXT[half]
            for kk in range(8):
                kc = half * 8 + kk
                K.pe(R=[Bmbf, Bidb], W=[BpT]).transpose(pT[:, kk * 128:(kk + 1) * 128], mbf[:, kc * 128:(kc + 1) * 128], identb[:])
            K.act(R=[BpT], W=[BmemT]).copy(memT[:, half * 8:(half + 1) * 8, mtile * 128:(mtile + 1) * 128], pT[:, :].rearrange("p (k t) -> p k t", k=8))
        for kc in range(KC):
            for cg in range(2):
                K.pe(R=[BmemT, BWkv], W=[psXA[cg][1]]).matmul(
                    psXA[cg][0][:, :], lhsT=memT[:, kc, mtile * 128:(mtile + 1) * 128], rhs=Wkv[:, kc, cg * 512:(cg + 1) * 512],
                    start=(kc == 0), stop=(kc == KC - 1))
        krf = kraw[:].rearrange("p h d -> p (h d)")
        K.act(R=[psXA[0][1]], W=[Bkraw]).copy(krf, psXA[0][0][:, :])
        K.act(R=[psXA[1][1]], W=[Bvm]).copy(vm[:, mtile, :, 0:128], psXA[1][0][:, :].rearrange("p (h d) -> p h d", h=4))
        head_fac(krf, Bkraw, 4, xsq, Bxsq, xssq, Bxssq, xfac, Bxfac)
        K.dve(R=[Bkraw, Bxfac], W=[Bkraw]).tensor_tensor(out=kraw[:], in0=kraw[:], in1=xfac[:].unsqueeze(2).to_broadcast([128, 4, 128]), op=ALU.mult)
        K.dve(R=[Bkraw, Bgxk], W=[Bknb]).tensor_tensor(out=knb[:], in0=kraw[:], in1=gxk[:].unsqueeze(1).to_broadcast([128, 4, 128]), op=ALU.mult)
        for h in range(4):
            K.pe(R=[Bknb, Bidb], W=[BpsXK]).transpose(psXK[:, h * 128:(h + 1) * 128], knb[:, h, :], identb[:])
        K.act(R=[BpsXK], W=[BkmT]).copy(kmT[:, :, mtile * 128:(mtile + 1) * 128], psXK[:, 0:512].rearrange("p (h t) -> p h t", h=4))
    for i in range(NS):
        for kc in range(KC):
            K.pe(R=[BhT_s, BWxq], W=[psXA[0][1]]).matmul(
                psXA[0][0][:, :], lhsT=hT[:, kc, i * 128:(i + 1) * 128], rhs=Wxq[:, kc, :], start=(kc == 0), stop=(kc == KC - 1))
        krf = kraw[:].rearrange("p h d -> p (h d)")
        K.act(R=[psXA[0][1]], W=[Bkraw]).copy(krf, psXA[0][0][:, :])
        head_fac(krf, Bkraw, 4, xsq, Bxsq, xssq, Bxssq, xfac, Bxfac)
        K.dve(R=[Bkraw, Bxfac], W=[Bkraw]).tensor_tensor(out=kraw[:], in0=kraw[:], in1=xfac[:].unsqueeze(2).to_broadcast([128, 4, 128]), op=ALU.mult)
        K.dve(R=[Bkraw, Bgxq], W=[Bknb]).tensor_tensor(out=knb[:], in0=kraw[:], in1=gxq[:].unsqueeze(1).to_broadcast([128, 4, 128]), op=ALU.mult)
        for h in range(4):
            K.pe(R=[Bknb, Bidb], W=[BpsXK]).transpose(psXK[:, h * 128:(h + 1) * 128], knb[:, h, :], identb[:])
        K.act(R=[BpsXK], W=[BxqT]).copy(xqT[:, :, i * 128:(i + 1) * 128], psXK[:, 0:512].rearrange("p (h t) -> p h t", h=4))
    xpc = 0
    for i in range(NS):
        tcol = slice(i * 128, (i + 1) * 128)
        for h in range(4):
            for mc in range(2):
                K.pe(R=[BkmT, BxqT], W=[BpsXL]).matmul(
                    psXL[:, mc * 128:(mc + 1) * 128], lhsT=kmT[:, h, mc * 128:(mc + 1) * 128], rhs=xqT[:, h, tcol], start=True, stop=True)
            p_t, Bp_t = xp[xpc % 2]
            xpc += 1
            K.act(R=[BpsXL], W=[Bp_t]).activation(out=p_t[:].rearrange("p c t -> p (c t)"), in_=psXL[:, 0:256], func=AF.Exp, scale=float(128 ** -0.5))
            for mc in range(2):
                K.pe(R=[Bp_t, Bvm], W=[BpsXO]).matmul(
                    psXO[:, 0:VW], lhsT=p_t[:, mc, :], rhs=vm[:, mc, h, :], start=(mc == 0), stop=(mc == 1))
            K.act(R=[BpsXO], W=[Bxo]).copy(xo[:, h, :], psXO[:, 0:VW])
        K.dve(R=[Bxo], W=[Bxrd]).reciprocal(xrd[:], xo[:, :, 128])
        K.dve(R=[Bxo, Bxrd], W=[Bcross_b]).tensor_tensor(
            out=cross_b[:], in0=xo[:, :, 0:128], in1=xrd[:].unsqueeze(2).to_broadcast([128, 4, 128]), op=ALU.mult)
        for h in range(4):
            K.pe(R=[Bcross_b, Bidb], W=[BpsXK]).transpose(psXK[:, h * 128:(h + 1) * 128], cross_b[:, h, :], identb[:])
        K.act(R=[BpsXK], W=[BcrossT]).copy(crossT[:, :, tcol], psXK[:, 0:512].rearrange("p (h t) -> p h t", h=4))
    K.barrier()
    stX.close()

    stP = ExitStack()
    Wup, BWup = sb(stP, "Wup", [128, KC, 768], BF16)
    Wpg, BWpg = sb(stP, "Wpg", [96, 4, 2, 192], BF16)
    pscale, Bpscale = sb(stP, "pscale", [96, 8])
    invcnt, Binvcnt = sb(stP, "invcnt", [96, 4, NOWN])
    U = [sb(stP, "U%d" % i, [96, NS, 144]) for i in range(2)]
    Wn = [sb(stP, "Wn%d" % i, [96, NS, 144]) for i in range(2)]
    pTt, BpTt = sb(stP, "pTt", [96, 8, NOWN], BF16)
    psU = [ps(stP, "psU%d" % i, [128, 512]) for i in range(3)]
    psG = [ps(stP, "psG%d" % i, [128, 512]) for i in range(2)]
    for c0 in (0, 512):
        w = min(512, 768 - c0)
        K.dma("pool", BWup, R=[Bw], W=[BWup]).dma_start(
            out=Wup[:, :, c0:c0 + w], in_=w_in[:, C_UP + c0:C_UP + c0 + w].rearrange("(k p) c -> p k c", p=128))
    K.dma("pool", BWpg, R=[Bw], W=[BWpg]).dma_start(out=Wpg[:], in_=wpg_d.rearrange("g (i p) o -> p g i o", p=96))
    K.dma("sp", Bpscale, W=[Bpscale]).dma_start(out=pscale[:], in_=pscale_d[:, :])
    K.dma("sp", Binvcnt, W=[Binvcnt]).dma_start(out=invcnt[:], in_=invcnt_d[:, :, :])
    tok_chunks = NTC + [(NOWN, 128)]
    for i in range(2):
        K.pool(W=[U[i][1]]).memset(U[i][0][:], 0.0)
        K.pool(W=[Wn[i][1]]).memset(Wn[i][0][:], 0.0)
    for cc in range(8):
        g = cc // 2
        for ti, (o, w) in enumerate(tok_chunks):
            pU, BpU = psU[ti % 3]
            for kc in range(KC):
                K.pe(R=[BWup, BhT_s], W=[BpU]).matmul(
                    pU[0:96, 0:w], lhsT=Wup[:, kc, cc * 96:(cc + 1) * 96], rhs=hT[:, kc, o:o + w], start=(kc == 0), stop=(kc == KC - 1))
            u_t, Bu_t = U[cc % 2]
            if o < NOWN:
                s0 = o // 128
                K.act(R=[BpU], W=[Bu_t]).copy(u_t[:, s0:s0 + w // 128, 16:144], pU[0:96, 0:w].rearrange("p (s t) -> p s t", t=128))
            else:
                K.act(R=[BpU], W=[Bu_t]).copy(u_t[:, :, 0:16], pU[0:96, 0:NS * 16].rearrange("p (s t) -> p s t", t=16))
        cur, Bcur = u_t, Bu_t
        d = 1
        wi_ = 0
        while d < (2 << g):
            nxt, Bnxt = Wn[wi_ % 2]
            wi_ += 1
            K.dve(R=[Bcur], W=[Bnxt]).tensor_tensor(out=nxt[:, :, d:144], in0=cur[:, :, d:144], in1=cur[:, :, 0:144 - d], op=ALU.add)
            cur, Bcur = nxt, Bnxt
            d *= 2
        fin, Bfin = Wn[wi_ % 2]
        K.dve(R=[Bcur, Binvcnt], W=[Bfin]).tensor_tensor(
            out=fin[:, :, 16:144], in0=cur[:, :, 16:144], in1=invcnt[:, g, :].rearrange("p (s t) -> p s t", t=128), op=ALU.mult)
        K.dve(R=[Bfin, Bu_t], W=[BpTt]).tensor_tensor(
            out=pTt[:, cc, :].rearrange("p (s t) -> p s t", t=128), in0=fin[:, :, 16:144], in1=u_t[:, :, 16:144], op=ALU.subtract)
    for co in range(8):
        g = co // 2
        for ti, (o, w) in enumerate(NTC):
            pG, BpG = psG[ti % 2]
            for ci in range(2):
                K.pe(R=[BWpg, BpTt], W=[BpG]).matmul(
                    pG[0:96, 0:w], lhsT=Wpg[:, g, ci, (co % 2) * 96:(co % 2) * 96 + 96], rhs=pTt[:, 2 * g + ci, o:o + w], start=(ci == 0), stop=(ci == 1))
            K.act(R=[BpG, Bpscale], W=[Bp2T]).activation(out=p2T[:, co, o:o + w], in_=pG[0:96, 0:w], func=AF.Copy, scale=pscale[:, co:co + 1])
    K.barrier()
    stP.close()

    stM = ExitStack()
    mergedT, BmergedT = sb(stM, "mergedT", [128, KC, NOWN], BF16)
    bgT, BbgT = sb(stM, "bgT", [128, 48])
    Wg_ = [sb(stM, "Wg%d" % i, [128, KC, 3, 128], BF16) for i in range(2)]
    Wpo_ = [sb(stM, "Wpo%d" % i, [96, 8, 128], BF16) for i in range(2)]
    Wao_ = [sb(stM, "Wao%d" % i, [128, 6, 128], BF16) for i in range(2)]
    Wco_ = [sb(stM, "Wco%d" % i, [128, 4, 128], BF16) for i in range(2)]
    sg = [sb(stM, "sg%d" % i, [128, 512]) for i in range(3)]
    macc = [sb(stM, "macc%d" % i, [128, 512]) for i in range(2)]
    mtmp = [sb(stM, "mtmp%d" % i, [128, 512]) for i in range(2)]
    psGa = [ps(stM, "psGa%d" % i, [128, 512]) for i in range(3)]
    psBr = [ps(stM, "psBr%d" % i, [128, 512]) for i in range(3)]
    K.dma("sp", BbgT, W=[BbgT]).dma_start(out=bgT[:], in_=bgT_d[:, :])
    mc_ = 0

    def merge_loads(j):
        wb = j % 2
        (Wg_t, BWg_t), (Wpo_t, BWpo_t), (Wao_t, BWao_t), (Wco_t, BWco_t) = Wg_[wb], Wpo_[wb], Wao_[wb], Wco_[wb]
        for br in range(3):
            c0 = C_G + br * D + j * 128
            K.dma("pool", BWg_t, R=[Bw], W=[BWg_t]).dma_start(
                out=Wg_t[:, :, br, :], in_=w_in[:, c0:c0 + 128].rearrange("(k p) c -> p k c", p=128))
        K.dma("pool", BWpo_t, R=[Bw], W=[BWpo_t]).dma_start(
            out=Wpo_t[:], in_=wpo_d[:, j * 128:(j + 1) * 128].rearrange("(k p) c -> p k c", p=96))
        K.dma("pool", BWao_t, R=[Bw], W=[BWao_t]).dma_start(
            out=Wao_t[:], in_=wao_d[:, j * 128:(j + 1) * 128].rearrange("(k p) c -> p k c", p=128))
        K.dma("pool", BWco_t, R=[Bw], W=[BWco_t]).dma_start(
            out=Wco_t[:], in_=wco_d[:, j * 128:(j + 1) * 128].rearrange("(k p) c -> p k c", p=128))

    merge_loads(0)
    for j in range(KC):
        if j + 1 < KC:
            merge_loads(j + 1)
        wb = j % 2
        (Wg_t, BWg_t), (Wpo_t, BWpo_t), (Wao_t, BWao_t), (Wco_t, BWco_t) = Wg_[wb], Wpo_[wb], Wao_[wb], Wco_[wb]
        for (o, w) in NTC:
            for br in range(3):
                pg_, Bpg_ = psGa[br]
                for kc in range(KC):
                    K.pe(R=[BWg_t, BhT_s], W=[Bpg_]).matmul(
                        pg_[:, 0:w], lhsT=Wg_t[:, kc, br, :], rhs=hT[:, kc, o:o + w], start=(kc == 0), stop=(kc == KC - 1))
                K.act(R=[Bpg_, BbgT], W=[sg[br][1]]).activation(
                    out=sg[br][0][:, 0:w], in_=pg_[:, 0:w], func=AF.Sigmoid, bias=bgT[:, br * 16 + j:br * 16 + j + 1], scale=1.0)
            pb0, Bpb0 = psBr[0]
            for kc in range(8):
                K.pe(R=[BWpo_t, Bp2T], W=[Bpb0]).matmul(pb0[:, 0:w], lhsT=Wpo_t[:, kc, :], rhs=p2T[:, kc, o:o + w], start=(kc == 0), stop=(kc == 7))
            pb1, Bpb1 = psBr[1]
            for kc in range(6):
                K.pe(R=[BWao_t, BattnT], W=[Bpb1]).matmul(pb1[:, 0:w], lhsT=Wao_t[:, kc, :], rhs=attnT[:, kc, o:o + w], start=(kc == 0), stop=(kc == 5))
            pb2, Bpb2 = psBr[2]
            for kc in range(4):
                K.pe(R=[BWco_t, BcrossT], W=[Bpb2]).matmul(pb2[:, 0:w], lhsT=Wco_t[:, kc, :], rhs=crossT[:, kc, o:o + w], start=(kc == 0), stop=(kc == 3))
            ma, Bma = macc[mc_ % 2]
            mt_, Bmt_ = mtmp[mc_ % 2]
            mc_ += 1
            K.dve(R=[sg[0][1], Bpb0], W=[Bma]).tensor_tensor(out=ma[:, 0:w], in0=sg[0][0][:, 0:w], in1=pb0[:, 0:w], op=ALU.mult)
            K.dve(R=[sg[1][1], Bpb1], W=[Bmt_]).tensor_tensor(out=mt_[:, 0:w], in0=sg[1][0][:, 0:w], in1=pb1[:, 0:w], op=ALU.mult)
            K.dve(R=[Bma, Bmt_], W=[Bma]).tensor_tensor(out=ma[:, 0:w], in0=ma[:, 0:w], in1=mt_[:, 0:w], op=ALU.add)
            K.dve(R=[sg[2][1], Bpb2], W=[Bmt_]).tensor_tensor(out=mt_[:, 0:w], in0=sg[2][0][:, 0:w], in1=pb2[:, 0:w], op=ALU.mult)
            K.dve(R=[Bma, Bmt_], W=[BmergedT]).tensor_tensor(out=mergedT[:, j, o:o + w], in0=ma[:, 0:w], in1=mt_[:, 0:w], op=ALU.add)
    K.dma("sp", BmergedT, R=[BmergedT], W=[BmT]).dma_start(out=mT_scr[:, :, :], in_=mergedT[:])
    K.barrier()
    stM.close()
    stD.close()

    stE = ExitStack()
    acc, Bacc = sb(stE, "acc", [128, NS, D])
    h2T, Bh2T = sb(stE, "h2T", [128, KC, NOWN], BF16)
    comb, Bcomb = sb(stE, "comb", [128, NS, 16])
    stO = ExitStack()
    mergedT, BmergedT = sb(stO, "mergedT2", [128, KC, NOWN], BF16)
    K.dma("sp", BmergedT, R=[BmT], W=[BmergedT]).dma_start(out=mergedT[:], in_=mT_scr[:, :, :])
    gffn, Bgffn = sb(stO, "gffn", [128, D])
    Wo_ = [sb(stO, "Wo%d" % i, [128, KC, 512], BF16) for i in range(2)]
    xres = [sb(stO, "xres%d" % i, [128, 512]) for i in range(2)]
    wr, Bwr = sb(stO, "wr", [128, KC, 20])
    x2n, Bx2n = sb(stO, "x2n", [128, D])
    x2b, Bx2b = sb(stO, "x2b", [128, D], BF16)
    x2nT, Bx2nT = sb(stO, "x2nT", [128, KC, 128])
    ojunk, Bojunk = sb(stO, "ojunk", [128, D], BF16)
    o_ss, Bo_ss = sb(stO, "o_ss", [128, 1])
    o_sd, Bo_sd = sb(stO, "o_sd", [128, 1])
    o_rs, Bo_rs = sb(stO, "o_rs", [128, 1])
    lg, Blg = sb(stO, "lg", [128, NS, 20])
    rt = {n: sb(stO, "rt_" + n, [128, NS] + sz) for n, sz in
          (("mg", []), ("ohg", [4]), ("eg", [4]), ("sg", []), ("pg", []), ("les", [4]), ("m1", []), ("oh1", [4]), ("le2", [4]),
           ("m2", []), ("oh2", [4]), ("dm", []), ("ex", []), ("den", []), ("w1", []), ("w2", []), ("cl", [4]), ("cl2", [4]), ("prod", [4, 4]))}
    psW = [ps(stO, "psW%d" % i, [128, 512]) for i in range(2)]
    psFT = [ps(stO, "psFT%d" % i, [128, 512]) for i in range(2)]
    psBT = [ps(stO, "psBT%d" % i, [128, 1024], BF16) for i in range(2)]
    psR, BpsR = ps(stO, "psR", [128, 512])
    K.dma("sp", Bgffn, W=[Bgffn]).dma_start(out=gffn[:], in_=gffn_d[:, :])
    K.dma("sp", Bwr, W=[Bwr]).dma_start(out=wr[:], in_=wr_d.rearrange("(k p) c -> p k c", p=128))
    xc = 0
    for cg in range(4):
        W_t, BW_t = Wo_[cg % 2]
        K.dma("pool", BW_t, R=[Bw], W=[BW_t]).dma_start(
            out=W_t[:], in_=wo_d[:, cg * 512:(cg + 1) * 512].rearrange("(k p) c -> p k c", p=128))
        for i in range(NS):
            pW, BpW = psW[i % 2]
            for kc in range(KC):
                K.pe(R=[BmergedT, BW_t], W=[BpW]).matmul(
                    pW[:, :], lhsT=mergedT[:, kc, i * 128:(i + 1) * 128], rhs=W_t[:, kc, :], start=(kc == 0), stop=(kc == KC - 1))
            xr, Bxr = xres[xc % 2]
            xc += 1
            K.dma("sp", Bxr, R=[Bx_own], W=[Bxr]).dma_start(out=xr[:], in_=x_own[i * 128:(i + 1) * 128, cg * 512:(cg + 1) * 512])
            K.dve(R=[BpW, Bxr], W=[Bacc]).tensor_tensor(out=acc[:, i, cg * 512:(cg + 1) * 512], in0=pW[:, :], in1=xr[:], op=ALU.add)
    R_ = lambda n: rt[n][0]
    B_ = lambda n: rt[n][1]
    for i in range(NS):
        x2 = acc[:, i, :]
        rms_stats(x2, Bacc, ojunk[:], Bojunk, o_ss[:], Bo_ss, o_sd[:], Bo_sd, o_rs[:], Bo_rs, D)
        K.dve(R=[Bacc, Bo_rs, Bgffn], W=[Bx2n]).scalar_tensor_tensor(
            out=x2n[:], in0=x2, scalar=o_rs[:, 0:1], in1=gffn[:], op0=ALU.mult, op1=ALU.mult)
        K.pool(R=[Bx2n], W=[Bx2b]).tensor_copy(x2b[:], x2n[:])
        for half in range(2):
            pT, BpT = psBT[half]
            for kk in range(8):
                kc = half * 8 + kk
                K.pe(R=[Bx2b, Bidb], W=[BpT]).transpose(pT[:, kk * 128:(kk + 1) * 128], x2b[:, kc * 128:(kc + 1) * 128], identb[:])
            K.act(R=[BpT], W=[Bh2T]).copy(h2T[:, half * 8:(half + 1) * 8, i * 128:(i + 1) * 128], pT[:, :].rearrange("p (k t) -> p k t", k=8))
        for q4 in range(4):
            pF, BpF = psFT[q4 % 2]
            for kk in range(4):
                kc = q4 * 4 + kk
                K.pe(R=[Bx2n, Bidf], W=[BpF]).transpose(pF[:, kk * 128:(kk + 1) * 128], x2n[:, kc * 128:(kc + 1) * 128], identf[:])
            K.dve(R=[BpF], W=[Bx2nT]).tensor_copy(x2nT[:, q4 * 4:(q4 + 1) * 4, :].rearrange("p k t -> p (k t)"), pF[:, :])
        for kc in range(KC):
            K.pe(R=[Bx2nT, Bwr], W=[BpsR]).matmul(psR[:, 0:20], lhsT=x2nT[:, kc, :], rhs=wr[:, kc, :], start=(kc == 0), stop=(kc == KC - 1))
        K.act(R=[BpsR], W=[Blg]).copy(lg[:, i, :], psR[:, 0:20])
    def bc(ap, shape):
        return ap.to_broadcast(shape)
    lgG = lg[:, :, 0:4]
    lgE = lg[:, :, 4:20].rearrange("p s (g j) -> p s g j", g=4)
    K.dve(R=[Blg], W=[B_("mg")]).tensor_reduce(out=R_("mg")[:], in_=lgG, axis=AX.X, op=ALU.max)
    K.dve(R=[Blg, B_("mg")], W=[B_("eg")]).tensor_tensor(out=R_("eg")[:], in0=lgG, in1=bc(R_("mg")[:].unsqueeze(2), [128, NS, 4]), op=ALU.subtract)
    K.dve(R=[B_("eg")], W=[B_("ohg")]).tensor_scalar(R_("ohg")[:], R_("eg")[:], 0.0, None, op0=ALU.is_ge)
    K.act(R=[B_("eg")], W=[B_("eg")]).activation(out=R_("eg")[:], in_=R_("eg")[:], func=AF.Exp)
    K.dve(R=[B_("eg")], W=[B_("sg")]).tensor_reduce(out=R_("sg")[:], in_=R_("eg")[:], axis=AX.X, op=ALU.add)
    K.dve(R=[B_("sg")], W=[B_("pg")]).reciprocal(R_("pg")[:], R_("sg")[:])
    K.dve(R=[Blg, B_("ohg")], W=[B_("prod")]).tensor_tensor(
        out=R_("prod")[:], in0=lgE, in1=bc(R_("ohg")[:].unsqueeze(3), [128, NS, 4, 4]), op=ALU.mult)
    K.dve(R=[B_("prod")], W=[B_("les")]).tensor_reduce(
        out=R_("les")[:], in_=R_("prod")[:].rearrange("p s g j -> p s j g"), axis=AX.X, op=ALU.add)
    K.dve(R=[B_("les")], W=[B_("m1")]).tensor_reduce(out=R_("m1")[:], in_=R_("les")[:], axis=AX.X, op=ALU.max)
    K.dve(R=[B_("les"), B_("m1")], W=[B_("oh1")]).tensor_tensor(out=R_("oh1")[:], in0=R_("les")[:], in1=bc(R_("m1")[:].unsqueeze(2), [128, NS, 4]), op=ALU.is_ge)
    K.dve(R=[B_("oh1"), B_("les")], W=[B_("le2")]).scalar_tensor_tensor(
        out=R_("le2")[:], in0=R_("oh1")[:], scalar=-1.0e30, in1=R_("les")[:], op0=ALU.mult, op1=ALU.add)
    K.dve(R=[B_("le2")], W=[B_("m2")]).tensor_reduce(out=R_("m2")[:], in_=R_("le2")[:], axis=AX.X, op=ALU.max)
    K.dve(R=[B_("le2"), B_("m2")], W=[B_("oh2")]).tensor_tensor(out=R_("oh2")[:], in0=R_("le2")[:], in1=bc(R_("m2")[:].unsqueeze(2), [128, NS, 4]), op=ALU.is_ge)
    K.dve(R=[B_("m2"), B_("m1")], W=[B_("dm")]).tensor_tensor(out=R_("dm")[:], in0=R_("m2")[:], in1=R_("m1")[:], op=ALU.subtract)
    K.act(R=[B_("dm")], W=[B_("ex")]).activation(out=R_("ex")[:], in_=R_("dm")[:], func=AF.Exp)
    K.dve(R=[B_("ex")], W=[B_("den")]).tensor_scalar(R_("den")[:], R_("ex")[:], 1.0, None, op0=ALU.add)
    K.dve(R=[B_("den")], W=[B_("w1")]).reciprocal(R_("w1")[:], R_("den")[:])
    K.dve(R=[B_("w1"), B_("pg")], W=[B_("w1")]).tensor_tensor(out=R_("w1")[:], in0=R_("w1")[:], in1=R_("pg")[:], op=ALU.mult)
    K.dve(R=[B_("w1"), B_("ex")], W=[B_("w2")]).tensor_tensor(out=R_("w2")[:], in0=R_("w1")[:], in1=R_("ex")[:], op=ALU.mult)
    K.dve(R=[B_("oh1"), B_("w1")], W=[B_("cl")]).tensor_tensor(out=R_("cl")[:], in0=R_("oh1")[:], in1=bc(R_("w1")[:].unsqueeze(2), [128, NS, 4]), op=ALU.mult)
    K.dve(R=[B_("oh2"), B_("w2")], W=[B_("cl2")]).tensor_tensor(out=R_("cl2")[:], in0=R_("oh2")[:], in1=bc(R_("w2")[:].unsqueeze(2), [128, NS, 4]), op=ALU.mult)
    K.dve(R=[B_("cl"), B_("cl2")], W=[B_("cl2")]).tensor_tensor(out=R_("cl2")[:], in0=R_("cl2")[:], in1=R_("cl")[:], op=ALU.add)
    K.dve(R=[B_("cl2"), B_("ohg")], W=[Bcomb]).tensor_tensor(
        out=comb[:].rearrange("p s (g j) -> p s g j", g=4), in0=bc(R_("cl2")[:].unsqueeze(2), [128, NS, 4, 4]),
        in1=bc(R_("ohg")[:].unsqueeze(3), [128, NS, 4, 4]), op=ALU.mult)
    K.barrier()
    stO.close()

    stF = ExitStack()
    Wgu = [sb(stF, "Wgu%d" % i, [128, KC, 2, 128], BF16) for i in range(4)]
    Wd = [sb(stF, "Wd%d" % i, [128, 4, D], BF16) for i in range(2)]
    actT = [sb(stF, "actT%d" % i, [128, 4, NOWN], BF16) for i in range(2)]
    sil = [sb(stF, "sil%d" % i, [128, 512]) for i in range(2)]
    psGU = [ps(stF, "psGU%d" % i, [128, 512]) for i in range(4)]
    psD = [ps(stF, "psD%d" % i, [128, 512]) for i in range(4)]
    guc = 0
    slc = 0
    pdc = 0
    for e in range(NE):
        Wd_t, BWd_t = Wd[e % 2]
        a_t, Ba_t = actT[e % 2]
        for fc in range(4):
            Wgu_t, BWgu_t = Wgu[guc % 4]
            guc += 1
            K.dma("pool", BWgu_t, R=[Bw], W=[BWgu_t]).dma_start(
                out=Wgu_t[:, :, 0, :], in_=weg_d[e, :, fc * 128:(fc + 1) * 128].rearrange("(k p) c -> p k c", p=128))
            K.dma("pool", BWgu_t, R=[Bw], W=[BWgu_t]).dma_start(
                out=Wgu_t[:, :, 1, :], in_=weu_d[e, :, fc * 128:(fc + 1) * 128].rearrange("(k p) c -> p k c", p=128))
            if fc == 0:
                K.dma("pool", BWd_t, R=[Bw], W=[BWd_t]).dma_start(
                    out=Wd_t[:], in_=wed_d[e, :, :].rearrange("(k p) c -> p k c", p=128))
            for (o, w) in NTC:
                pg_, Bpg_ = psGU[pdc % 2 * 2]
                pu_, Bpu_ = psGU[pdc % 2 * 2 + 1]
                pdc += 1
                for kc in range(KC):
                    K.pe(R=[BWgu_t, Bh2T], W=[Bpg_]).matmul(pg_[:, 0:w], lhsT=Wgu_t[:, kc, 0, :], rhs=h2T[:, kc, o:o + w], start=(kc == 0), stop=(kc == KC - 1))
                for kc in range(KC):
                    K.pe(R=[BWgu_t, Bh2T], W=[Bpu_]).matmul(pu_[:, 0:w], lhsT=Wgu_t[:, kc, 1, :], rhs=h2T[:, kc, o:o + w], start=(kc == 0), stop=(kc == KC - 1))
                s_t, Bs_t = sil[slc % 2]
                slc += 1
                K.act(R=[Bpg_], W=[Bs_t]).activation(out=s_t[:, 0:w], in_=pg_[:, 0:w], func=AF.Silu)
                K.dve(R=[Bs_t, Bpu_], W=[Ba_t]).tensor_tensor(out=a_t[:, fc, o:o + w], in0=s_t[:, 0:w], in1=pu_[:, 0:w], op=ALU.mult)
        for i in range(NS):
            for cg in range(4):
                pD, BpD = psD[cg]
                for fc in range(4):
                    K.pe(R=[Ba_t, BWd_t], W=[BpD]).matmul(
                        pD[:, :], lhsT=a_t[:, fc, i * 128:(i + 1) * 128], rhs=Wd_t[:, fc, cg * 512:(cg + 1) * 512], start=(fc == 0), stop=(fc == 3))
                dst = acc[:, i, cg * 512:(cg + 1) * 512]
                K.dve(R=[BpD, Bcomb, Bacc], W=[Bacc]).scalar_tensor_tensor(
                    out=dst, in0=pD[:, :], scalar=comb[:, i, e:e + 1], in1=dst, op0=ALU.mult, op1=ALU.add)
    for i in range(NS):
        K.dma("sp", By, R=[Bacc], W=[By]).dma_start(out=y_out[i * 128:(i + 1) * 128, :], in_=acc[:, i, :])
    K.finish()
    K.emit()
    stF.close()
    stE.close()
    es.close()
    return nc


def _prep_inputs(inp, NS):
    f32 = np.float32
    S = 1024 * NS
    NT = S // 128
    x = np.ascontiguousarray(np.asarray(inp["x"], dtype=f32)[0])
    pos = np.asarray(inp["positions"])[0].astype(np.int32)
    sq = lambda k: np.asarray(inp[k], dtype=f32)[0]
    rep = lambda v: np.ascontiguousarray(np.broadcast_to(np.asarray(v, dtype=f32)[None, :], (128, v.shape[0])))
    w_router = np.ascontiguousarray(np.concatenate([sq("w_router_group"), sq("w_router_expert")], axis=1))
    invf128 = (10000.0 ** (-np.arange(0, 128, 2, dtype=np.float32) / np.float32(128))).astype(f32)
    invf64 = (10000.0 ** (-np.arange(0, 64, 2, dtype=np.float32) / np.float32(64))).astype(f32)
    invfT = rep(np.concatenate([invf128, invf128, invf64, invf64]))
    hp = np.float32(np.pi / 2)
    offsT = rep(np.concatenate([np.zeros(64, f32), np.full(64, hp, f32), np.zeros(32, f32), np.full(32, hp, f32)]))
    shared = {
        "x_all": x,
        "posT_all": np.ascontiguousarray(pos.reshape(NT, 128).T),
        "mem": np.ascontiguousarray(np.asarray(inp["mem"], dtype=f32)[0]),
        "w_in": sq("w_in"),
        "gmix_rep": rep(sq("g_mix")), "gffn_rep": rep(sq("g_ffn")), "gmem_rep": rep(sq("g_mem")),
        "bgT": np.ascontiguousarray(sq("b_gate").reshape(48, 128).T),
        "w_pool_grp": sq("w_pool_grp"),
        "pscaleT": np.ascontiguousarray(sq("pool_scale").reshape(8, 96).T),
        "gq_rep": rep(sq("q_norm_g")), "gk_rep": rep(sq("k_norm_g")),
        "gxq_rep": rep(sq("xq_norm_g")), "gxk_rep": rep(sq("xk_norm_g")),
        "w_mem_kv": sq("w_mem_kv"), "w_pool_out": sq("w_pool_out"), "w_attn_out": sq("w_attn_out"),
        "w_cross_out": sq("w_cross_out"), "w_o": sq("w_o"), "w_router": w_router,
        "w_e_gate": sq("w_e_gate"), "w_e_up": sq("w_e_up"), "w_e_down": sq("w_e_down"),
        "ident": np.eye(128, dtype=f32), "invfT": invfT, "offsT": offsT,
    }
    maps = []
    for c in range(NCORES):
        rows = np.concatenate([np.arange((8 * s + c) * 128, (8 * s + c + 1) * 128) for s in range(NS)])
        x_own = np.zeros((NS * 128 + 128, D), f32)
        x_own[:NS * 128] = x[rows]
        for s in range(NS):
            st = (8 * s + c) * 128
            if st >= 16:
                x_own[NS * 128 + s * 16:NS * 128 + (s + 1) * 16] = x[st - 16:st]
        t_i = np.arange(128)[:, None]
        j_i = np.arange(1024)[None, :]
        tailmask = np.where(j_i > 128 * c + t_i, np.float32(NEG), np.float32(0)).astype(f32)
        invcnt = np.zeros((96, 4, NS * 128), f32)
        for g, w in enumerate((2, 4, 8, 16)):
            invcnt[:, g, :] = (1.0 / np.minimum(rows + 1, w).astype(np.float64)).astype(f32)[None, :]
        m = dict(shared)
        m.update({"x_own": x_own, "posT_own": np.ascontiguousarray(pos[rows].reshape(NS, 128).T),
                  "tailmask": tailmask, "invcnt": invcnt})
        maps.append(m)
    return maps


_NC_CACHE = {}


def run(inp, NS):
    if NS not in _NC_CACHE:
        _NC_CACHE[NS] = build(NS)
    nc = _NC_CACHE[NS]
    maps = _prep_inputs(inp, NS)
    res = run_bass_kernel_spmd(nc, maps, core_ids=list(range(NCORES)))
    S = 1024 * NS
    out = np.zeros((1, S, D), np.float32)
    for c in range(NCORES):
        y = res.results[c]["y_own"]
        for s in range(NS):
            st = (8 * s + c) * 128
            out[0, st:st + 128] = y[s * 128:(s + 1) * 128]
    return out


def kernel(**inputs):
    return run(inputs, 8)
```

```python
import numpy as np
from contextlib import ExitStack
import concourse.bass as bass
import concourse.mybir as mybir
from concourse.bass_utils import run_bass_kernel_spmd

F32 = mybir.dt.float32
BF16 = mybir.dt.bfloat16
I32 = mybir.dt.int32
AF = mybir.ActivationFunctionType
ALU = mybir.AluOpType
AX = mybir.AxisListType

NCORES = 8
D = 2048
KC = 16
C_UP, C_Q, C_K, C_V, C_QI, C_KI, C_WI, C_XQ, C_G = 0, 768, 1536, 2304, 3072, 3328, 3392, 3396, 3908
IN_COLS = 10052
EPS = 1e-6
TWO_PI = 2.0 * np.pi
CW1 = 6.28125
CW2 = TWO_PI - 6.28125
NEG = -1.0e30
NITER = 18
VW = 132
NE = 16
FF = 512
DEBUG_NAMES = None
DVE_GAP = 2
PI_LO = 3.1415925


class Buf:
    __slots__ = ("name", "lw", "rd", "psum")

    def __init__(self, name, psum=False):
        self.name = name
        self.lw = None
        self.rd = []
        self.psum = psum


class _Rec:
    def __init__(self, K, eng, R, W, sem):
        self.K, self.eng, self.R, self.W, self.sem = K, eng, R, W, sem

    def __getattr__(self, name):
        def f(*a, **kw):
            self.K._record(self.eng, name, a, kw, self.R, self.W, self.sem)
        return f


class Kern:
    ENGS = ("pe", "act", "dve", "pool", "sp")

    def __init__(self, nc, es):
        self.nc, self.es = nc, es
        self.ops = {e: [] for e in self.ENGS}
        self.cnt = {e: 0 for e in self.ENGS}
        self.pending = {e: [] for e in self.ENGS}
        self.dsem = {}
        self.pad_ap = None
        self.csem = {e: es.enter_context(nc.semaphore("cs_" + e)) for e in ("pe", "act", "dve", "pool")}

    def pe(self, R=(), W=()): return _Rec(self, "pe", R, W, None)
    def act(self, R=(), W=()): return _Rec(self, "act", R, W, None)
    def dve(self, R=(), W=()): return _Rec(self, "dve", R, W, None)
    def pool(self, R=(), W=()): return _Rec(self, "pool", R, W, None)
    def dma(self, q, sem, R=(), W=()): return _Rec(self, q, R, W, sem)

    def _record(self, eng, name, a, kw, R, W, sem):
        toks = list(self.pending[eng])
        self.pending[eng] = []
        for b in R:
            if b.lw is not None:
                toks.append(b.lw)
            if b.psum:
                toks.extend(t for t in b.rd if not (t[0] == "c" and t[1] == eng))
        for b in W:
            if b.lw is not None:
                toks.append(b.lw)
            toks.extend(b.rd)
        if sem is None:
            if eng == "pe":
                toks = [t for t in toks if not (t[0] == "c" and t[1] == "pe")]
            if eng == "dve" and self.pad_ap is not None:
                own = [t[2] for t in toks if t[0] == "c" and t[1] == "dve"]
                toks = [t for t in toks if not (t[0] == "c" and t[1] == "dve")]
                if own:
                    between = self.cnt["dve"] - max(own)
                    for _ in range(max(0, DVE_GAP - between)):
                        self.cnt["dve"] += 1
                        self.ops["dve"].append(([], "memset", (self.pad_ap, 0.0), {}, ("c", "dve", self.cnt["dve"])))
            self.cnt[eng] += 1
            tok = ("c", eng, self.cnt[eng])
        else:
            if sem not in self.dsem:
                self.dsem[sem] = [self.es.enter_context(self.nc.semaphore("ds_%d" % len(self.dsem))), 0]
            self.dsem[sem][1] += 1
            tok = ("d", sem, 16 * self.dsem[sem][1])
        self.ops[eng].append((toks, name, a, kw, tok))
        for b in R:
            b.rd.append(tok)
        for b in W:
            b.lw = tok
            b.rd = []

    def barrier(self):
        toks = [("c", e, self.cnt[e]) for e in ("pe", "act", "dve", "pool") if self.cnt[e] > 0]
        toks += [("d", s, 16 * v[1]) for s, v in self.dsem.items()]
        for e in self.ENGS:
            self.pending[e].extend(toks)

    def finish(self):
        self.barrier()
        for e in self.ENGS:
            self.ops[e].append((self.pending[e], None, (), {}, None))
            self.pending[e] = []

    def emit(self):
        nc = self.nc
        miles = {e: set() for e in self.ENGS}
        for e in self.ENGS:
            for toks, _, _, _, _ in self.ops[e]:
                for t in toks:
                    if t[0] == "c":
                        miles[t[1]].add(t[2])
        rank = {}
        for e in self.ENGS:
            rank[e] = {idx: r + 1 for r, idx in enumerate(sorted(miles[e]))}
        with nc.Block() as block:
            for ename, attr in (("pe", "tensor"), ("act", "scalar"), ("dve", "vector"), ("pool", "gpsimd"), ("sp", "sync")):
                ops = self.ops[ename]

                def body(e, ops=ops, ename=ename):
                    seen = {}
                    for toks, name, a, kw, tok in ops:
                        need = {}
                        for t in toks:
                            if t[0] == "c":
                                key, val = ("c", t[1]), rank[t[1]][t[2]]
                            else:
                                key, val = ("d", t[1]), t[2]
                            if seen.get(key, 0) >= val:
                                continue
                            need[key] = max(need.get(key, 0), val)
                        for key, val in need.items():
                            seen[key] = val
                            sem = self.csem[key[1]] if key[0] == "c" else self.dsem[key[1]][0]
                            e.wait_ge(sem, val)
                        if name is None:
                            continue
                        ins = getattr(e, name)(*a, **kw)
                        if DEBUG_NAMES is not None:
                            DEBUG_NAMES[ins.ins.name] = (ename, name, {k: str(v) for k, v in kw.items() if k.startswith("op") or k == "func"})
                        if tok[0] == "c":
                            if tok[2] in rank[ename]:
                                ins.then_inc(self.csem[ename], 1)
                        else:
                            ins.then_inc(self.dsem[tok[1]][0], 16)

                getattr(block, attr)(body)


def build(NS):
    S = 1024 * NS
    NT = S // 128
    NOWN = NS * 128
    NTOK = NOWN + 128
    nc = bass.Bass("TRN2", target_bir_lowering=False)
    es = ExitStack()
    K = Kern(nc, es)

    def din(name, shape, dt=F32):
        return nc.dram_tensor(name, list(shape), dt, kind="ExternalInput").ap()

    x_all = din("x_all", [S, D])
    x_own = din("x_own", [NTOK, D])
    posT_all = din("posT_all", [128, NT], I32)
    posT_own = din("posT_own", [128, NS], I32)
    mem = din("mem", [256, D])
    w_in = din("w_in", [D, IN_COLS])
    gmix_d = din("gmix_rep", [128, D])
    gffn_d = din("gffn_rep", [128, D])
    gmem_d = din("gmem_rep", [128, D])
    bgT_d = din("bgT", [128, 48])
    wpg_d = din("w_pool_grp", [4, 192, 192])
    pscale_d = din("pscaleT", [96, 8])
    gq_d = din("gq_rep", [128, 128])
    gk_d = din("gk_rep", [128, 128])
    gxq_d = din("gxq_rep", [128, 128])
    gxk_d = din("gxk_rep", [128, 128])
    wmkv_d = din("w_mem_kv", [D, 1024])
    wpo_d = din("w_pool_out", [768, D])
    wao_d = din("w_attn_out", [768, D])
    wco_d = din("w_cross_out", [512, D])
    wo_d = din("w_o", [D, D])
    wr_d = din("w_router", [D, 20])
    weg_d = din("w_e_gate", [NE, D, FF])
    weu_d = din("w_e_up", [NE, D, FF])
    wed_d = din("w_e_down", [NE, FF, D])
    ident_d = din("ident", [128, 128])
    invf_d = din("invfT", [128, 192])
    offs_d = din("offsT", [128, 192])
    tailmask_d = din("tailmask", [128, 1024])
    invcnt_d = din("invcnt", [96, 4, NOWN])
    y_out = nc.dram_tensor("y_own", [NOWN, D], F32, kind="ExternalOutput").ap()
    KT_scr = nc.dram_tensor("KT_scr", [6, 128, S], BF16, kind="Internal").ap()
    V_scr = nc.dram_tensor("V_scr", [6, 128, NT, VW], BF16, kind="Internal").ap()
    hT_scr = nc.dram_tensor("hT_scr", [128, KC, NTOK], BF16, kind="Internal").ap()
    aT_scr = nc.dram_tensor("aT_scr", [128, 6, NOWN], BF16, kind="Internal").ap()
    mT_scr = nc.dram_tensor("mT_scr", [128, KC, NOWN], BF16, kind="Internal").ap()

    def sb(st, name, shape, dt=F32):
        t = st.enter_context(nc.sbuf_tensor("s_" + name, list(shape), dt))
        return t, Buf(name)

    def ps(st, name, shape, dt=F32):
        t = st.enter_context(nc.psum_tensor("p_" + name, list(shape), dt))
        return t, Buf(name, psum=True)

    Bx_all, Bx_own, Bw = Buf("x_all"), Buf("x_own"), Buf("weights")
    BKT, BV, BhT, By = Buf("KT_scr"), Buf("V_scr"), Buf("hT_scr"), Buf("y")
    BaT, BmT = Buf("aT_scr"), Buf("mT_scr")

    identf, Bidf = sb(es, "identf", [128, 128])
    identb, Bidb = sb(es, "identb", [128, 128], BF16)
    invf, Binvf = sb(es, "invf", [128, 192])
    offs, Boffs = sb(es, "offs", [128, 192])
    K.dma("sp", Bidf, W=[Bidf]).dma_start(out=identf[:], in_=ident_d[:, :])
    K.dma("sp", Binvf, W=[Binvf]).dma_start(out=invf[:], in_=invf_d[:, :])
    K.dma("sp", Boffs, W=[Boffs]).dma_start(out=offs[:], in_=offs_d[:, :])
    K.dve(R=[Bidf], W=[Bidb]).tensor_copy(identb[:], identf[:])

    def rms_stats(xt_ap, Bxt, junk_ap, Bjunk, ss, Bss, sd, Bsd, rstd, Brstd, width):
        K.dve(W=[Bss]).memset(ss, 0.0)
        K.act(R=[Bxt, Bss], W=[Bjunk, Bss]).activation(out=junk_ap, in_=xt_ap, func=AF.Square, accum_out=ss)
        K.act(R=[Bss], W=[Bsd]).activation(out=sd, in_=ss, func=AF.Sqrt, bias=EPS, scale=1.0 / width)
        K.dve(R=[Bsd], W=[Brstd]).reciprocal(rstd, sd)

    def trig_group(pos4, Bpos, n, ang_t, Bang, angk_t, Bangk, angi_t, Bangi, csg_t, Bcsg):
        shp = [128, n, 192]
        ang, angk, angi, csg = ang_t[:, 0:n, :], angk_t[:, 0:n, :], angi_t[:, 0:n, :], csg_t[:, 0:n, :]
        K.dve(R=[Binvf, Bpos], W=[Bang]).tensor_tensor(
            out=ang, in0=pos4.unsqueeze(2).to_broadcast(shp), in1=invf[:].unsqueeze(1).to_broadcast(shp), op=ALU.mult)
        K.dve(R=[Bang, Boffs], W=[Bang]).tensor_tensor(out=ang, in0=ang, in1=offs[:].unsqueeze(1).to_broadcast(shp), op=ALU.add)
        K.dve(R=[Bang], W=[Bangk]).tensor_scalar(angk, ang, 1.0 / TWO_PI, None, op0=ALU.mult)
        K.dve(R=[Bangk], W=[Bangi]).tensor_copy(angi, angk)
        K.dve(R=[Bangi], W=[Bangk]).tensor_copy(angk, angi)
        K.dve(R=[Bang, Bangk], W=[Bang]).scalar_tensor_tensor(out=ang, in0=angk, scalar=-CW1, in1=ang, op0=ALU.mult, op1=ALU.add)
        K.dve(R=[Bang, Bangk], W=[Bang]).scalar_tensor_tensor(out=ang, in0=angk, scalar=-CW2, in1=ang, op0=ALU.mult, op1=ALU.add)
        K.dve(R=[Bang], W=[Bang]).tensor_scalar(ang, ang, -PI_LO, PI_LO, op0=ALU.max, op1=ALU.min)
        K.act(R=[Bang], W=[Bcsg]).activation(out=csg, in_=ang, func=AF.Sin)

    def rotary(eng, src, Bsrc, dst, Bdst, cosv, sinv, Bcs, tmp, Btmp, nh, hd):
        x1, x2 = src[:, :, 0:hd], src[:, :, hd:2 * hd]
        cb = cosv.unsqueeze(1).to_broadcast([128, nh, hd])
        sbb = sinv.unsqueeze(1).to_broadcast([128, nh, hd])
        t1, t2 = tmp[:, 0:nh, 0:hd], tmp[:, 0:nh, hd:2 * hd]
        eng(R=[Bsrc, Bcs], W=[Btmp]).tensor_tensor(out=t1, in0=x1, in1=cb, op=ALU.mult)
        eng(R=[Bsrc, Bcs], W=[Btmp]).tensor_tensor(out=t2, in0=x2, in1=sbb, op=ALU.mult)
        eng(R=[Btmp], W=[Bdst]).tensor_tensor(out=dst[:, :, 0:hd], in0=t1, in1=t2, op=ALU.subtract)
        eng(R=[Bsrc, Bcs], W=[Btmp]).tensor_tensor(out=t1, in0=x2, in1=cb, op=ALU.mult)
        eng(R=[Bsrc, Bcs], W=[Btmp]).tensor_tensor(out=t2, in0=x1, in1=sbb, op=ALU.mult)
        eng(R=[Btmp], W=[Bdst]).tensor_tensor(out=dst[:, :, hd:2 * hd], in0=t1, in1=t2, op=ALU.add)

    def head_fac(raw, Braw, nh, sq, Bsq, ssq, Bssq, fac, Bfac):
        K.act(R=[Braw], W=[Bsq]).activation(out=sq[:, 0:nh * 128], in_=raw, func=AF.Square)
        K.dve(R=[Bsq], W=[Bssq]).tensor_reduce(
            out=ssq[:, 0:nh], in_=sq[:, 0:nh * 128].rearrange("p (h d) -> p h d", h=nh), axis=AX.X, op=ALU.add)
        K.act(R=[Bssq], W=[Bssq]).activation(out=ssq[:, 0:nh], in_=ssq[:, 0:nh], func=AF.Sqrt, bias=EPS, scale=1.0 / 128)
        K.dve(R=[Bssq], W=[Bfac]).reciprocal(fac[:, 0:nh], ssq[:, 0:nh])

    stAC = ExitStack()
    kiT, BkiT = sb(stAC, "kiT", [64, S], BF16)
    qT, BqT = sb(stAC, "qT", [128, 6, NOWN], BF16)
    qiT, BqiT = sb(stAC, "qiT", [64, 4, NOWN], BF16)
    sgn, Bsgn = sb(stAC, "sgn", [128, NS, 4])

    stAB = ExitStack()
    gmix, Bgmix = sb(stAB, "gmix", [128, D])
    gk, Bgk = sb(stAB, "gk", [128, 128])
    gq, Bgq = sb(stAB, "gq", [128, 128])
    posA_i, BposAi = sb(stAB, "posA_i", [128, NT], I32)
    posA, BposA = sb(stAB, "posA", [128, NT])
    posO_i, BposOi = sb(stAB, "posO_i", [128, NS], I32)
    posO, BposO = sb(stAB, "posO", [128, NS])
    WA, BWA = sb(stAB, "WA", [128, KC, 1600], BF16)
    xt = [sb(stAB, "xt%d" % i, [128, D]) for i in range(2)]
    xb = [sb(stAB, "xb%d" % i, [128, D], BF16) for i in range(2)]
    xT = [sb(stAB, "xT%d" % i, [128, KC, 128], BF16) for i in range(2)]
    st_ss = [sb(stAB, "ss%d" % i, [128, 1]) for i in range(2)]
    st_sd = [sb(stAB, "sd%d" % i, [128, 1]) for i in range(2)]
    st_rs = [sb(stAB, "rs%d" % i, [128, 1]) for i in range(2)]
    ang, Bang_ = sb(stAB, "ang", [128, 4, 192])
    angk, Bangk_ = sb(stAB, "angk", [128, 4, 192])
    angi, Bangi_ = sb(stAB, "angi", [128, 4, 192], I32)
    csg = [sb(stAB, "csg%d" % i, [128, 4, 192]) for i in range(2)]
    Ksb = [sb(stAB, "Ksb%d" % i, [128, 6, 128]) for i in range(2)]
    sq, Bsq = sb(stAB, "sq", [128, 768])
    ssq, Bssq = sb(stAB, "ssq", [128, 6])
    fac, Bfac = sb(stAB, "fac", [128, 6])
    kn, Bkn = sb(stAB, "kn", [128, 6, 128])
    rtmp, Brtmp = sb(stAB, "rtmp", [128, 6, 128])
    kr = [sb(stAB, "kr%d" % i, [128, 6, 128], BF16) for i in range(3)]
    kif = [sb(stAB, "kif%d" % i, [128, 4, 64]) for i in range(2)]
    itmp, Bitmp = sb(stAB, "itmp", [128, 4, 64])
    kir = [sb(stAB, "kir%d" % i, [128, 4, 64], BF16) for i in range(3)]
    KTst = [sb(stAB, "KTst%d" % i, [128, 6, 512], BF16) for i in range(2)]
    Vst = [sb(stAB, "Vst%d" % i, [128, 6, 4, VW], BF16) for i in range(2)]
    wisb = [sb(stAB, "wis%d" % i, [128, 4]) for i in range(2)]
    aw, Baw = sb(stAB, "aw", [128, 4])
    psT = [ps(stAB, "psT%d" % i, [128, 1024], BF16) for i in range(2)]
    psA = [ps(stAB, "psA%d" % i, [128, 512]) for i in range(4)]
    psK, BpsK = ps(stAB, "psK", [128, 1024], BF16)
    psK2 = ps(stAB, "psK2", [128, 1024], BF16)

    K.dma("sp", Bgmix, W=[Bgmix]).dma_start(out=gmix[:], in_=gmix_d[:, :])
    K.dma("sp", Bgk, W=[Bgk]).dma_start(out=gk[:], in_=gk_d[:, :])
    K.dma("sp", Bgq, W=[Bgq]).dma_start(out=gq[:], in_=gq_d[:, :])
    K.dma("sp", BposAi, W=[BposAi]).dma_start(out=posA_i[:], in_=posT_all[:, :])
    K.dma("sp", BposOi, W=[BposOi]).dma_start(out=posO_i[:], in_=posT_own[:, :])
    K.dve(R=[BposAi], W=[BposA]).tensor_copy(posA[:], posA_i[:])
    K.dve(R=[BposOi], W=[BposO]).tensor_copy(posO[:], posO_i[:])
    for c0 in range(0, 1536, 512):
        K.dma("pool", BWA, R=[Bw], W=[BWA]).dma_start(
            out=WA[:, :, c0:c0 + 512], in_=w_in[:, C_K + c0:C_K + c0 + 512].rearrange("(k p) c -> p k c", p=128))
    K.dma("pool", BWA, R=[Bw], W=[BWA]).dma_start(
        out=WA[:, :, 1536:1600], in_=w_in[:, C_KI:C_KI + 64].rearrange("(k p) c -> p k c", p=128))
    for i in range(2):
        K.pool(W=[Vst[i][1]]).memset(Vst[i][0][:], 1.0)

    colsA = [(0, 512), (512, 512), (1024, 512), (1536, 64)]
    colsB = [(0, 512), (512, 256), (768, 324)]
    items = [("A", j) for j in range(NT)] + [("B", i) for i in range(NS + 1)]

    def stL(g):
        kind, t = items[g]
        src_rows, Bsrc = (x_all[t * 128:(t + 1) * 128, :], Bx_all) if kind == "A" else (x_own[t * 128:(t + 1) * 128, :], Bx_own)
        x_t, Bx_t = xt[g % 2]
        K.dma("sp", Bx_t, R=[Bsrc], W=[Bx_t]).dma_start(out=x_t[:], in_=src_rows)

    def stN(g):
        b = g % 2
        (x_t, Bx_t), (xb_t, Bxb_t) = xt[b], xb[b]
        (ss, Bss), (sd, Bsd), (rs, Brs) = st_ss[b], st_sd[b], st_rs[b]
        rms_stats(x_t[:], Bx_t, xb_t[:], Bxb_t, ss[:], Bss, sd[:], Bsd, rs[:], Brs, D)
        K.dve(R=[Bx_t, Brs, Bgmix], W=[Bxb_t]).scalar_tensor_tensor(
            out=xb_t[:], in0=x_t[:], scalar=rs[:, 0:1], in1=gmix[:], op0=ALU.mult, op1=ALU.mult)

    def stT(g):
        kind, t = items[g]
        b = g % 2
        (xb_t, Bxb_t), (xT_t, BxT_t) = xb[b], xT[b]
        for half in range(2):
            pT, BpT = psT[half]
            for kk in range(8):
                kc = half * 8 + kk
                K.pe(R=[Bxb_t, Bidb], W=[BpT]).transpose(pT[:, kk * 128:(kk + 1) * 128], xb_t[:, kc * 128:(kc + 1) * 128], identb[:])
            dst = xT_t[:, half * 8:(half + 1) * 8, :].rearrange("p k t -> p (k t)")
            if half == 0:
                K.act(R=[BpT], W=[BxT_t]).copy(dst, pT[:, :])
            else:
                K.dve(R=[BpT], W=[BxT_t]).tensor_copy(dst, pT[:, :])
        if kind == "B":
            K.dma("sp", BxT_t, R=[BxT_t], W=[BhT]).dma_start(out=hT_scr[:, :, t * 128:(t + 1) * 128], in_=xT_t[:])

    def stM(g):
        kind, t = items[g]
        b = g % 2
        xT_t, BxT_t = xT[b]
        Ks, BKs = Ksb[b]
        Ksf = Ks[:].rearrange("p h d -> p (h d)")
        ki_f, Bki_f = kif[b]
        if kind == "B" and t == 0:
            K.dma("pool", BWA, R=[Bw], W=[BWA]).dma_start(
                out=WA[:, :, 0:512], in_=w_in[:, C_Q:C_Q + 512].rearrange("(k p) c -> p k c", p=128))
            K.dma("pool", BWA, R=[Bw], W=[BWA]).dma_start(
                out=WA[:, :, 512:768], in_=w_in[:, C_Q + 512:C_Q + 768].rearrange("(k p) c -> p k c", p=128))
            K.dma("pool", BWA, R=[Bw], W=[BWA]).dma_start(
                out=WA[:, :, 768:1092], in_=w_in[:, C_QI:C_QI + 324].rearrange("(k p) c -> p k c", p=128))
        if kind == "B" and t == NS:
            return
        if g % 4 == 0 and g + 4 < NI - 1:
            emit_trig(g // 4 + 1)
        cols = colsA if kind == "A" else colsB
        for kc in range(KC):
            for cg, (c0, w) in enumerate(cols):
                K.pe(R=[BxT_t, BWA], W=[psA[cg][1]]).matmul(
                    psA[cg][0][:, 0:w], lhsT=xT_t[:, kc, :], rhs=WA[:, kc, c0:c0 + w], start=(kc == 0), stop=(kc == KC - 1))
        K.act(R=[psA[0][1]], W=[BKs]).copy(Ksf[:, 0:512], psA[0][0][:, 0:512])
        K.act(R=[psA[1][1]], W=[BKs]).copy(Ksf[:, 512:768], psA[1][0][:, 0:256])
        if kind == "A":
            Vs, BVs = Vst[(t // 4) % 2]
            jj = t % 4
            K.act(R=[psA[1][1]], W=[BVs]).copy(Vs[:, 0:2, jj, 0:128], psA[1][0][:, 256:512].rearrange("p (h d) -> p h d", h=2))
            K.act(R=[psA[2][1]], W=[BVs]).copy(Vs[:, 2:6, jj, 0:128], psA[2][0][:, 0:512].rearrange("p (h d) -> p h d", h=4))
            K.act(R=[psA[3][1]], W=[Bki_f]).copy(ki_f[:, 0, :], psA[3][0][:, 0:64])
        else:
            K.act(R=[psA[2][1]], W=[Bki_f]).copy(ki_f[:], psA[2][0][:, 0:256].rearrange("p (h d) -> p h d", h=4))
            K.act(R=[psA[2][1]], W=[wisb[b][1]]).copy(wisb[b][0][:], psA[2][0][:, 320:324])

    def stR(g):
        kind, t = items[g]
        if kind == "B" and t == NS:
            return
        b = g % 2
        b3 = g % 3
        Ks, BKs = Ksb[b]
        cs_t, Bcs_t = csg[(g // 4) % 2][0][:, g % 4, :], csg[(g // 4) % 2][1]
        ki_f, Bki_f = kif[b]
        ki_r, Bki_r = kir[b3]
        kr_t, Bkr_t = kr[b3]
        gain, Bgain = (gk, Bgk) if kind == "A" else (gq, Bgq)
        Ksf = Ks[:].rearrange("p h d -> p (h d)")
        head_fac(Ksf, BKs, 6, sq, Bsq, ssq, Bssq, fac, Bfac)
        K.dve(R=[BKs, Bfac], W=[Bkn]).tensor_tensor(out=kn[:], in0=Ks[:], in1=fac[:, 0:6].unsqueeze(2).to_broadcast([128, 6, 128]), op=ALU.mult)
        K.pool(R=[Bkn, Bgain], W=[Bkn]).tensor_tensor(out=kn[:], in0=kn[:], in1=gain[:].unsqueeze(1).to_broadcast([128, 6, 128]), op=ALU.mult)
        rotary(K.pool, kn, Bkn, kr_t, Bkr_t, cs_t[:, 64:128], cs_t[:, 0:64], Bcs_t, rtmp, Brtmp, 6, 64)
        if kind == "A":
            rotary(K.pool, ki_f[:, 0:1, :], Bki_f, ki_r[:, 0:1, :], Bki_r, cs_t[:, 160:192], cs_t[:, 128:160], Bcs_t, itmp, Bitmp, 1, 32)
        else:
            wis, Bwis = wisb[b]
            K.dve(R=[Bwis], W=[Bsgn]).tensor_scalar(sgn[:, t, :], wis[:], 0.0, 2.0, op0=ALU.is_ge, op1=ALU.mult)
            K.dve(R=[Bsgn], W=[Bsgn]).tensor_scalar(sgn[:, t, :], sgn[:, t, :], -1.0, None, op0=ALU.add)
            K.dve(R=[Bwis, Bsgn], W=[Baw]).scalar_tensor_tensor(out=aw[:], in0=wis[:], scalar=1.0 / 16, in1=sgn[:, t, :], op0=ALU.mult, op1=ALU.mult)
            rotary(K.pool, ki_f, Bki_f, itmp, Bitmp, cs_t[:, 160:192], cs_t[:, 128:160], Bcs_t, rtmp, Brtmp, 4, 32)
            K.pool(R=[Bitmp, Baw], W=[Bki_r]).tensor_tensor(out=ki_r[:], in0=itmp[:], in1=aw[:].unsqueeze(2).to_broadcast([128, 4, 64]), op=ALU.mult)

    def stO(g):
        kind, t = items[g]
        if kind == "B" and t == NS:
            return
        b3 = g % 3
        ki_r, Bki_r = kir[b3]
        kr_t, Bkr_t = kr[b3]
        for h in range(6):
            K.pe(R=[Bkr_t, Bidb], W=[BpsK]).transpose(psK[:, h * 128:(h + 1) * 128], kr_t[:, h, :], identb[:])
        if kind == "A":
            sbi = (t // 4) % 2
            jj = t % 4
            Vs, BVs = Vst[sbi]
            KTs, BKTs = KTst[sbi]
            K.pe(R=[Bki_r, Bidb], W=[psK2[1]]).transpose(psK2[0][0:64, 0:128], ki_r[:, 0, :], identb[:])
            K.act(R=[BpsK], W=[BKTs]).copy(KTs[:, :, jj * 128:(jj + 1) * 128], psK[:, 0:768].rearrange("p (h t) -> p h t", h=6))
            K.dve(R=[psK2[1]], W=[BkiT]).tensor_copy(kiT[:, t * 128:(t + 1) * 128], psK2[0][0:64, 0:128])
            if jj == 3:
                t0 = (t - 3) * 128
                K.dma("sp", BKTs, R=[BKTs], W=[BKT]).dma_start(
                    out=KT_scr[:, :, t0:t0 + 512].rearrange("h d t -> d h t"), in_=KTs[:])
                K.dma("sp", BVs, R=[BVs], W=[BV]).dma_start(
                    out=V_scr[:, :, t - 3:t + 1, :].rearrange("h p j c -> p h j c"), in_=Vs[:])
        else:
            K.act(R=[BpsK], W=[BqT]).copy(qT[:, :, t * 128:(t + 1) * 128], psK[:, 0:768].rearrange("p (h t) -> p h t", h=6))
            for h in range(4):
                K.pe(R=[Bki_r, Bidb], W=[psK2[1]]).transpose(psK2[0][0:64, h * 128:(h + 1) * 128], ki_r[:, h, :], identb[:])
            K.dve(R=[psK2[1]], W=[BqiT]).tensor_copy(qiT[:, :, t * 128:(t + 1) * 128], psK2[0][0:64, 0:512].rearrange("p (h t) -> p h t", h=4))

    NI = len(items)

    def emit_trig(q):
        kind, t0 = items[4 * q]
        n = min(4, NI - 1 - 4 * q)
        pos4, Bpos = (posA[:, t0:t0 + n], BposA) if kind == "A" else (posO[:, t0:t0 + n], BposO)
        c_t, Bc_t = csg[q % 2]
        trig_group(pos4, Bpos, n, ang, Bang_, angk, Bangk_, angi, Bangi_, c_t, Bc_t)

    emit_trig(0)
    stL(0)
    stL(1)
    stN(0)
    stL(2)
    stN(1)
    stT(0)
    for n in range(NI + 3):
        if n + 3 < NI:
            stL(n + 3)
        if n + 2 < NI:
            stN(n + 2)
        if n + 1 < NI:
            stT(n + 1)
        if 0 <= n - 3 < NI:
            stO(n - 3)
        if 0 <= n - 1 < NI:
            stR(n - 1)
        if n < NI:
            stM(n)
    K.barrier()
    stAB.close()

    stC = ExitStack()
    LMAX = S
    PIECE = 1024
    U8 = mybir.dt.uint8
    sm2 = [sb(stC, "sm%d" % i, [128, LMAX]) for i in range(2)]
    cjunk, Bcjunk = sb(stC, "cjunk", [128, LMAX], U8)
    tmask, Btmask = sb(stC, "tmask", [128, 1024])
    ttmp, Bttmp = sb(stC, "ttmp", [128, 1024])
    rh = [sb(stC, "rh%d" % i, [128, 512]) for i in range(4)]
    mb = [sb(stC, "mb%d" % i, [128, 1024], BF16) for i in range(2)]
    maskT2 = [sb(stC, "maskT%d" % i, [128, LMAX // 128, 128], BF16) for i in range(2)]
    KTp = [sb(stC, "KTp%d" % i, [128, PIECE], BF16) for i in range(3)]
    Vp = [sb(stC, "Vp%d" % i, [128, PIECE // 128, VW], BF16) for i in range(3)]
    pT_ = [sb(stC, "pT%d" % i, [128, 512], BF16) for i in range(3)]
    hi0, Bhi0 = sb(stC, "hi0", [128, 1])
    m1, Bm1 = sb(stC, "m1", [128, 1])
    m2, Bm2 = sb(stC, "m2", [128, 1])
    lo, Blo = sb(stC, "lo", [128, 1])
    w0, Bw0 = sb(stC, "w0", [128, 1])
    mid, Bmid = sb(stC, "mid", [128, 1])
    gew, Bgew = sb(stC, "gew", [128, 1])
    cnt, Bcnt = sb(stC, "cnt", [128, 32])
    pw, Bpw = sb(stC, "pw", [128, NITER])
    wt, Bwt = sb(stC, "wt", [128, NITER])
    wt2, Bwt2 = sb(stC, "wt2", [128, NITER])
    zcol, Bzcol = sb(stC, "zcol", [128, 1])
    bb = [sb(stC, "bb%d" % i, [128, 1]) for i in range(2)]
    aa = [sb(stC, "aa%d" % i, [128, 1]) for i in range(2)]
    osb2 = [sb(stC, "osb%d" % i, [128, 6, VW]) for i in range(2)]
    rden, Brden = sb(stC, "rden", [128, 6])
    attn_b, Battn_b = sb(stC, "attn_b", [128, 6, 128], BF16)
    aTst = [sb(stC, "aTst%d" % i, [128, 6, 128], BF16) for i in range(2)]
    psI = [ps(stC, "psI%d" % i, [128, 512]) for i in range(2)]
    psM, BpsM = ps(stC, "psM", [128, 1024], BF16)
    psL = [ps(stC, "psL%d" % i, [128, 512]) for i in range(2)]
    psO = [ps(stC, "psO%d" % i, [128, 512]) for i in range(2)]
    psX, BpsX = ps(stC, "psX", [128, 1024], BF16)

    K.dma("sp", Btmask, W=[Btmask]).dma_start(out=tmask[:], in_=tailmask_d[:, :])
    cstate = {"rh": 0, "pt": 0, "kv": 0}
    K.pool(W=[Bzcol]).memset(zcol[:], 0.0)
    for it in range(NITER):
        K.pool(W=[Bpw]).memset(pw[:, it:it + 1], float(2.0 ** -(it + 2)))

    def c_indexer(s):
        L = 1024 * (s + 1)
        NG = L // 512
        tcol = slice(s * 128, (s + 1) * 128)
        sm, Bsm = sm2[s % 2]
        for g in range(NG):
            for h in range(4):
                pI, BpI = psI[(g * 4 + h) % 2]
                K.pe(R=[BqiT, BkiT], W=[BpI]).matmul(pI[:, :], lhsT=qiT[:, h, tcol], rhs=kiT[:, g * 512:(g + 1) * 512], start=True, stop=True)
                r_t, Br_t = rh[cstate["rh"] % 4]
                cstate["rh"] += 1
                K.act(R=[BpI], W=[Br_t]).activation(out=r_t[:], in_=pI[:, :], func=AF.Relu)
                dst = sm[:, g * 512:(g + 1) * 512]
                if h == 0:
                    if g >= NG - 2:
                        tg = g - (NG - 2)
                        K.dve(R=[Br_t, Bsgn, Btmask], W=[Bsm]).scalar_tensor_tensor(
                            out=dst, in0=r_t[:], scalar=sgn[:, s, 0:1], in1=tmask[:, tg * 512:(tg + 1) * 512], op0=ALU.mult, op1=ALU.add)
                    else:
                        K.dve(R=[Br_t, Bsgn], W=[Bsm]).tensor_scalar(dst, r_t[:], sgn[:, s, 0:1], None, op0=ALU.mult)
                else:
                    K.dve(R=[Br_t, Bsgn, Bsm], W=[Bsm]).scalar_tensor_tensor(
                        out=dst, in0=r_t[:], scalar=sgn[:, s, h:h + 1], in1=dst, op0=ALU.mult, op1=ALU.add)

    def c_threshold(s):
        L = 1024 * (s + 1)
        sm, Bsm = sm2[s % 2]
        maskT, BmaskT = maskT2[s % 2]
        K.dve(R=[Bsm], W=[Bhi0]).tensor_reduce(out=hi0[:], in_=sm[:, 0:L], axis=AX.X, op=ALU.max)
        K.dve(R=[Bsm, Btmask], W=[Bttmp]).scalar_tensor_tensor(
            out=ttmp[:], in0=tmask[:], scalar=-2.0, in1=sm[:, L - 1024:L], op0=ALU.mult, op1=ALU.add)
        K.dve(R=[Bttmp], W=[Bm1]).tensor_reduce(out=m1[:], in_=ttmp[:], axis=AX.X, op=ALU.min)
        if L > 1024:
            K.dve(R=[Bsm], W=[Bm2]).tensor_reduce(out=m2[:], in_=sm[:, 0:L - 1024], axis=AX.X, op=ALU.min)
            K.dve(R=[Bm1, Bm2], W=[Blo]).tensor_tensor(out=lo[:], in0=m1[:], in1=m2[:], op=ALU.min)
        else:
            K.dve(R=[Bm1], W=[Blo]).tensor_copy(lo[:], m1[:])
        K.dve(R=[Bhi0, Blo], W=[Bw0]).tensor_tensor(out=w0[:], in0=hi0[:], in1=lo[:], op=ALU.subtract)
        K.dve(R=[Bw0], W=[Bgew]).tensor_scalar(gew[:], w0[:], 0.01, 1e-6, op0=ALU.mult, op1=ALU.add)
        K.dve(R=[Blo, Bgew], W=[Blo]).tensor_tensor(out=lo[:], in0=lo[:], in1=gew[:], op=ALU.subtract)
        K.dve(R=[Bw0], W=[Bw0]).tensor_scalar(w0[:], w0[:], 1.011, 2e-6, op0=ALU.mult, op1=ALU.add)
        K.dve(R=[Bpw, Bw0], W=[Bwt]).tensor_scalar(wt[:], pw[:], w0[:, 0:1], None, op0=ALU.mult)
        K.dve(R=[Bwt], W=[Bwt2]).tensor_scalar(wt2[:], wt[:], 2.0, None, op0=ALU.mult)
        K.dve(R=[Blo, Bwt2], W=[bb[0][1]]).tensor_tensor(out=bb[0][0][:], in0=lo[:], in1=wt2[:, 0:1], op=ALU.add)
        a_prev, Ba_prev = zcol, Bzcol
        for it in range(NITER):
            b_t, Bb_t = bb[it % 2]
            b_n, Bb_n = bb[(it + 1) % 2]
            a_n, Ba_n = aa[it % 2]
            K.dve(R=[Bsm, Ba_prev, Bb_t], W=[Bcjunk, Bcnt]).scalar_tensor_tensor(
                out=cjunk[:, 0:L], in0=sm[:, 0:L], scalar=a_prev[:, 0:1], in1=b_t[:, 0:1].to_broadcast([128, L]),
                op0=ALU.subtract, op1=ALU.is_ge, accum_out=cnt[:, it:it + 1])
            K.dve(R=[Ba_prev, Bb_t, Bwt], W=[Bb_n]).scalar_tensor_tensor(
                out=b_n[:], in0=a_prev[:], scalar=wt[:, it:it + 1], in1=b_t[:], op0=ALU.subtract, op1=ALU.add)
            K.dve(R=[Bcnt, Bwt2], W=[Ba_n]).tensor_scalar(a_n[:], cnt[:, it:it + 1], 255.5, wt2[:, it:it + 1], op0=ALU.is_ge, op1=ALU.mult)
            a_prev, Ba_prev = a_n, Ba_n
        b_f, Bb_f = bb[NITER % 2]
        K.dve(R=[Ba_prev, Bb_f, Bwt2], W=[Blo]).scalar_tensor_tensor(
            out=lo[:], in0=a_prev[:], scalar=wt2[:, NITER - 1:NITER], in1=b_f[:], op0=ALU.subtract, op1=ALU.add)
        for pc in range(L // 1024):
            m_t, Bm_t = mb[pc % 2]
            K.dve(R=[Bsm, Blo], W=[Bm_t]).tensor_scalar(
                m_t[:], sm[:, pc * 1024:(pc + 1) * 1024], lo[:, 0:1], -30000.0, op0=ALU.is_lt, op1=ALU.mult)
            for c in range(8):
                K.pe(R=[Bm_t, Bidb], W=[BpsM]).transpose(psM[:, c * 128:(c + 1) * 128], m_t[:, c * 128:(c + 1) * 128], identb[:])
            K.act(R=[BpsM], W=[BmaskT]).copy(maskT[:, pc * 8:(pc + 1) * 8, :].rearrange("p c t -> p (c t)"), psM[:, :])

    def c_attention(s):
        L = 1024 * (s + 1)
        NCH = L // 128
        tcol = slice(s * 128, (s + 1) * 128)
        maskT, BmaskT = maskT2[s % 2]
        osb, Bosb = osb2[s % 2]
        for h in range(6):
            pO, BpO = psO[h // 3]
            ocol = (h % 3) * VW
            for p0 in range(0, L, PIECE):
                pw = min(PIECE, L - p0)
                KT_t, BKT_t = KTp[cstate["kv"] % 3]
                V_t, BV_t = Vp[cstate["kv"] % 3]
                cstate["kv"] += 1
                K.dma("sp", BKT_t, R=[BKT], W=[BKT_t]).dma_start(out=KT_t[:, 0:pw], in_=KT_scr[h, :, p0:p0 + pw])
                K.dma("sp", BV_t, R=[BV], W=[BV_t]).dma_start(out=V_t[:, 0:pw // 128, :], in_=V_scr[h, :, p0 // 128:(p0 + pw) // 128, :])
                for gl in range(pw // 512):
                    g = p0 // 512 + gl
                    pL, BpL = psL[g % 2]
                    K.pe(R=[BmaskT, Bidb], W=[BpL]).matmul(
                        pL[:, :], lhsT=identb[:], rhs=maskT[:, g * 4:(g + 1) * 4, :].rearrange("p c t -> p (c t)"),
                        start=True, stop=False, skip_group_check=True)
                    for c in range(4):
                        cl = gl * 4 + c
                        K.pe(R=[BKT_t, BqT], W=[BpL]).matmul(
                            pL[:, c * 128:(c + 1) * 128], lhsT=KT_t[:, cl * 128:(cl + 1) * 128], rhs=qT[:, h, tcol],
                            start=False, stop=(c == 3), skip_group_check=True)
                    p_t, Bp_t = pT_[cstate["pt"] % 3]
                    cstate["pt"] += 1
                    K.act(R=[BpL], W=[Bp_t]).activation(out=p_t[:], in_=pL[:, :], func=AF.Exp, scale=float(128 ** -0.5))
                    for c in range(4):
                        cl = gl * 4 + c
                        ch = g * 4 + c
                        K.pe(R=[Bp_t, BV_t], W=[BpO]).matmul(
                            pO[:, ocol:ocol + VW], lhsT=p_t[:, c * 128:(c + 1) * 128], rhs=V_t[:, cl, :],
                            start=(ch == 0), stop=(ch == NCH - 1), skip_group_check=True)
            K.act(R=[BpO], W=[Bosb]).copy(osb[:, h, :], pO[:, ocol:ocol + VW])

    def c_finalize(s):
        osb, Bosb = osb2[s % 2]
        a_st, Ba_st = aTst[s % 2]
        K.dve(R=[Bosb], W=[Brden]).reciprocal(rden[:], osb[:, :, 128])
        K.dve(R=[Bosb, Brden], W=[Battn_b]).tensor_tensor(
            out=attn_b[:], in0=osb[:, :, 0:128], in1=rden[:].unsqueeze(2).to_broadcast([128, 6, 128]), op=ALU.mult)
        for h in range(6):
            K.pe(R=[Battn_b, Bidb], W=[BpsX]).transpose(psX[:, h * 128:(h + 1) * 128], attn_b[:, h, :], identb[:])
        K.act(R=[BpsX], W=[Ba_st]).copy(a_st[:], psX[:, 0:768].rearrange("p (h t) -> p h t", h=6))
        K.dma("sp", Ba_st, R=[Ba_st], W=[BaT]).dma_start(out=aT_scr[:, :, s * 128:(s + 1) * 128], in_=a_st[:])

    c_indexer(0)
    c_threshold(0)
    for s in range(NS):
        if s + 1 < NS:
            c_indexer(s + 1)
        c_attention(s)
        if s + 1 < NS:
            c_threshold(s + 1)
        c_finalize(s)
    K.barrier()
    stC.close()
    stAC.close()

    stD = ExitStack()
    hT, BhT_s = sb(stD, "hT", [128, KC, NTOK], BF16)
    K.dma("sp", BhT_s, R=[BhT], W=[BhT_s]).dma_start(out=hT[:], in_=hT_scr[:, :, :])
    attnT, BattnT = sb(stD, "attnT2", [128, 6, NOWN], BF16)
    K.dma("sp", BattnT, R=[BaT], W=[BattnT]).dma_start(out=attnT[:], in_=aT_scr[:, :, :])
    crossT, BcrossT = sb(stD, "crossT", [128, 4, NOWN], BF16)
    p2T, Bp2T = sb(stD, "p2T", [96, 8, NOWN], BF16)
    NTC = [(o, min(512, NOWN - o)) for o in range(0, NOWN, 512)]

    stX = ExitStack()
    gmem, Bgmem = sb(stX, "gmem", [128, D])
    gxq, Bgxq = sb(stX, "gxq", [128, 128])
    gxk, Bgxk = sb(stX, "gxk", [128, 128])
    Wkv, BWkv = sb(stX, "Wkv", [128, KC, 1024], BF16)
    Wxq, BWxq = sb(stX, "Wxq", [128, KC, 512], BF16)
    mt = [sb(stX, "mt%d" % i, [128, D]) for i in range(2)]
    mbf, Bmbf = sb(stX, "mbf", [128, D], BF16)
    mjunk, Bmjunk = sb(stX, "mjunk", [128, D], BF16)
    memT, BmemT = sb(stX, "memT", [128, KC, 256], BF16)
    xs_ss = [sb(stX, "xss%d" % i, [128, 1]) for i in range(2)]
    xs_sd = [sb(stX, "xsd%d" % i, [128, 1]) for i in range(2)]
    xs_rs = [sb(stX, "xrs%d" % i, [128, 1]) for i in range(2)]
    kraw, Bkraw = sb(stX, "kraw", [128, 4, 128])
    xsq, Bxsq = sb(stX, "xsq", [128, 512])
    xssq, Bxssq = sb(stX, "xssq", [128, 4])
    xfac, Bxfac = sb(stX, "xfac", [128, 4])
    knb, Bknb = sb(stX, "knb", [128, 4, 128], BF16)
    kmT, BkmT = sb(stX, "kmT", [128, 4, 256], BF16)
    vm, Bvm = sb(stX, "vm", [128, 2, 4, VW], BF16)
    xqT, BxqT = sb(stX, "xqT", [128, 4, NOWN], BF16)
    xp = [sb(stX, "xp%d" % i, [128, 2, 128], BF16) for i in range(2)]
    xo, Bxo = sb(stX, "xo", [128, 4, VW])
    xrd, Bxrd = sb(stX, "xrd", [128, 4])
    cross_b, Bcross_b = sb(stX, "cross_b", [128, 4, 128], BF16)
    psXT = [ps(stX, "psXT%d" % i, [128, 1024], BF16) for i in range(2)]
    psXA = [ps(stX, "psXA%d" % i, [128, 512]) for i in range(2)]
    psXL, BpsXL = ps(stX, "psXL", [128, 512])
    psXO, BpsXO = ps(stX, "psXO", [128, 512])
    psXK, BpsXK = ps(stX, "psXK", [128, 1024], BF16)

    K.dma("sp", Bgmem, W=[Bgmem]).dma_start(out=gmem[:], in_=gmem_d[:, :])
    K.dma("sp", Bgxq, W=[Bgxq]).dma_start(out=gxq[:], in_=gxq_d[:, :])
    K.dma("sp", Bgxk, W=[Bgxk]).dma_start(out=gxk[:], in_=gxk_d[:, :])
    for c0 in range(0, 1024, 512):
        K.dma("pool", BWkv, R=[Bw], W=[BWkv]).dma_start(
            out=Wkv[:, :, c0:c0 + 512], in_=wmkv_d[:, c0:c0 + 512].rearrange("(k p) c -> p k c", p=128))
    K.dma("pool", BWxq, R=[Bw], W=[BWxq]).dma_start(
        out=Wxq[:], in_=w_in[:, C_XQ:C_XQ + 512].rearrange("(k p) c -> p k c", p=128))
    K.pool(W=[Bvm]).memset(vm[:], 1.0)
    for mtile in range(2):
        (m_t, Bm_t) = mt[mtile]
        (ss, Bss), (sd, Bsd), (rs, Brs) = xs_ss[mtile], xs_sd[mtile], xs_rs[mtile]
        K.dma("sp", Bm_t, W=[Bm_t]).dma_start(out=m_t[:], in_=mem[mtile * 128:(mtile + 1) * 128, :])
        rms_stats(m_t[:], Bm_t, mjunk[:], Bmjunk, ss[:], Bss, sd[:], Bsd, rs[:], Brs, D)
        K.dve(R=[Bm_t, Brs, Bgmem], W=[Bmbf]).scalar_tensor_tensor(
            out=mbf[:], in0=m_t[:], scalar=rs[:, 0:1], in1=gmem[:], op0=ALU.mult, op1=ALU.mult)
        for half in range(2):
            pT, BpT = psXT[half]
            for kk in range(8):
                kc = half * 8 + kk
                K.pe(R=[Bmbf, Bidb], W=[BpT]).transpose(pT[:, kk * 128:(kk + 1) * 128], mbf[:, kc * 128:(kc + 1) * 128], identb[:])
            K.act(R=[BpT], W=[BmemT]).copy(memT[:, half * 8:(half + 1) * 8, mtile * 128:(mtile + 1) * 128], pT[:, :].rearrange("p (k t) -> p k t", k=8))
        for kc in range(KC):
            for cg in range(2):
                K.pe(R=[BmemT, BWkv], W=[psXA[cg][1]]).matmul(
                    psXA[cg][0][:, :], lhsT=memT[:, kc, mtile * 128:(mtile + 1) * 128], rhs=Wkv[:, kc, cg * 512:(cg + 1) * 512],
                    start=(kc == 0), stop=(kc == KC - 1))
        krf = kraw[:].rearrange("p h d -> p (h d)")
        K.act(R=[psXA[0][1]], W=[Bkraw]).copy(krf, psXA[0][0][:, :])
        K.act(R=[psXA[1][1]], W=[Bvm]).copy(vm[:, mtile, :, 0:128], psXA[1][0][:, :].rearrange("p (h d) -> p h d", h=4))
        head_fac(krf, Bkraw, 4, xsq, Bxsq, xssq, Bxssq, xfac, Bxfac)
        K.dve(R=[Bkraw, Bxfac], W=[Bkraw]).tensor_tensor(out=kraw[:], in0=kraw[:], in1=xfac[:].unsqueeze(2).to_broadcast([128, 4, 128]), op=ALU.mult)
        K.dve(R=[Bkraw, Bgxk], W=[Bknb]).tensor_tensor(out=knb[:], in0=kraw[:], in1=gxk[:].unsqueeze(1).to_broadcast([128, 4, 128]), op=ALU.mult)
        for h in range(4):
            K.pe(R=[Bknb, Bidb], W=[BpsXK]).transpose(psXK[:, h * 128:(h + 1) * 128], knb[:, h, :], identb[:])
        K.act(R=[BpsXK], W=[BkmT]).copy(kmT[:, :, mtile * 128:(mtile + 1) * 128], psXK[:, 0:512].rearrange("p (h t) -> p h t", h=4))
    for i in range(NS):
        for kc in range(KC):
            K.pe(R=[BhT_s, BWxq], W=[psXA[0][1]]).matmul(
                psXA[0][0][:, :], lhsT=hT[:, kc, i * 128:(i + 1) * 128], rhs=Wxq[:, kc, :], start=(kc == 0), stop=(kc == KC - 1))
        krf = kraw[:].rearrange("p h d -> p (h d)")
        K.act(R=[psXA[0][1]], W=[Bkraw]).copy(krf, psXA[0][0][:, :])
        head_fac(krf, Bkraw, 4, xsq, Bxsq, xssq, Bxssq, xfac, Bxfac)
        K.dve(R=[Bkraw, Bxfac], W=[Bkraw]).tensor_tensor(out=kraw[:], in0=kraw[:], in1=xfac[:].unsqueeze(2).to_broadcast([128, 4, 128]), op=ALU.mult)
        K.dve(R=[Bkraw, Bgxq], W=[Bknb]).tensor_tensor(out=knb[:], in0=kraw[:], in1=gxq[:].unsqueeze(1).to_broadcast([128, 4, 128]), op=ALU.mult)
        for h in range(4):
            K.pe(R=[Bknb, Bidb], W=[BpsXK]).transpose(psXK[:, h * 128:(h + 1) * 128], knb[:, h, :], identb[:])
        K.act(R=[BpsXK], W=[BxqT]).copy(xqT[:, :, i * 128:(i + 1) * 128], psXK[:, 0:512].rearrange("p (h t) -> p h t", h=4))
    xpc = 0
    for i in range(NS):
        tcol = slice(i * 128, (i + 1) * 128)
        for h in range(4):
            for mc in range(2):
                K.pe(R=[BkmT, BxqT], W=[BpsXL]).matmul(
                    psXL[:, mc * 128:(mc + 1) * 128], lhsT=kmT[:, h, mc * 128:(mc + 1) * 128], rhs=xqT[:, h, tcol], start=True, stop=True)
            p_t, Bp_t = xp[xpc % 2]
            xpc += 1
            K.act(R=[BpsXL], W=[Bp_t]).activation(out=p_t[:].rearrange("p c t -> p (c t)"), in_=psXL[:, 0:256], func=AF.Exp, scale=float(128 ** -0.5))
            for mc in range(2):
                K.pe(R=[Bp_t, Bvm], W=[BpsXO]).matmul(
                    psXO[:, 0:VW], lhsT=p_t[:, mc, :], rhs=vm[:, mc, h, :], start=(mc == 0), stop=(mc == 1))
            K.act(R=[BpsXO], W=[Bxo]).copy(xo[:, h, :], psXO[:, 0:VW])
        K.dve(R=[Bxo], W=[Bxrd]).reciprocal(xrd[:], xo[:, :, 128])
        K.dve(R=[Bxo, Bxrd], W=[Bcross_b]).tensor_tensor(
            out=cross_b[:], in0=xo[:, :, 0:128], in1=xrd[:].unsqueeze(2).to_broadcast([128, 4, 128]), op=ALU.mult)
        for h in range(4):
            K.pe(R=[Bcross_b, Bidb], W=[BpsXK]).transpose(psXK[:, h * 128:(h + 1) * 128], cross_b[:, h, :], identb[:])
        K.act(R=[BpsXK], W=[BcrossT]).copy(crossT[:, :, tcol], psXK[:, 0:512].rearrange("p (h t) -> p h t", h=4))
    K.barrier()
    stX.close()

    stP = ExitStack()
    Wup, BWup = sb(stP, "Wup", [128, KC, 768], BF16)
    Wpg, BWpg = sb(stP, "Wpg", [96, 4, 2, 192], BF16)
    pscale, Bpscale = sb(stP, "pscale", [96, 8])
    invcnt, Binvcnt = sb(stP, "invcnt", [96, 4, NOWN])
    U = [sb(stP, "U%d" % i, [96, NS, 144]) for i in range(2)]
    Wn = [sb(stP, "Wn%d" % i, [96, NS, 144]) for i in range(2)]
    pTt, BpTt = sb(stP, "pTt", [96, 8, NOWN], BF16)
    psU = [ps(stP, "psU%d" % i, [128, 512]) for i in range(3)]
    psG = [ps(stP, "psG%d" % i, [128, 512]) for i in range(2)]
    for c0 in (0, 512):
        w = min(512, 768 - c0)
        K.dma("pool", BWup, R=[Bw], W=[BWup]).dma_start(
            out=Wup[:, :, c0:c0 + w], in_=w_in[:, C_UP + c0:C_UP + c0 + w].rearrange("(k p) c -> p k c", p=128))
    K.dma("pool", BWpg, R=[Bw], W=[BWpg]).dma_start(out=Wpg[:], in_=wpg_d.rearrange("g (i p) o -> p g i o", p=96))
    K.dma("sp", Bpscale, W=[Bpscale]).dma_start(out=pscale[:], in_=pscale_d[:, :])
    K.dma("sp", Binvcnt, W=[Binvcnt]).dma_start(out=invcnt[:], in_=invcnt_d[:, :, :])
    tok_chunks = NTC + [(NOWN, 128)]
    for i in range(2):
        K.pool(W=[U[i][1]]).memset(U[i][0][:], 0.0)
        K.pool(W=[Wn[i][1]]).memset(Wn[i][0][:], 0.0)
    for cc in range(8):
        g = cc // 2
        for ti, (o, w) in enumerate(tok_chunks):
            pU, BpU = psU[ti % 3]
            for kc in range(KC):
                K.pe(R=[BWup, BhT_s], W=[BpU]).matmul(
                    pU[0:96, 0:w], lhsT=Wup[:, kc, cc * 96:(cc + 1) * 96], rhs=hT[:, kc, o:o + w], start=(kc == 0), stop=(kc == KC - 1))
            u_t, Bu_t = U[cc % 2]
            if o < NOWN:
                s0 = o // 128
                K.act(R=[BpU], W=[Bu_t]).copy(u_t[:, s0:s0 + w // 128, 16:144], pU[0:96, 0:w].rearrange("p (s t) -> p s t", t=128))
            else:
                K.act(R=[BpU], W=[Bu_t]).copy(u_t[:, :, 0:16], pU[0:96, 0:NS * 16].rearrange("p (s t) -> p s t", t=16))
        cur, Bcur = u_t, Bu_t
        d = 1
        wi_ = 0
        while d < (2 << g):
            nxt, Bnxt = Wn[wi_ % 2]
            wi_ += 1
            K.dve(R=[Bcur], W=[Bnxt]).tensor_tensor(out=nxt[:, :, d:144], in0=cur[:, :, d:144], in1=cur[:, :, 0:144 - d], op=ALU.add)
            cur, Bcur = nxt, Bnxt
            d *= 2
        fin, Bfin = Wn[wi_ % 2]
        K.dve(R=[Bcur, Binvcnt], W=[Bfin]).tensor_tensor(
            out=fin[:, :, 16:144], in0=cur[:, :, 16:144], in1=invcnt[:, g, :].rearrange("p (s t) -> p s t", t=128), op=ALU.mult)
        K.dve(R=[Bfin, Bu_t], W=[BpTt]).tensor_tensor(
            out=pTt[:, cc, :].rearrange("p (s t) -> p s t", t=128), in0=fin[:, :, 16:144], in1=u_t[:, :, 16:144], op=ALU.subtract)
    for co in range(8):
        g = co // 2
        for ti, (o, w) in enumerate(NTC):
            pG, BpG = psG[ti % 2]
            for ci in range(2):
                K.pe(R=[BWpg, BpTt], W=[BpG]).matmul(
                    pG[0:96, 0:w], lhsT=Wpg[:, g, ci, (co % 2) * 96:(co % 2) * 96 + 96], rhs=pTt[:, 2 * g + ci, o:o + w], start=(ci == 0), stop=(ci == 1))
            K.act(R=[BpG, Bpscale], W=[Bp2T]).activation(out=p2T[:, co, o:o + w], in_=pG[0:96, 0:w], func=AF.Copy, scale=pscale[:, co:co + 1])
    K.barrier()
    stP.close()

    stM = ExitStack()
    mergedT, BmergedT = sb(stM, "mergedT", [128, KC, NOWN], BF16)
    bgT, BbgT = sb(stM, "bgT", [128, 48])
    Wg_ = [sb(stM, "Wg%d" % i, [128, KC, 3, 128], BF16) for i in range(2)]
    Wpo_ = [sb(stM, "Wpo%d" % i, [96, 8, 128], BF16) for i in range(2)]
    Wao_ = [sb(stM, "Wao%d" % i, [128, 6, 128], BF16) for i in range(2)]
    Wco_ = [sb(stM, "Wco%d" % i, [128, 4, 128], BF16) for i in range(2)]
    sg = [sb(stM, "sg%d" % i, [128, 512]) for i in range(3)]
    macc = [sb(stM, "macc%d" % i, [128, 512]) for i in range(2)]
    mtmp = [sb(stM, "mtmp%d" % i, [128, 512]) for i in range(2)]
    psGa = [ps(stM, "psGa%d" % i, [128, 512]) for i in range(3)]
    psBr = [ps(stM, "psBr%d" % i, [128, 512]) for i in range(3)]
    K.dma("sp", BbgT, W=[BbgT]).dma_start(out=bgT[:], in_=bgT_d[:, :])
    mc_ = 0

    def merge_loads(j):
        wb = j % 2
        (Wg_t, BWg_t), (Wpo_t, BWpo_t), (Wao_t, BWao_t), (Wco_t, BWco_t) = Wg_[wb], Wpo_[wb], Wao_[wb], Wco_[wb]
        for br in range(3):
            c0 = C_G + br * D + j * 128
            K.dma("pool", BWg_t, R=[Bw], W=[BWg_t]).dma_start(
                out=Wg_t[:, :, br, :], in_=w_in[:, c0:c0 + 128].rearrange("(k p) c -> p k c", p=128))
        K.dma("pool", BWpo_t, R=[Bw], W=[BWpo_t]).dma_start(
            out=Wpo_t[:], in_=wpo_d[:, j * 128:(j + 1) * 128].rearrange("(k p) c -> p k c", p=96))
        K.dma("pool", BWao_t, R=[Bw], W=[BWao_t]).dma_start(
            out=Wao_t[:], in_=wao_d[:, j * 128:(j + 1) * 128].rearrange("(k p) c -> p k c", p=128))
        K.dma("pool", BWco_t, R=[Bw], W=[BWco_t]).dma_start(
            out=Wco_t[:], in_=wco_d[:, j * 128:(j + 1) * 128].rearrange("(k p) c -> p k c", p=128))

    merge_loads(0)
    for j in range(KC):
        if j + 1 < KC:
            merge_loads(j + 1)
        wb = j % 2
        (Wg_t, BWg_t), (Wpo_t, BWpo_t), (Wao_t, BWao_t), (Wco_t, BWco_t) = Wg_[wb], Wpo_[wb], Wao_[wb], Wco_[wb]
        for (o, w) in NTC:
            for br in range(3):
                pg_, Bpg_ = psGa[br]
                for kc in range(KC):
                    K.pe(R=[BWg_t, BhT_s], W=[Bpg_]).matmul(
                        pg_[:, 0:w], lhsT=Wg_t[:, kc, br, :], rhs=hT[:, kc, o:o + w], start=(kc == 0), stop=(kc == KC - 1))
                K.act(R=[Bpg_, BbgT], W=[sg[br][1]]).activation(
                    out=sg[br][0][:, 0:w], in_=pg_[:, 0:w], func=AF.Sigmoid, bias=bgT[:, br * 16 + j:br * 16 + j + 1], scale=1.0)
            pb0, Bpb0 = psBr[0]
            for kc in range(8):
                K.pe(R=[BWpo_t, Bp2T], W=[Bpb0]).matmul(pb0[:, 0:w], lhsT=Wpo_t[:, kc, :], rhs=p2T[:, kc, o:o + w], start=(kc == 0), stop=(kc == 7))
            pb1, Bpb1 = psBr[1]
            for kc in range(6):
                K.pe(R=[BWao_t, BattnT], W=[Bpb1]).matmul(pb1[:, 0:w], lhsT=Wao_t[:, kc, :], rhs=attnT[:, kc, o:o + w], start=(kc == 0), stop=(kc == 5))
            pb2, Bpb2 = psBr[2]
            for kc in range(4):
                K.pe(R=[BWco_t, BcrossT], W=[Bpb2]).matmul(pb2[:, 0:w], lhsT=Wco_t[:, kc, :], rhs=crossT[:, kc, o:o + w], start=(kc == 0), stop=(kc == 3))
            ma, Bma = macc[mc_ % 2]
            mt_, Bmt_ = mtmp[mc_ % 2]
            mc_ += 1
            K.dve(R=[sg[0][1], Bpb0], W=[Bma]).tensor_tensor(out=ma[:, 0:w], in0=sg[0][0][:, 0:w], in1=pb0[:, 0:w], op=ALU.mult)
            K.dve(R=[sg[1][1], Bpb1], W=[Bmt_]).tensor_tensor(out=mt_[:, 0:w], in0=sg[1][0][:, 0:w], in1=pb1[:, 0:w], op=ALU.mult)
            K.dve(R=[Bma, Bmt_], W=[Bma]).tensor_tensor(out=ma[:, 0:w], in0=ma[:, 0:w], in1=mt_[:, 0:w], op=ALU.add)
            K.dve(R=[sg[2][1], Bpb2], W=[Bmt_]).tensor_tensor(out=mt_[:, 0:w], in0=sg[2][0][:, 0:w], in1=pb2[:, 0:w], op=ALU.mult)
            K.dve(R=[Bma, Bmt_], W=[BmergedT]).tensor_tensor(out=mergedT[:, j, o:o + w], in0=ma[:, 0:w], in1=mt_[:, 0:w], op=ALU.add)
    K.dma("sp", BmergedT, R=[BmergedT], W=[BmT]).dma_start(out=mT_scr[:, :, :], in_=mergedT[:])
    K.barrier()
    stM.close()
    stD.close()

    stE = ExitStack()
    acc, Bacc = sb(stE, "acc", [128, NS, D])
    h2T, Bh2T = sb(stE, "h2T", [128, KC, NOWN], BF16)
    comb, Bcomb = sb(stE, "comb", [128, NS, 16])
    stO = ExitStack()
    mergedT, BmergedT = sb(stO, "mergedT2", [128, KC, NOWN], BF16)
    K.dma("sp", BmergedT, R=[BmT], W=[BmergedT]).dma_start(out=mergedT[:], in_=mT_scr[:, :, :])
    gffn, Bgffn = sb(stO, "gffn", [128, D])
    Wo_ = [sb(stO, "Wo%d" % i, [128, KC, 512], BF16) for i in range(2)]
    xres = [sb(stO, "xres%d" % i, [128, 512]) for i in range(2)]
    wr, Bwr = sb(stO, "wr", [128, KC, 20])
    x2n, Bx2n = sb(stO, "x2n", [128, D])
    x2b, Bx2b = sb(stO, "x2b", [128, D], BF16)
    x2nT, Bx2nT = sb(stO, "x2nT", [128, KC, 128])
    ojunk, Bojunk = sb(stO, "ojunk", [128, D], BF16)
    o_ss, Bo_ss = sb(stO, "o_ss", [128, 1])
    o_sd, Bo_sd = sb(stO, "o_sd", [128, 1])
    o_rs, Bo_rs = sb(stO, "o_rs", [128, 1])
    lg, Blg = sb(stO, "lg", [128, NS, 20])
    rt = {n: sb(stO, "rt_" + n, [128, NS] + sz) for n, sz in
          (("mg", []), ("ohg", [4]), ("eg", [4]), ("sg", []), ("pg", []), ("les", [4]), ("m1", []), ("oh1", [4]), ("le2", [4]),
           ("m2", []), ("oh2", [4]), ("dm", []), ("ex", []), ("den", []), ("w1", []), ("w2", []), ("cl", [4]), ("cl2", [4]), ("prod", [4, 4]))}
    psW = [ps(stO, "psW%d" % i, [128, 512]) for i in range(2)]
    psFT = [ps(stO, "psFT%d" % i, [128, 512]) for i in range(2)]
    psBT = [ps(stO, "psBT%d" % i, [128, 1024], BF16) for i in range(2)]
    psR, BpsR = ps(stO, "psR", [128, 512])
    K.dma("sp", Bgffn, W=[Bgffn]).dma_start(out=gffn[:], in_=gffn_d[:, :])
    K.dma("sp", Bwr, W=[Bwr]).dma_start(out=wr[:], in_=wr_d.rearrange("(k p) c -> p k c", p=128))
    xc = 0
    for cg in range(4):
        W_t, BW_t = Wo_[cg % 2]
        K.dma("pool", BW_t, R=[Bw], W=[BW_t]).dma_start(
            out=W_t[:], in_=wo_d[:, cg * 512:(cg + 1) * 512].rearrange("(k p) c -> p k c", p=128))
        for i in range(NS):
            pW, BpW = psW[i % 2]
            for kc in range(KC):
                K.pe(R=[BmergedT, BW_t], W=[BpW]).matmul(
                    pW[:, :], lhsT=mergedT[:, kc, i * 128:(i + 1) * 128], rhs=W_t[:, kc, :], start=(kc == 0), stop=(kc == KC - 1))
            xr, Bxr = xres[xc % 2]
            xc += 1
            K.dma("sp", Bxr, R=[Bx_own], W=[Bxr]).dma_start(out=xr[:], in_=x_own[i * 128:(i + 1) * 128, cg * 512:(cg + 1) * 512])
            K.dve(R=[BpW, Bxr], W=[Bacc]).tensor_tensor(out=acc[:, i, cg * 512:(cg + 1) * 512], in0=pW[:, :], in1=xr[:], op=ALU.add)
    R_ = lambda n: rt[n][0]
    B_ = lambda n: rt[n][1]
    for i in range(NS):
        x2 = acc[:, i, :]
        rms_stats(x2, Bacc, ojunk[:], Bojunk, o_ss[:], Bo_ss, o_sd[:], Bo_sd, o_rs[:], Bo_rs, D)
        K.dve(R=[Bacc, Bo_rs, Bgffn], W=[Bx2n]).scalar_tensor_tensor(
            out=x2n[:], in0=x2, scalar=o_rs[:, 0:1], in1=gffn[:], op0=ALU.mult, op1=ALU.mult)
        K.pool(R=[Bx2n], W=[Bx2b]).tensor_copy(x2b[:], x2n[:])
        for half in range(2):
            pT, BpT = psBT[half]
            for kk in range(8):
                kc = half * 8 + kk
                K.pe(R=[Bx2b, Bidb], W=[BpT]).transpose(pT[:, kk * 128:(kk + 1) * 128], x2b[:, kc * 128:(kc + 1) * 128], identb[:])
            K.act(R=[BpT], W=[Bh2T]).copy(h2T[:, half * 8:(half + 1) * 8, i * 128:(i + 1) * 128], pT[:, :].rearrange("p (k t) -> p k t", k=8))
        for q4 in range(4):
            pF, BpF = psFT[q4 % 2]
            for kk in range(4):
                kc = q4 * 4 + kk
                K.pe(R=[Bx2n, Bidf], W=[BpF]).transpose(pF[:, kk * 128:(kk + 1) * 128], x2n[:, kc * 128:(kc + 1) * 128], identf[:])
            K.dve(R=[BpF], W=[Bx2nT]).tensor_copy(x2nT[:, q4 * 4:(q4 + 1) * 4, :].rearrange("p k t -> p (k t)"), pF[:, :])
        for kc in range(KC):
            K.pe(R=[Bx2nT, Bwr], W=[BpsR]).matmul(psR[:, 0:20], lhsT=x2nT[:, kc, :], rhs=wr[:, kc, :], start=(kc == 0), stop=(kc == KC - 1))
        K.act(R=[BpsR], W=[Blg]).copy(lg[:, i, :], psR[:, 0:20])
    def bc(ap, shape):
        return ap.to_broadcast(shape)
    lgG = lg[:, :, 0:4]
    lgE = lg[:, :, 4:20].rearrange("p s (g j) -> p s g j", g=4)
    K.dve(R=[Blg], W=[B_("mg")]).tensor_reduce(out=R_("mg")[:], in_=lgG, axis=AX.X, op=ALU.max)
    K.dve(R=[Blg, B_("mg")], W=[B_("eg")]).tensor_tensor(out=R_("eg")[:], in0=lgG, in1=bc(R_("mg")[:].unsqueeze(2), [128, NS, 4]), op=ALU.subtract)
    K.dve(R=[B_("eg")], W=[B_("ohg")]).tensor_scalar(R_("ohg")[:], R_("eg")[:], 0.0, None, op0=ALU.is_ge)
    K.act(R=[B_("eg")], W=[B_("eg")]).activation(out=R_("eg")[:], in_=R_("eg")[:], func=AF.Exp)
    K.dve(R=[B_("eg")], W=[B_("sg")]).tensor_reduce(out=R_("sg")[:], in_=R_("eg")[:], axis=AX.X, op=ALU.add)
    K.dve(R=[B_("sg")], W=[B_("pg")]).reciprocal(R_("pg")[:], R_("sg")[:])
    K.dve(R=[Blg, B_("ohg")], W=[B_("prod")]).tensor_tensor(
        out=R_("prod")[:], in0=lgE, in1=bc(R_("ohg")[:].unsqueeze(3), [128, NS, 4, 4]), op=ALU.mult)
    K.dve(R=[B_("prod")], W=[B_("les")]).tensor_reduce(
        out=R_("les")[:], in_=R_("prod")[:].rearrange("p s g j -> p s j g"), axis=AX.X, op=ALU.add)
    K.dve(R=[B_("les")], W=[B_("m1")]).tensor_reduce(out=R_("m1")[:], in_=R_("les")[:], axis=AX.X, op=ALU.max)
    K.dve(R=[B_("les"), B_("m1")], W=[B_("oh1")]).tensor_tensor(out=R_("oh1")[:], in0=R_("les")[:], in1=bc(R_("m1")[:].unsqueeze(2), [128, NS, 4]), op=ALU.is_ge)
    K.dve(R=[B_("oh1"), B_("les")], W=[B_("le2")]).scalar_tensor_tensor(
        out=R_("le2")[:], in0=R_("oh1")[:], scalar=-1.0e30, in1=R_("les")[:], op0=ALU.mult, op1=ALU.add)
    K.dve(R=[B_("le2")], W=[B_("m2")]).tensor_reduce(out=R_("m2")[:], in_=R_("le2")[:], axis=AX.X, op=ALU.max)
    K.dve(R=[B_("le2"), B_("m2")], W=[B_("oh2")]).tensor_tensor(out=R_("oh2")[:], in0=R_("le2")[:], in1=bc(R_("m2")[:].unsqueeze(2), [128, NS, 4]), op=ALU.is_ge)
    K.dve(R=[B_("m2"), B_("m1")], W=[B_("dm")]).tensor_tensor(out=R_("dm")[:], in0=R_("m2")[:], in1=R_("m1")[:], op=ALU.subtract)
    K.act(R=[B_("dm")], W=[B_("ex")]).activation(out=R_("ex")[:], in_=R_("dm")[:], func=AF.Exp)
    K.dve(R=[B_("ex")], W=[B_("den")]).tensor_scalar(R_("den")[:], R_("ex")[:], 1.0, None, op0=ALU.add)
    K.dve(R=[B_("den")], W=[B_("w1")]).reciprocal(R_("w1")[:], R_("den")[:])
    K.dve(R=[B_("w1"), B_("pg")], W=[B_("w1")]).tensor_tensor(out=R_("w1")[:], in0=R_("w1")[:], in1=R_("pg")[:], op=ALU.mult)
    K.dve(R=[B_("w1"), B_("ex")], W=[B_("w2")]).tensor_tensor(out=R_("w2")[:], in0=R_("w1")[:], in1=R_("ex")[:], op=ALU.mult)
    K.dve(R=[B_("oh1"), B_("w1")], W=[B_("cl")]).tensor_tensor(out=R_("cl")[:], in0=R_("oh1")[:], in1=bc(R_("w1")[:].unsqueeze(2), [128, NS, 4]), op=ALU.mult)
    K.dve(R=[B_("oh2"), B_("w2")], W=[B_("cl2")]).tensor_tensor(out=R_("cl2")[:], in0=R_("oh2")[:], in1=bc(R_("w2")[:].unsqueeze(2), [128, NS, 4]), op=ALU.mult)
    K.dve(R=[B_("cl"), B_("cl2")], W=[B_("cl2")]).tensor_tensor(out=R_("cl2")[:], in0=R_("cl2")[:], in1=R_("cl")[:], op=ALU.add)
    K.dve(R=[B_("cl2"), B_("ohg")], W=[Bcomb]).tensor_tensor(
        out=comb[:].rearrange("p s (g j) -> p s g j", g=4), in0=bc(R_("cl2")[:].unsqueeze(2), [128, NS, 4, 4]),
        in1=bc(R_("ohg")[:].unsqueeze(3), [128, NS, 4, 4]), op=ALU.mult)
    K.barrier()
    stO.close()

    stF = ExitStack()
    Wgu = [sb(stF, "Wgu%d" % i, [128, KC, 2, 128], BF16) for i in range(4)]
    Wd = [sb(stF, "Wd%d" % i, [128, 4, D], BF16) for i in range(2)]
    actT = [sb(stF, "actT%d" % i, [128, 4, NOWN], BF16) for i in range(2)]
    sil = [sb(stF, "sil%d" % i, [128, 512]) for i in range(2)]
    psGU = [ps(stF, "psGU%d" % i, [128, 512]) for i in range(4)]
    psD = [ps(stF, "psD%d" % i, [128, 512]) for i in range(4)]
    guc = 0
    slc = 0
    pdc = 0
    for e in range(NE):
        Wd_t, BWd_t = Wd[e % 2]
        a_t, Ba_t = actT[e % 2]
        for fc in range(4):
            Wgu_t, BWgu_t = Wgu[guc % 4]
            guc += 1
            K.dma("pool", BWgu_t, R=[Bw], W=[BWgu_t]).dma_start(
                out=Wgu_t[:, :, 0, :], in_=weg_d[e, :, fc * 128:(fc + 1) * 128].rearrange("(k p) c -> p k c", p=128))
            K.dma("pool", BWgu_t, R=[Bw], W=[BWgu_t]).dma_start(
                out=Wgu_t[:, :, 1, :], in_=weu_d[e, :, fc * 128:(fc + 1) * 128].rearrange("(k p) c -> p k c", p=128))
            if fc == 0:
                K.dma("pool", BWd_t, R=[Bw], W=[BWd_t]).dma_start(
                    out=Wd_t[:], in_=wed_d[e, :, :].rearrange("(k p) c -> p k c", p=128))
            for (o, w) in NTC:
                pg_, Bpg_ = psGU[pdc % 2 * 2]
                pu_, Bpu_ = psGU[pdc % 2 * 2 + 1]
                pdc += 1
                for kc in range(KC):
                    K.pe(R=[BWgu_t, Bh2T], W=[Bpg_]).matmul(pg_[:, 0:w], lhsT=Wgu_t[:, kc, 0, :], rhs=h2T[:, kc, o:o + w], start=(kc == 0), stop=(kc == KC - 1))
                for kc in range(KC):
                    K.pe(R=[BWgu_t, Bh2T], W=[Bpu_]).matmul(pu_[:, 0:w], lhsT=Wgu_t[:, kc, 1, :], rhs=h2T[:, kc, o:o + w], start=(kc == 0), stop=(kc == KC - 1))
                s_t, Bs_t = sil[slc % 2]
                slc += 1
                K.act(R=[Bpg_], W=[Bs_t]).activation(out=s_t[:, 0:w], in_=pg_[:, 0:w], func=AF.Silu)
                K.dve(R=[Bs_t, Bpu_], W=[Ba_t]).tensor_tensor(out=a_t[:, fc, o:o + w], in0=s_t[:, 0:w], in1=pu_[:, 0:w], op=ALU.mult)
        for i in range(NS):
            for cg in range(4):
                pD, BpD = psD[cg]
                for fc in range(4):
                    K.pe(R=[Ba_t, BWd_t], W=[BpD]).matmul(
                        pD[:, :], lhsT=a_t[:, fc, i * 128:(i + 1) * 128], rhs=Wd_t[:, fc, cg * 512:(cg + 1) * 512], start=(fc == 0), stop=(fc == 3))
                dst = acc[:, i, cg * 512:(cg + 1) * 512]
                K.dve(R=[BpD, Bcomb, Bacc], W=[Bacc]).scalar_tensor_tensor(
                    out=dst, in0=pD[:, :], scalar=comb[:, i, e:e + 1], in1=dst, op0=ALU.mult, op1=ALU.add)
    for i in range(NS):
        K.dma("sp", By, R=[Bacc], W=[By]).dma_start(out=y_out[i * 128:(i + 1) * 128, :], in_=acc[:, i, :])
    K.finish()
    K.emit()
    stF.close()
    stE.close()
    es.close()
    return nc


def _prep_inputs(inp, NS):
    f32 = np.float32
    S = 1024 * NS
    NT = S // 128
    x = np.ascontiguousarray(np.asarray(inp["x"], dtype=f32)[0])
    pos = np.asarray(inp["positions"])[0].astype(np.int32)
    sq = lambda k: np.asarray(inp[k], dtype=f32)[0]
    rep = lambda v: np.ascontiguousarray(np.broadcast_to(np.asarray(v, dtype=f32)[None, :], (128, v.shape[0])))
    w_router = np.ascontiguousarray(np.concatenate([sq("w_router_group"), sq("w_router_expert")], axis=1))
    invf128 = (10000.0 ** (-np.arange(0, 128, 2, dtype=np.float32) / np.float32(128))).astype(f32)
    invf64 = (10000.0 ** (-np.arange(0, 64, 2, dtype=np.float32) / np.float32(64))).astype(f32)
    invfT = rep(np.concatenate([invf128, invf128, invf64, invf64]))
    hp = np.float32(np.pi / 2)
    offsT = rep(np.concatenate([np.zeros(64, f32), np.full(64, hp, f32), np.zeros(32, f32), np.full(32, hp, f32)]))
    shared = {
        "x_all": x,
        "posT_all": np.ascontiguousarray(pos.reshape(NT, 128).T),
        "mem": np.ascontiguousarray(np.asarray(inp["mem"], dtype=f32)[0]),
        "w_in": sq("w_in"),
        "gmix_rep": rep(sq("g_mix")), "gffn_rep": rep(sq("g_ffn")), "gmem_rep": rep(sq("g_mem")),
        "bgT": np.ascontiguousarray(sq("b_gate").reshape(48, 128).T),
        "w_pool_grp": sq("w_pool_grp"),
        "pscaleT": np.ascontiguousarray(sq("pool_scale").reshape(8, 96).T),
        "gq_rep": rep(sq("q_norm_g")), "gk_rep": rep(sq("k_norm_g")),
        "gxq_rep": rep(sq("xq_norm_g")), "gxk_rep": rep(sq("xk_norm_g")),
        "w_mem_kv": sq("w_mem_kv"), "w_pool_out": sq("w_pool_out"), "w_attn_out": sq("w_attn_out"),
        "w_cross_out": sq("w_cross_out"), "w_o": sq("w_o"), "w_router": w_router,
        "w_e_gate": sq("w_e_gate"), "w_e_up": sq("w_e_up"), "w_e_down": sq("w_e_down"),
        "ident": np.eye(128, dtype=f32), "invfT": invfT, "offsT": offsT,
    }
    maps = []
    for c in range(NCORES):
        rows = np.concatenate([np.arange((8 * s + c) * 128, (8 * s + c + 1) * 128) for s in range(NS)])
        x_own = np.zeros((NS * 128 + 128, D), f32)
        x_own[:NS * 128] = x[rows]
        for s in range(NS):
            st = (8 * s + c) * 128
            if st >= 16:
                x_own[NS * 128 + s * 16:NS * 128 + (s + 1) * 16] = x[st - 16:st]
        t_i = np.arange(128)[:, None]
        j_i = np.arange(1024)[None, :]
        tailmask = np.where(j_i > 128 * c + t_i, np.float32(NEG), np.float32(0)).astype(f32)
        invcnt = np.zeros((96, 4, NS * 128), f32)
        for g, w in enumerate((2, 4, 8, 16)):
            invcnt[:, g, :] = (1.0 / np.minimum(rows + 1, w).astype(np.float64)).astype(f32)[None, :]
        m = dict(shared)
        m.update({"x_own": x_own, "posT_own": np.ascontiguousarray(pos[rows].reshape(NS, 128).T),
                  "tailmask": tailmask, "invcnt": invcnt})
        maps.append(m)
    return maps


_NC_CACHE = {}


def run(inp, NS):
    if NS not in _NC_CACHE:
        _NC_CACHE[NS] = build(NS)
    nc = _NC_CACHE[NS]
    maps = _prep_inputs(inp, NS)
    res = run_bass_kernel_spmd(nc, maps, core_ids=list(range(NCORES)))
    S = 1024 * NS
    out = np.zeros((1, S, D), np.float32)
    for c in range(NCORES):
        y = res.results[c]["y_own"]
        for s in range(NS):
            st = (8 * s + c) * 128
            out[0, st:st + 128] = y[s * 128:(s + 1) * 128]
    return out


def kernel(**inputs):
    return run(inputs, 8)
```

```python
import numpy as np
from contextlib import ExitStack
import concourse.bass as bass
import concourse.mybir as mybir
from concourse.bass_utils import run_bass_kernel_spmd

F32 = mybir.dt.float32
BF16 = mybir.dt.bfloat16
I32 = mybir.dt.int32
AF = mybir.ActivationFunctionType
ALU = mybir.AluOpType
AX = mybir.AxisListType

NCORES = 8
D = 2048
KC = 16
C_UP, C_Q, C_K, C_V, C_QI, C_KI, C_WI, C_XQ, C_G = 0, 768, 1536, 2304, 3072, 3328, 3392, 3396, 3908
IN_COLS = 10052
EPS = 1e-6
TWO_PI = 2.0 * np.pi
CW1 = 6.28125
CW2 = TWO_PI - 6.28125
NEG = -1.0e30
NITER = 18
VW = 132
NE = 16
FF = 512
DEBUG_NAMES = None
DVE_GAP = 2
PI_LO = 3.1415925


class Buf:
    __slots__ = ("name", "lw", "rd", "psum")

    def __init__(self, name, psum=False):
        self.name = name
        self.lw = None
        self.rd = []
        self.psum = psum


class _Rec:
    def __init__(self, K, eng, R, W, sem):
        self.K, self.eng, self.R, self.W, self.sem = K, eng, R, W, sem

    def __getattr__(self, name):
        def f(*a, **kw):
            self.K._record(self.eng, name, a, kw, self.R, self.W, self.sem)
        return f


class Kern:
    ENGS = ("pe", "act", "dve", "pool", "sp")

    def __init__(self, nc, es):
        self.nc, self.es = nc, es
        self.ops = {e: [] for e in self.ENGS}
        self.cnt = {e: 0 for e in self.ENGS}
        self.pending = {e: [] for e in self.ENGS}
        self.dsem = {}
        self.pad_ap = None
        self.csem = {e: es.enter_context(nc.semaphore("cs_" + e)) for e in ("pe", "act", "dve", "pool")}

    def pe(self, R=(), W=()): return _Rec(self, "pe", R, W, None)
    def act(self, R=(), W=()): return _Rec(self, "act", R, W, None)
    def dve(self, R=(), W=()): return _Rec(self, "dve", R, W, None)
    def pool(self, R=(), W=()): return _Rec(self, "pool", R, W, None)
    def dma(self, q, sem, R=(), W=()): return _Rec(self, q, R, W, sem)

    def _record(self, eng, name, a, kw, R, W, sem):
        toks = list(self.pending[eng])
        self.pending[eng] = []
        for b in R:
            if b.lw is not None:
                toks.append(b.lw)
            if b.psum:
                toks.extend(t for t in b.rd if not (t[0] == "c" and t[1] == eng))
        for b in W:
            if b.lw is not None:
                toks.append(b.lw)
            toks.extend(b.rd)
        if sem is None:
            if eng == "pe":
                toks = [t for t in toks if not (t[0] == "c" and t[1] == "pe")]
            if eng == "dve" and self.pad_ap is not None:
                own = [t[2] for t in toks if t[0] == "c" and t[1] == "dve"]
                toks = [t for t in toks if not (t[0] == "c" and t[1] == "dve")]
                if own:
                    between = self.cnt["dve"] - max(own)
                    for _ in range(max(0, DVE_GAP - between)):
                        self.cnt["dve"] += 1
                        self.ops["dve"].append(([], "memset", (self.pad_ap, 0.0), {}, ("c", "dve", self.cnt["dve"])))
            self.cnt[eng] += 1
            tok = ("c", eng, self.cnt[eng])
        else:
            if sem not in self.dsem:
                self.dsem[sem] = [self.es.enter_context(self.nc.semaphore("ds_%d" % len(self.dsem))), 0]
            self.dsem[sem][1] += 1
            tok = ("d", sem, 16 * self.dsem[sem][1])
        self.ops[eng].append((toks, name, a, kw, tok))
        for b in R:
            b.rd.append(tok)
        for b in W:
            b.lw = tok
            b.rd = []

    def barrier(self):
        toks = [("c", e, self.cnt[e]) for e in ("pe", "act", "dve", "pool") if self.cnt[e] > 0]
        toks += [("d", s, 16 * v[1]) for s, v in self.dsem.items()]
        for e in self.ENGS:
            self.pending[e].extend(toks)

    def finish(self):
        self.barrier()
        for e in self.ENGS:
            self.ops[e].append((self.pending[e], None, (), {}, None))
            self.pending[e] = []

    def emit(self):
        nc = self.nc
        miles = {e: set() for e in self.ENGS}
        for e in self.ENGS:
            for toks, _, _, _, _ in self.ops[e]:
                for t in toks:
                    if t[0] == "c":
                        miles[t[1]].add(t[2])
        rank = {}
        for e in self.ENGS:
            rank[e] = {idx: r + 1 for r, idx in enumerate(sorted(miles[e]))}
        with nc.Block() as block:
            for ename, attr in (("pe", "tensor"), ("act", "scalar"), ("dve", "vector"), ("pool", "gpsimd"), ("sp", "sync")):
                ops = self.ops[ename]

                def body(e, ops=ops, ename=ename):
                    seen = {}
                    for toks, name, a, kw, tok in ops:
                        need = {}
                        for t in toks:
                            if t[0] == "c":
                                key, val = ("c", t[1]), rank[t[1]][t[2]]
                            else:
                                key, val = ("d", t[1]), t[2]
                            if seen.get(key, 0) >= val:
                                continue
                            need[key] = max(need.get(key, 0), val)
                        for key, val in need.items():
                            seen[key] = val
                            sem = self.csem[key[1]] if key[0] == "c" else self.dsem[key[1]][0]
                            e.wait_ge(sem, val)
                        if name is None:
                            continue
                        ins = getattr(e, name)(*a, **kw)
                        if DEBUG_NAMES is not None:
                            DEBUG_NAMES[ins.ins.name] = (ename, name, {k: str(v) for k, v in kw.items() if k.startswith("op") or k == "func"})
                        if tok[0] == "c":
                            if tok[2] in rank[ename]:
                                ins.then_inc(self.csem[ename], 1)
                        else:
                            ins.then_inc(self.dsem[tok[1]][0], 16)

                getattr(block, attr)(body)


def build(NS):
    S = 1024 * NS
    NT = S // 128
    NOWN = NS * 128
    NTOK = NOWN + 128
    nc = bass.Bass("TRN2", target_bir_lowering=False)
    es = ExitStack()
    K = Kern(nc, es)

    def din(name, shape, dt=F32):
        return nc.dram_tensor(name, list(shape), dt, kind="ExternalInput").ap()

    x_all = din("x_all", [S, D])
    x_own = din("x_own", [NTOK, D])
    posT_all = din("posT_all", [128, NT], I32)
    posT_own = din("posT_own", [128, NS], I32)
    mem = din("mem", [256, D])
    w_in = din("w_in", [D, IN_COLS])
    gmix_d = din("gmix_rep", [128, D])
    gffn_d = din("gffn_rep", [128, D])
    gmem_d = din("gmem_rep", [128, D])
    bgT_d = din("bgT", [128, 48])
    wpg_d = din("w_pool_grp", [4, 192, 192])
    pscale_d = din("pscaleT", [96, 8])
    gq_d = din("gq_rep", [128, 128])
    gk_d = din("gk_rep", [128, 128])
    gxq_d = din("gxq_rep", [128, 128])
    gxk_d = din("gxk_rep", [128, 128])
    wmkv_d = din("w_mem_kv", [D, 1024])
    wpo_d = din("w_pool_out", [768, D])
    wao_d = din("w_attn_out", [768, D])
    wco_d = din("w_cross_out", [512, D])
    wo_d = din("w_o", [D, D])
    wr_d = din("w_router", [D, 20])
    weg_d = din("w_e_gate", [NE, D, FF])
    weu_d = din("w_e_up", [NE, D, FF])
    wed_d = din("w_e_down", [NE, FF, D])
    ident_d = din("ident", [128, 128])
    invf_d = din("invfT", [128, 192])
    offs_d = din("offsT", [128, 192])
    tailmask_d = din("tailmask", [128, 1024])
    invcnt_d = din("invcnt", [96, 4, NOWN])
    y_out = nc.dram_tensor("y_own", [NOWN, D], F32, kind="ExternalOutput").ap()
    KT_scr = nc.dram_tensor("KT_scr", [6, 128, S], BF16, kind="Internal").ap()
    V_scr = nc.dram_tensor("V_scr", [6, 128, NT, VW], BF16, kind="Internal").ap()
    hT_scr = nc.dram_tensor("hT_scr", [128, KC, NTOK], BF16, kind="Internal").ap()
    aT_scr = nc.dram_tensor("aT_scr", [128, 6, NOWN], BF16, kind="Internal").ap()
    mT_scr = nc.dram_tensor("mT_scr", [128, KC, NOWN], BF16, kind="Internal").ap()

    def sb(st, name, shape, dt=F32):
        t = st.enter_context(nc.sbuf_tensor("s_" + name, list(shape), dt))
        return t, Buf(name)

    def ps(st, name, shape, dt=F32):
        t = st.enter_context(nc.psum_tensor("p_" + name, list(shape), dt))
        return t, Buf(name, psum=True)

    Bx_all, Bx_own, Bw = Buf("x_all"), Buf("x_own"), Buf("weights")
    BKT, BV, BhT, By = Buf("KT_scr"), Buf("V_scr"), Buf("hT_scr"), Buf("y")
    BaT, BmT = Buf("aT_scr"), Buf("mT_scr")

    identf, Bidf = sb(es, "identf", [128, 128])
    identb, Bidb = sb(es, "identb", [128, 128], BF16)
    invf, Binvf = sb(es, "invf", [128, 192])
    offs, Boffs = sb(es, "offs", [128, 192])
    K.dma("sp", Bidf, W=[Bidf]).dma_start(out=identf[:], in_=ident_d[:, :])
    K.dma("sp", Binvf, W=[Binvf]).dma_start(out=invf[:], in_=invf_d[:, :])
    K.dma("sp", Boffs, W=[Boffs]).dma_start(out=offs[:], in_=offs_d[:, :])
    K.dve(R=[Bidf], W=[Bidb]).tensor_copy(identb[:], identf[:])

    def rms_stats(xt_ap, Bxt, junk_ap, Bjunk, ss, Bss, sd, Bsd, rstd, Brstd, width):
        K.dve(W=[Bss]).memset(ss, 0.0)
        K.act(R=[Bxt, Bss], W=[Bjunk, Bss]).activation(out=junk_ap, in_=xt_ap, func=AF.Square, accum_out=ss)
        K.act(R=[Bss], W=[Bsd]).activation(out=sd, in_=ss, func=AF.Sqrt, bias=EPS, scale=1.0 / width)
        K.dve(R=[Bsd], W=[Brstd]).reciprocal(rstd, sd)

    def trig_group(pos4, Bpos, n, ang_t, Bang, angk_t, Bangk, angi_t, Bangi, csg_t, Bcsg):
        shp = [128, n, 192]
        ang, angk, angi, csg = ang_t[:, 0:n, :], angk_t[:, 0:n, :], angi_t[:, 0:n, :], csg_t[:, 0:n, :]
        K.dve(R=[Binvf, Bpos], W=[Bang]).tensor_tensor(
            out=ang, in0=pos4.unsqueeze(2).to_broadcast(shp), in1=invf[:].unsqueeze(1).to_broadcast(shp), op=ALU.mult)
        K.dve(R=[Bang, Boffs], W=[Bang]).tensor_tensor(out=ang, in0=ang, in1=offs[:].unsqueeze(1).to_broadcast(shp), op=ALU.add)
        K.dve(R=[Bang], W=[Bangk]).tensor_scalar(angk, ang, 1.0 / TWO_PI, None, op0=ALU.mult)
        K.dve(R=[Bangk], W=[Bangi]).tensor_copy(angi, angk)
        K.dve(R=[Bangi], W=[Bangk]).tensor_copy(angk, angi)
        K.dve(R=[Bang, Bangk], W=[Bang]).scalar_tensor_tensor(out=ang, in0=angk, scalar=-CW1, in1=ang, op0=ALU.mult, op1=ALU.add)
        K.dve(R=[Bang, Bangk], W=[Bang]).scalar_tensor_tensor(out=ang, in0=angk, scalar=-CW2, in1=ang, op0=ALU.mult, op1=ALU.add)
        K.dve(R=[Bang], W=[Bang]).tensor_scalar(ang, ang, -PI_LO, PI_LO, op0=ALU.max, op1=ALU.min)
        K.act(R=[Bang], W=[Bcsg]).activation(out=csg, in_=ang, func=AF.Sin)

    def rotary(eng, src, Bsrc, dst, Bdst, cA, sA, cB, sB, Bcs, tmp, Btmp, nh, hd):
        x1, x2 = src[:, :, 0:hd], src[:, :, hd:2 * hd]
        bcst = lambda v: v.unsqueeze(1).to_broadcast([128, nh, hd])
        t1, t2 = tmp[:, 0:nh, 0:hd], tmp[:, 0:nh, hd:2 * hd]
        eng(R=[Bsrc, Bcs], W=[Btmp]).tensor_tensor(out=t1, in0=x1, in1=bcst(cA), op=ALU.mult)
        eng(R=[Bsrc, Bcs], W=[Btmp]).tensor_tensor(out=t2, in0=x2, in1=bcst(sA), op=ALU.mult)
        eng(R=[Btmp], W=[Bdst]).tensor_tensor(out=dst[:, :, 0:hd], in0=t1, in1=t2, op=ALU.subtract)
        eng(R=[Bsrc, Bcs], W=[Btmp]).tensor_tensor(out=t1, in0=x2, in1=bcst(cB), op=ALU.mult)
        eng(R=[Bsrc, Bcs], W=[Btmp]).tensor_tensor(out=t2, in0=x1, in1=bcst(sB), op=ALU.mult)
        eng(R=[Btmp], W=[Bdst]).tensor_tensor(out=dst[:, :, hd:2 * hd], in0=t1, in1=t2, op=ALU.add)

    def head_fac(raw, Braw, nh, sq, Bsq, ssq, Bssq, fac, Bfac):
        K.act(R=[Braw], W=[Bsq]).activation(out=sq[:, 0:nh * 128], in_=raw, func=AF.Square)
        K.dve(R=[Bsq], W=[Bssq]).tensor_reduce(
            out=ssq[:, 0:nh], in_=sq[:, 0:nh * 128].rearrange("p (h d) -> p h d", h=nh), axis=AX.X, op=ALU.add)
        K.act(R=[Bssq], W=[Bssq]).activation(out=ssq[:, 0:nh], in_=ssq[:, 0:nh], func=AF.Sqrt, bias=EPS, scale=1.0 / 128)
        K.dve(R=[Bssq], W=[Bfac]).reciprocal(fac[:, 0:nh], ssq[:, 0:nh])

    stAC = ExitStack()
    kiT, BkiT = sb(stAC, "kiT", [64, S], BF16)
    qT, BqT = sb(stAC, "qT", [128, 6, NOWN], BF16)
    qiT, BqiT = sb(stAC, "qiT", [64, 4, NOWN], BF16)
    sgn, Bsgn = sb(stAC, "sgn", [128, NS, 4])

    stAB = ExitStack()
    gmix, Bgmix = sb(stAB, "gmix", [128, D])
    gk, Bgk = sb(stAB, "gk", [128, 128])
    gq, Bgq = sb(stAB, "gq", [128, 128])
    posA_i, BposAi = sb(stAB, "posA_i", [128, NT], I32)
    posA, BposA = sb(stAB, "posA", [128, NT])
    posO_i, BposOi = sb(stAB, "posO_i", [128, NS], I32)
    posO, BposO = sb(stAB, "posO", [128, NS])
    WA, BWA = sb(stAB, "WA", [128, KC, 1600], BF16)
    xt = [sb(stAB, "xt%d" % i, [128, D]) for i in range(2)]
    xb = [sb(stAB, "xb%d" % i, [128, D], BF16) for i in range(2)]
    xT = [sb(stAB, "xT%d" % i, [128, KC, 128], BF16) for i in range(2)]
    st_ss = [sb(stAB, "ss%d" % i, [128, 1]) for i in range(2)]
    st_sd = [sb(stAB, "sd%d" % i, [128, 1]) for i in range(2)]
    st_rs = [sb(stAB, "rs%d" % i, [128, 1]) for i in range(2)]
    ang, Bang_ = sb(stAB, "ang", [128, 4, 192])
    angk, Bangk_ = sb(stAB, "angk", [128, 4, 192])
    angi, Bangi_ = sb(stAB, "angi", [128, 4, 192], I32)
    csg = [sb(stAB, "csg%d" % i, [128, 4, 192]) for i in range(2)]
    gtab = [sb(stAB, "gtab%d" % i, [128, 4, 256]) for i in range(2)]
    Ksb = [sb(stAB, "Ksb%d" % i, [128, 6, 128]) for i in range(2)]
    sq, Bsq = sb(stAB, "sq", [128, 768])
    ssq, Bssq = sb(stAB, "ssq", [128, 6])
    fac, Bfac = sb(stAB, "fac", [128, 6])
    kn, Bkn = sb(stAB, "kn", [128, 6, 128])
    rtmp, Brtmp = sb(stAB, "rtmp", [128, 6, 128])
    kr = [sb(stAB, "kr%d" % i, [128, 6, 128], BF16) for i in range(3)]
    kif = [sb(stAB, "kif%d" % i, [128, 4, 64]) for i in range(2)]
    itmp, Bitmp = sb(stAB, "itmp", [128, 4, 64])
    kir = [sb(stAB, "kir%d" % i, [128, 4, 64], BF16) for i in range(3)]
    KTst = [sb(stAB, "KTst%d" % i, [128, 6, 512], BF16) for i in range(2)]
    Vst = [sb(stAB, "Vst%d" % i, [128, 6, 4, VW], BF16) for i in range(2)]
    wisb = [sb(stAB, "wis%d" % i, [128, 4]) for i in range(2)]
    aw, Baw = sb(stAB, "aw", [128, 4])
    psT = [ps(stAB, "psT%d" % i, [128, 1024], BF16) for i in range(2)]
    psA = [ps(stAB, "psA%d" % i, [128, 512]) for i in range(4)]
    psK, BpsK = ps(stAB, "psK", [128, 1024], BF16)
    psK2 = ps(stAB, "psK2", [128, 1024], BF16)

    K.dma("sp", Bgmix, W=[Bgmix]).dma_start(out=gmix[:], in_=gmix_d[:, :])
    K.dma("sp", Bgk, W=[Bgk]).dma_start(out=gk[:], in_=gk_d[:, :])
    K.dma("sp", Bgq, W=[Bgq]).dma_start(out=gq[:], in_=gq_d[:, :])
    K.dma("sp", BposAi, W=[BposAi]).dma_start(out=posA_i[:], in_=posT_all[:, :])
    K.dma("sp", BposOi, W=[BposOi]).dma_start(out=posO_i[:], in_=posT_own[:, :])
    K.dve(R=[BposAi], W=[BposA]).tensor_copy(posA[:], posA_i[:])
    K.dve(R=[BposOi], W=[BposO]).tensor_copy(posO[:], posO_i[:])
    for c0 in range(0, 1536, 512):
        K.dma("pool", BWA, R=[Bw], W=[BWA]).dma_start(
            out=WA[:, :, c0:c0 + 512], in_=w_in[:, C_K + c0:C_K + c0 + 512].rearrange("(k p) c -> p k c", p=128))
    K.dma("pool", BWA, R=[Bw], W=[BWA]).dma_start(
        out=WA[:, :, 1536:1600], in_=w_in[:, C_KI:C_KI + 64].rearrange("(k p) c -> p k c", p=128))
    for i in range(2):
        K.pool(W=[Vst[i][1]]).memset(Vst[i][0][:], 1.0)

    colsA = [(0, 512), (512, 512), (1024, 512), (1536, 64)]
    colsB = [(0, 512), (512, 256), (768, 324)]
    items = [("A", j) for j in range(NT)] + [("B", i) for i in range(NS + 1)]

    def stL(g):
        kind, t = items[g]
        src_rows, Bsrc = (x_all[t * 128:(t + 1) * 128, :], Bx_all) if kind == "A" else (x_own[t * 128:(t + 1) * 128, :], Bx_own)
        x_t, Bx_t = xt[g % 2]
        K.dma("sp", Bx_t, R=[Bsrc], W=[Bx_t]).dma_start(out=x_t[:], in_=src_rows)

    def stN(g):
        b = g % 2
        (x_t, Bx_t), (xb_t, Bxb_t) = xt[b], xb[b]
        (ss, Bss), (sd, Bsd), (rs, Brs) = st_ss[b], st_sd[b], st_rs[b]
        rms_stats(x_t[:], Bx_t, xb_t[:], Bxb_t, ss[:], Bss, sd[:], Bsd, rs[:], Brs, D)
        K.dve(R=[Bx_t, Brs, Bgmix], W=[Bxb_t]).scalar_tensor_tensor(
            out=xb_t[:], in0=x_t[:], scalar=rs[:, 0:1], in1=gmix[:], op0=ALU.mult, op1=ALU.mult)

    def stT(g):
        kind, t = items[g]
        b = g % 2
        (xb_t, Bxb_t), (xT_t, BxT_t) = xb[b], xT[b]
        for half in range(2):
            pT, BpT = psT[half]
            for kk in range(8):
                kc = half * 8 + kk
                K.pe(R=[Bxb_t, Bidb], W=[BpT]).transpose(pT[:, kk * 128:(kk + 1) * 128], xb_t[:, kc * 128:(kc + 1) * 128], identb[:])
            dst = xT_t[:, half * 8:(half + 1) * 8, :].rearrange("p k t -> p (k t)")
            if half == 0:
                K.act(R=[BpT], W=[BxT_t]).copy(dst, pT[:, :])
            else:
                K.dve(R=[BpT], W=[BxT_t]).tensor_copy(dst, pT[:, :])
        if kind == "B":
            K.dma("sp", BxT_t, R=[BxT_t], W=[BhT]).dma_start(out=hT_scr[:, :, t * 128:(t + 1) * 128], in_=xT_t[:])

    def stM(g):
        kind, t = items[g]
        b = g % 2
        xT_t, BxT_t = xT[b]
        Ks, BKs = Ksb[b]
        Ksf = Ks[:].rearrange("p h d -> p (h d)")
        ki_f, Bki_f = kif[b]
        if kind == "B" and t == 0:
            K.dma("pool", BWA, R=[Bw], W=[BWA]).dma_start(
                out=WA[:, :, 0:512], in_=w_in[:, C_Q:C_Q + 512].rearrange("(k p) c -> p k c", p=128))
            K.dma("pool", BWA, R=[Bw], W=[BWA]).dma_start(
                out=WA[:, :, 512:768], in_=w_in[:, C_Q + 512:C_Q + 768].rearrange("(k p) c -> p k c", p=128))
            K.dma("pool", BWA, R=[Bw], W=[BWA]).dma_start(
                out=WA[:, :, 768:1092], in_=w_in[:, C_QI:C_QI + 324].rearrange("(k p) c -> p k c", p=128))
        if kind == "B" and t == NS:
            return
        if g % 4 == 0 and g + 4 < NI - 1:
            emit_trig(g // 4 + 1)
        cols = colsA if kind == "A" else colsB
        for kc in range(KC):
            for cg, (c0, w) in enumerate(cols):
                K.pe(R=[BxT_t, BWA], W=[psA[cg][1]]).matmul(
                    psA[cg][0][:, 0:w], lhsT=xT_t[:, kc, :], rhs=WA[:, kc, c0:c0 + w], start=(kc == 0), stop=(kc == KC - 1))
        K.act(R=[psA[0][1]], W=[BKs]).copy(Ksf[:, 0:512], psA[0][0][:, 0:512])
        K.act(R=[psA[1][1]], W=[BKs]).copy(Ksf[:, 512:768], psA[1][0][:, 0:256])
        if kind == "A":
            Vs, BVs = Vst[(t // 4) % 2]
            jj = t % 4
            K.act(R=[psA[1][1]], W=[BVs]).copy(Vs[:, 0:2, jj, 0:128], psA[1][0][:, 256:512].rearrange("p (h d) -> p h d", h=2))
            K.act(R=[psA[2][1]], W=[BVs]).copy(Vs[:, 2:6, jj, 0:128], psA[2][0][:, 0:512].rearrange("p (h d) -> p h d", h=4))
            K.act(R=[psA[3][1]], W=[Bki_f]).copy(ki_f[:, 0, :], psA[3][0][:, 0:64])
        else:
            K.act(R=[psA[2][1]], W=[Bki_f]).copy(ki_f[:], psA[2][0][:, 0:256].rearrange("p (h d) -> p h d", h=4))
            K.act(R=[psA[2][1]], W=[wisb[b][1]]).copy(wisb[b][0][:], psA[2][0][:, 320:324])

    def stR(g):
        kind, t = items[g]
        if kind == "B" and t == NS:
            return
        b = g % 2
        b3 = g % 3
        Ks, BKs = Ksb[b]
        cs_t, Bcs_t = csg[(g // 4) % 2][0][:, g % 4, :], csg[(g // 4) % 2][1]
        ki_f, Bki_f = kif[b]
        ki_r, Bki_r = kir[b3]
        kr_t, Bkr_t = kr[b3]
        gain, Bgain = (gk, Bgk) if kind == "A" else (gq, Bgq)
        Ksf = Ks[:].rearrange("p h d -> p (h d)")
        head_fac(Ksf, BKs, 6, sq, Bsq, ssq, Bssq, fac, Bfac)
        K.dve(R=[BKs, Bfac], W=[Bkn]).tensor_tensor(out=kn[:], in0=Ks[:], in1=fac[:, 0:6].unsqueeze(2).to_broadcast([128, 6, 128]), op=ALU.mult)
        g_t, Bg_t = gtab[(g // 4) % 2][0][:, g % 4, :], gtab[(g // 4) % 2][1]
        rotary(K.pool, kn, Bkn, kr_t, Bkr_t, g_t[:, 0:64], g_t[:, 64:128], g_t[:, 128:192], g_t[:, 192:256], Bg_t, rtmp, Brtmp, 6, 64)
        cI, sI = cs_t[:, 160:192], cs_t[:, 128:160]
        if kind == "A":
            rotary(K.pool, ki_f[:, 0:1, :], Bki_f, ki_r[:, 0:1, :], Bki_r, cI, sI, cI, sI, Bcs_t, itmp, Bitmp, 1, 32)
        else:
            wis, Bwis = wisb[b]
            K.dve(R=[Bwis], W=[Bsgn]).tensor_scalar(sgn[:, t, :], wis[:], 0.0, 2.0, op0=ALU.is_ge, op1=ALU.mult)
            K.dve(R=[Bsgn], W=[Bsgn]).tensor_scalar(sgn[:, t, :], sgn[:, t, :], -1.0, None, op0=ALU.add)
            K.dve(R=[Bwis, Bsgn], W=[Baw]).scalar_tensor_tensor(out=aw[:], in0=wis[:], scalar=1.0 / 16, in1=sgn[:, t, :], op0=ALU.mult, op1=ALU.mult)
            rotary(K.pool, ki_f, Bki_f, itmp, Bitmp, cI, sI, cI, sI, Bcs_t, rtmp, Brtmp, 4, 32)
            K.pool(R=[Bitmp, Baw], W=[Bki_r]).tensor_tensor(out=ki_r[:], in0=itmp[:], in1=aw[:].unsqueeze(2).to_broadcast([128, 4, 64]), op=ALU.mult)

    def stO(g):
        kind, t = items[g]
        if kind == "B" and t == NS:
            return
        b3 = g % 3
        ki_r, Bki_r = kir[b3]
        kr_t, Bkr_t = kr[b3]
        for h in range(6):
            K.pe(R=[Bkr_t, Bidb], W=[BpsK]).transpose(psK[:, h * 128:(h + 1) * 128], kr_t[:, h, :], identb[:])
        if kind == "A":
            sbi = (t // 4) % 2
            jj = t % 4
            Vs, BVs = Vst[sbi]
            KTs, BKTs = KTst[sbi]
            K.pe(R=[Bki_r, Bidb], W=[psK2[1]]).transpose(psK2[0][0:64, 0:128], ki_r[:, 0, :], identb[:])
            K.act(R=[BpsK], W=[BKTs]).copy(KTs[:, :, jj * 128:(jj + 1) * 128], psK[:, 0:768].rearrange("p (h t) -> p h t", h=6))
            K.dve(R=[psK2[1]], W=[BkiT]).tensor_copy(kiT[:, t * 128:(t + 1) * 128], psK2[0][0:64, 0:128])
            if jj == 3:
                t0 = (t - 3) * 128
                K.dma("sp", BKTs, R=[BKTs], W=[BKT]).dma_start(
                    out=KT_scr[:, :, t0:t0 + 512].rearrange("h d t -> d h t"), in_=KTs[:])
                K.dma("sp", BVs, R=[BVs], W=[BV]).dma_start(
                    out=V_scr[:, :, t - 3:t + 1, :].rearrange("h p j c -> p h j c"), in_=Vs[:])
        else:
            K.act(R=[BpsK], W=[BqT]).copy(qT[:, :, t * 128:(t + 1) * 128], psK[:, 0:768].rearrange("p (h t) -> p h t", h=6))
            for h in range(4):
                K.pe(R=[Bki_r, Bidb], W=[psK2[1]]).transpose(psK2[0][0:64, h * 128:(h + 1) * 128], ki_r[:, h, :], identb[:])
            K.dve(R=[psK2[1]], W=[BqiT]).tensor_copy(qiT[:, :, t * 128:(t + 1) * 128], psK2[0][0:64, 0:512].rearrange("p (h t) -> p h t", h=4))

    NI = len(items)

    def emit_trig(q):
        kind, t0 = items[4 * q]
        n = min(4, NI - 1 - 4 * q)
        pos4, Bpos = (posA[:, t0:t0 + n], BposA) if kind == "A" else (posO[:, t0:t0 + n], BposO)
        c_t, Bc_t = csg[q % 2]
        trig_group(pos4, Bpos, n, ang, Bang_, angk, Bangk_, angi, Bangi_, c_t, Bc_t)
        gain, Bgain = (gk, Bgk) if kind == "A" else (gq, Bgq)
        gt, Bgt = gtab[q % 2]
        cosv, sinv = c_t[:, 0:n, 64:128], c_t[:, 0:n, 0:64]
        g1 = gain[:, 0:64].unsqueeze(1).to_broadcast([128, n, 64])
        g2 = gain[:, 64:128].unsqueeze(1).to_broadcast([128, n, 64])
        K.dve(R=[Bc_t, Bgain], W=[Bgt]).tensor_tensor(out=gt[:, 0:n, 0:64], in0=cosv, in1=g1, op=ALU.mult)
        K.dve(R=[Bc_t, Bgain], W=[Bgt]).tensor_tensor(out=gt[:, 0:n, 64:128], in0=sinv, in1=g2, op=ALU.mult)
        K.dve(R=[Bc_t, Bgain], W=[Bgt]).tensor_tensor(out=gt[:, 0:n, 128:192], in0=cosv, in1=g2, op=ALU.mult)
        K.dve(R=[Bc_t, Bgain], W=[Bgt]).tensor_tensor(out=gt[:, 0:n, 192:256], in0=sinv, in1=g1, op=ALU.mult)

    emit_trig(0)
    stL(0)
    stL(1)
    stN(0)
    stL(2)
    stN(1)
    stT(0)
    for n in range(NI + 3):
        if n + 3 < NI:
            stL(n + 3)
        if n + 2 < NI:
            stN(n + 2)
        if n + 1 < NI:
            stT(n + 1)
        if 0 <= n - 3 < NI:
            stO(n - 3)
        if 0 <= n - 1 < NI:
            stR(n - 1)
        if n < NI:
            stM(n)
    K.barrier()
    stAB.close()

    stC = ExitStack()
    LMAX = S
    PIECE = 1024
    U8 = mybir.dt.uint8
    sm2 = [sb(stC, "sm%d" % i, [128, LMAX]) for i in range(2)]
    cjunk, Bcjunk = sb(stC, "cjunk", [128, LMAX], U8)
    tmask, Btmask = sb(stC, "tmask", [128, 1024])
    ttmp, Bttmp = sb(stC, "ttmp", [128, 1024])
    rh = [sb(stC, "rh%d" % i, [128, 512]) for i in range(4)]
    mb = [sb(stC, "mb%d" % i, [128, 1024], BF16) for i in range(2)]
    maskT2 = [sb(stC, "maskT%d" % i, [128, LMAX // 128, 128], BF16) for i in range(2)]
    KTp = [sb(stC, "KTp%d" % i, [128, PIECE], BF16) for i in range(3)]
    Vp = [sb(stC, "Vp%d" % i, [128, PIECE // 128, VW], BF16) for i in range(3)]
    pT_ = [sb(stC, "pT%d" % i, [128, 512], BF16) for i in range(3)]
    hi0, Bhi0 = sb(stC, "hi0", [128, 1])
    m1, Bm1 = sb(stC, "m1", [128, 1])
    m2, Bm2 = sb(stC, "m2", [128, 1])
    lo, Blo = sb(stC, "lo", [128, 1])
    w0, Bw0 = sb(stC, "w0", [128, 1])
    mid, Bmid = sb(stC, "mid", [128, 1])
    gew, Bgew = sb(stC, "gew", [128, 1])
    cnt, Bcnt = sb(stC, "cnt", [128, 32])
    pw, Bpw = sb(stC, "pw", [128, NITER])
    wt, Bwt = sb(stC, "wt", [128, NITER])
    wt2, Bwt2 = sb(stC, "wt2", [128, NITER])
    zcol, Bzcol = sb(stC, "zcol", [128, 1])
    bb = [sb(stC, "bb%d" % i, [128, 1]) for i in range(2)]
    aa = [sb(stC, "aa%d" % i, [128, 1]) for i in range(2)]
    osb2 = [sb(stC, "osb%d" % i, [128, 6, VW]) for i in range(2)]
    rden, Brden = sb(stC, "rden", [128, 6])
    attn_b, Battn_b = sb(stC, "attn_b", [128, 6, 128], BF16)
    aTst = [sb(stC, "aTst%d" % i, [128, 6, 128], BF16) for i in range(2)]
    psI = [ps(stC, "psI%d" % i, [128, 512]) for i in range(2)]
    psM, BpsM = ps(stC, "psM", [128, 1024], BF16)
    psL = [ps(stC, "psL%d" % i, [128, 512]) for i in range(2)]
    psO = [ps(stC, "psO%d" % i, [128, 512]) for i in range(2)]
    psX, BpsX = ps(stC, "psX", [128, 1024], BF16)

    K.dma("sp", Btmask, W=[Btmask]).dma_start(out=tmask[:], in_=tailmask_d[:, :])
    cstate = {"rh": 0, "pt": 0, "kv": 0}
    K.pool(W=[Bzcol]).memset(zcol[:], 0.0)
    for it in range(NITER):
        K.pool(W=[Bpw]).memset(pw[:, it:it + 1], float(2.0 ** -(it + 2)))

    def c_indexer(s):
        L = 1024 * (s + 1)
        NG = L // 512
        tcol = slice(s * 128, (s + 1) * 128)
        sm, Bsm = sm2[s % 2]
        for g in range(NG):
            for h in range(4):
                pI, BpI = psI[(g * 4 + h) % 2]
                K.pe(R=[BqiT, BkiT], W=[BpI]).matmul(pI[:, :], lhsT=qiT[:, h, tcol], rhs=kiT[:, g * 512:(g + 1) * 512], start=True, stop=True)
                r_t, Br_t = rh[cstate["rh"] % 4]
                cstate["rh"] += 1
                K.act(R=[BpI], W=[Br_t]).activation(out=r_t[:], in_=pI[:, :], func=AF.Relu)
                dst = sm[:, g * 512:(g + 1) * 512]
                if h == 0:
                    if g >= NG - 2:
                        tg = g - (NG - 2)
                        K.dve(R=[Br_t, Bsgn, Btmask], W=[Bsm]).scalar_tensor_tensor(
                            out=dst, in0=r_t[:], scalar=sgn[:, s, 0:1], in1=tmask[:, tg * 512:(tg + 1) * 512], op0=ALU.mult, op1=ALU.add)
                    else:
                        K.dve(R=[Br_t, Bsgn], W=[Bsm]).tensor_scalar(dst, r_t[:], sgn[:, s, 0:1], None, op0=ALU.mult)
                else:
                    K.dve(R=[Br_t, Bsgn, Bsm], W=[Bsm]).scalar_tensor_tensor(
                        out=dst, in0=r_t[:], scalar=sgn[:, s, h:h + 1], in1=dst, op0=ALU.mult, op1=ALU.add)

    def c_threshold(s):
        L = 1024 * (s + 1)
        sm, Bsm = sm2[s % 2]
        maskT, BmaskT = maskT2[s % 2]
        K.dve(R=[Bsm], W=[Bhi0]).tensor_reduce(out=hi0[:], in_=sm[:, 0:L], axis=AX.X, op=ALU.max)
        K.dve(R=[Bsm, Btmask], W=[Bttmp]).scalar_tensor_tensor(
            out=ttmp[:], in0=tmask[:], scalar=-2.0, in1=sm[:, L - 1024:L], op0=ALU.mult, op1=ALU.add)
        K.dve(R=[Bttmp], W=[Bm1]).tensor_reduce(out=m1[:], in_=ttmp[:], axis=AX.X, op=ALU.min)
        if L > 1024:
            K.dve(R=[Bsm], W=[Bm2]).tensor_reduce(out=m2[:], in_=sm[:, 0:L - 1024], axis=AX.X, op=ALU.min)
            K.dve(R=[Bm1, Bm2], W=[Blo]).tensor_tensor(out=lo[:], in0=m1[:], in1=m2[:], op=ALU.min)
        else:
            K.dve(R=[Bm1], W=[Blo]).tensor_copy(lo[:], m1[:])
        K.dve(R=[Bhi0, Blo], W=[Bw0]).tensor_tensor(out=w0[:], in0=hi0[:], in1=lo[:], op=ALU.subtract)
        K.dve(R=[Bw0], W=[Bgew]).tensor_scalar(gew[:], w0[:], 0.01, 1e-6, op0=ALU.mult, op1=ALU.add)
        K.dve(R=[Blo, Bgew], W=[Blo]).tensor_tensor(out=lo[:], in0=lo[:], in1=gew[:], op=ALU.subtract)
        K.dve(R=[Bw0], W=[Bw0]).tensor_scalar(w0[:], w0[:], 1.011, 2e-6, op0=ALU.mult, op1=ALU.add)
        K.dve(R=[Bpw, Bw0], W=[Bwt]).tensor_scalar(wt[:], pw[:], w0[:, 0:1], None, op0=ALU.mult)
        K.dve(R=[Bwt], W=[Bwt2]).tensor_scalar(wt2[:], wt[:], 2.0, None, op0=ALU.mult)
        K.dve(R=[Blo, Bwt2], W=[bb[0][1]]).tensor_tensor(out=bb[0][0][:], in0=lo[:], in1=wt2[:, 0:1], op=ALU.add)
        a_prev, Ba_prev = zcol, Bzcol
        for it in range(NITER):
            b_t, Bb_t = bb[it % 2]
            b_n, Bb_n = bb[(it + 1) % 2]
            a_n, Ba_n = aa[it % 2]
            K.dve(R=[Bsm, Ba_prev, Bb_t], W=[Bcjunk, Bcnt]).scalar_tensor_tensor(
                out=cjunk[:, 0:L], in0=sm[:, 0:L], scalar=a_prev[:, 0:1], in1=b_t[:, 0:1].to_broadcast([128, L]),
                op0=ALU.subtract, op1=ALU.is_ge, accum_out=cnt[:, it:it + 1])
            K.dve(R=[Ba_prev, Bb_t, Bwt], W=[Bb_n]).scalar_tensor_tensor(
                out=b_n[:], in0=a_prev[:], scalar=wt[:, it:it + 1], in1=b_t[:], op0=ALU.subtract, op1=ALU.add)
            K.dve(R=[Bcnt, Bwt2], W=[Ba_n]).tensor_scalar(a_n[:], cnt[:, it:it + 1], 255.5, wt2[:, it:it + 1], op0=ALU.is_ge, op1=ALU.mult)
            a_prev, Ba_prev = a_n, Ba_n
        b_f, Bb_f = bb[NITER % 2]
        K.dve(R=[Ba_prev, Bb_f, Bwt2], W=[Blo]).scalar_tensor_tensor(
            out=lo[:], in0=a_prev[:], scalar=wt2[:, NITER - 1:NITER], in1=b_f[:], op0=ALU.subtract, op1=ALU.add)
        for pc in range(L // 1024):
            m_t, Bm_t = mb[pc % 2]
            K.dve(R=[Bsm, Blo], W=[Bm_t]).tensor_scalar(
                m_t[:], sm[:, pc * 1024:(pc + 1) * 1024], lo[:, 0:1], -30000.0, op0=ALU.is_lt, op1=ALU.mult)
            for c in range(8):
                K.pe(R=[Bm_t, Bidb], W=[BpsM]).transpose(psM[:, c * 128:(c + 1) * 128], m_t[:, c * 128:(c + 1) * 128], identb[:])
            K.act(R=[BpsM], W=[BmaskT]).copy(maskT[:, pc * 8:(pc + 1) * 8, :].rearrange("p c t -> p (c t)"), psM[:, :])

    def c_attention(s):
        L = 1024 * (s + 1)
        NCH = L // 128
        tcol = slice(s * 128, (s + 1) * 128)
        maskT, BmaskT = maskT2[s % 2]
        osb, Bosb = osb2[s % 2]
        for h in range(6):
            pO, BpO = psO[h // 3]
            ocol = (h % 3) * VW
            for p0 in range(0, L, PIECE):
                pw = min(PIECE, L - p0)
                KT_t, BKT_t = KTp[cstate["kv"] % 3]
                V_t, BV_t = Vp[cstate["kv"] % 3]
                cstate["kv"] += 1
                K.dma("sp", BKT_t, R=[BKT], W=[BKT_t]).dma_start(out=KT_t[:, 0:pw], in_=KT_scr[h, :, p0:p0 + pw])
                K.dma("sp", BV_t, R=[BV], W=[BV_t]).dma_start(out=V_t[:, 0:pw // 128, :], in_=V_scr[h, :, p0 // 128:(p0 + pw) // 128, :])
                for gl in range(pw // 512):
                    g = p0 // 512 + gl
                    pL, BpL = psL[g % 2]
                    K.pe(R=[BmaskT, Bidb], W=[BpL]).matmul(
                        pL[:, :], lhsT=identb[:], rhs=maskT[:, g * 4:(g + 1) * 4, :].rearrange("p c t -> p (c t)"),
                        start=True, stop=False, skip_group_check=True)
                    for c in range(4):
                        cl = gl * 4 + c
                        K.pe(R=[BKT_t, BqT], W=[BpL]).matmul(
                            pL[:, c * 128:(c + 1) * 128], lhsT=KT_t[:, cl * 128:(cl + 1) * 128], rhs=qT[:, h, tcol],
                            start=False, stop=(c == 3), skip_group_check=True)
                    p_t, Bp_t = pT_[cstate["pt"] % 3]
                    cstate["pt"] += 1
                    K.act(R=[BpL], W=[Bp_t]).activation(out=p_t[:], in_=pL[:, :], func=AF.Exp, scale=float(128 ** -0.5))
                    for c in range(4):
                        cl = gl * 4 + c
                        ch = g * 4 + c
                        K.pe(R=[Bp_t, BV_t], W=[BpO]).matmul(
                            pO[:, ocol:ocol + VW], lhsT=p_t[:, c * 128:(c + 1) * 128], rhs=V_t[:, cl, :],
                            start=(ch == 0), stop=(ch == NCH - 1), skip_group_check=True)
            K.act(R=[BpO], W=[Bosb]).copy(osb[:, h, :], pO[:, ocol:ocol + VW])

    def c_finalize(s):
        osb, Bosb = osb2[s % 2]
        a_st, Ba_st = aTst[s % 2]
        K.dve(R=[Bosb], W=[Brden]).reciprocal(rden[:], osb[:, :, 128])
        K.dve(R=[Bosb, Brden], W=[Battn_b]).tensor_tensor(
            out=attn_b[:], in0=osb[:, :, 0:128], in1=rden[:].unsqueeze(2).to_broadcast([128, 6, 128]), op=ALU.mult)
        for h in range(6):
            K.pe(R=[Battn_b, Bidb], W=[BpsX]).transpose(psX[:, h * 128:(h + 1) * 128], attn_b[:, h, :], identb[:])
        K.act(R=[BpsX], W=[Ba_st]).copy(a_st[:], psX[:, 0:768].rearrange("p (h t) -> p h t", h=6))
        K.dma("sp", Ba_st, R=[Ba_st], W=[BaT]).dma_start(out=aT_scr[:, :, s * 128:(s + 1) * 128], in_=a_st[:])

    c_indexer(0)
    c_threshold(0)
    for s in range(NS):
        if s + 1 < NS:
            c_indexer(s + 1)
        c_attention(s)
        if s + 1 < NS:
            c_threshold(s + 1)
        c_finalize(s)
    K.barrier()
    stC.close()
    stAC.close()

    stD = ExitStack()
    hT, BhT_s = sb(stD, "hT", [128, KC, NTOK], BF16)
    K.dma("sp", BhT_s, R=[BhT], W=[BhT_s]).dma_start(out=hT[:], in_=hT_scr[:, :, :])
    attnT, BattnT = sb(stD, "attnT2", [128, 6, NOWN], BF16)
    K.dma("sp", BattnT, R=[BaT], W=[BattnT]).dma_start(out=attnT[:], in_=aT_scr[:, :, :])
    crossT, BcrossT = sb(stD, "crossT", [128, 4, NOWN], BF16)
    p2T, Bp2T = sb(stD, "p2T", [96, 8, NOWN], BF16)
    NTC = [(o, min(512, NOWN - o)) for o in range(0, NOWN, 512)]

    stX = ExitStack()
    gmem, Bgmem = sb(stX, "gmem", [128, D])
    gxq, Bgxq = sb(stX, "gxq", [128, 128])
    gxk, Bgxk = sb(stX, "gxk", [128, 128])
    Wkv, BWkv = sb(stX, "Wkv", [128, KC, 1024], BF16)
    Wxq, BWxq = sb(stX, "Wxq", [128, KC, 512], BF16)
    mt = [sb(stX, "mt%d" % i, [128, D]) for i in range(2)]
    mbf, Bmbf = sb(stX, "mbf", [128, D], BF16)
    mjunk, Bmjunk = sb(stX, "mjunk", [128, D], BF16)
    memT, BmemT = sb(stX, "memT", [128, KC, 256], BF16)
    xs_ss = [sb(stX, "xss%d" % i, [128, 1]) for i in range(2)]
    xs_sd = [sb(stX, "xsd%d" % i, [128, 1]) for i in range(2)]
    xs_rs = [sb(stX, "xrs%d" % i, [128, 1]) for i in range(2)]
    kraw, Bkraw = sb(stX, "kraw", [128, 4, 128])
    xsq, Bxsq = sb(stX, "xsq", [128, 512])
    xssq, Bxssq = sb(stX, "xssq", [128, 4])
    xfac, Bxfac = sb(stX, "xfac", [128, 4])
    knb, Bknb = sb(stX, "knb", [128, 4, 128], BF16)
    kmT, BkmT = sb(stX, "kmT", [128, 4, 256], BF16)
    vm, Bvm = sb(stX, "vm", [128, 2, 4, VW], BF16)
    xqT, BxqT = sb(stX, "xqT", [128, 4, NOWN], BF16)
    xp = [sb(stX, "xp%d" % i, [128, 2, 128], BF16) for i in range(2)]
    xo, Bxo = sb(stX, "xo", [128, 4, VW])
    xrd, Bxrd = sb(stX, "xrd", [128, 4])
    cross_b, Bcross_b = sb(stX, "cross_b", [128, 4, 128], BF16)
    psXT = [ps(stX, "psXT%d" % i, [128, 1024], BF16) for i in range(2)]
    psXA = [ps(stX, "psXA%d" % i, [128, 512]) for i in range(2)]
    psXL, BpsXL = ps(stX, "psXL", [128, 512])
    psXO, BpsXO = ps(stX, "psXO", [128, 512])
    psXK, BpsXK = ps(stX, "psXK", [128, 1024], BF16)

    K.dma("sp", Bgmem, W=[Bgmem]).dma_start(out=gmem[:], in_=gmem_d[:, :])
    K.dma("sp", Bgxq, W=[Bgxq]).dma_start(out=gxq[:], in_=gxq_d[:, :])
    K.dma("sp", Bgxk, W=[Bgxk]).dma_start(out=gxk[:], in_=gxk_d[:, :])
    for c0 in range(0, 1024, 512):
        K.dma("pool", BWkv, R=[Bw], W=[BWkv]).dma_start(
            out=Wkv[:, :, c0:c0 + 512], in_=wmkv_d[:, c0:c0 + 512].rearrange("(k p) c -> p k c", p=128))
    K.dma("pool", BWxq, R=[Bw], W=[BWxq]).dma_start(
        out=Wxq[:], in_=w_in[:, C_XQ:C_XQ + 512].rearrange("(k p) c -> p k c", p=128))
    K.pool(W=[Bvm]).memset(vm[:], 1.0)
    for mtile in range(2):
        (m_t, Bm_t) = mt[mtile]
        (ss, Bss), (sd, Bsd), (rs, Brs) = xs_ss[mtile], xs_sd[mtile], xs_rs[mtile]
        K.dma("sp", Bm_t, W=[Bm_t]).dma_start(out=m_t[:], in_=mem[mtile * 128:(mtile + 1) * 128, :])
        rms_stats(m_t[:], Bm_t, mjunk[:], Bmjunk, ss[:], Bss, sd[:], Bsd, rs[:], Brs, D)
        K.dve(R=[Bm_t, Brs, Bgmem], W=[Bmbf]).scalar_tensor_tensor(
            out=mbf[:], in0=m_t[:], scalar=rs[:, 0:1], in1=gmem[:], op0=ALU.mult, op1=ALU.mult)
        for half in range(2):
            pT, BpT = psXT[half]
            for kk in range(8):
                kc = half * 8 + kk
                K.pe(R=[Bmbf, Bidb], W=[BpT]).transpose(pT[:, kk * 128:(kk + 1) * 128], mbf[:, kc * 128:(kc + 1) * 128], identb[:])
            K.act(R=[BpT], W=[BmemT]).copy(memT[:, half * 8:(half + 1) * 8, mtile * 128:(mtile + 1) * 128], pT[:, :].rearrange("p (k t) -> p k t", k=8))
        for kc in range(KC):
            for cg in range(2):
                K.pe(R=[BmemT, BWkv], W=[psXA[cg][1]]).matmul(
                    psXA[cg][0][:, :], lhsT=memT[:, kc, mtile * 128:(mtile + 1) * 128], rhs=Wkv[:, kc, cg * 512:(cg + 1) * 512],
                    start=(kc == 0), stop=(kc == KC - 1))
        krf = kraw[:].rearrange("p h d -> p (h d)")
        K.act(R=[psXA[0][1]], W=[Bkraw]).copy(krf, psXA[0][0][:, :])
        K.act(R=[psXA[1][1]], W=[Bvm]).copy(vm[:, mtile, :, 0:128], psXA[1][0][:, :].rearrange("p (h d) -> p h d", h=4))
        head_fac(krf, Bkraw, 4, xsq, Bxsq, xssq, Bxssq, xfac, Bxfac)
        K.dve(R=[Bkraw, Bxfac], W=[Bkraw]).tensor_tensor(out=kraw[:], in0=kraw[:], in1=xfac[:].unsqueeze(2).to_broadcast([128, 4, 128]), op=ALU.mult)
        K.dve(R=[Bkraw, Bgxk], W=[Bknb]).tensor_tensor(out=knb[:], in0=kraw[:], in1=gxk[:].unsqueeze(1).to_broadcast([128, 4, 128]), op=ALU.mult)
        for h in range(4):
            K.pe(R=[Bknb, Bidb], W=[BpsXK]).transpose(psXK[:, h * 128:(h + 1) * 128], knb[:, h, :], identb[:])
        K.act(R=[BpsXK], W=[BkmT]).copy(kmT[:, :, mtile * 128:(mtile + 1) * 128], psXK[:, 0:512].rearrange("p (h t) -> p h t", h=4))
    for i in range(NS):
        for kc in range(KC):
            K.pe(R=[BhT_s, BWxq], W=[psXA[0][1]]).matmul(
                psXA[0][0][:, :], lhsT=hT[:, kc, i * 128:(i + 1) * 128], rhs=Wxq[:, kc, :], start=(kc == 0), stop=(kc == KC - 1))
        krf = kraw[:].rearrange("p h d -> p (h d)")
        K.act(R=[psXA[0][1]], W=[Bkraw]).copy(krf, psXA[0][0][:, :])
        head_fac(krf, Bkraw, 4, xsq, Bxsq, xssq, Bxssq, xfac, Bxfac)
        K.dve(R=[Bkraw, Bxfac], W=[Bkraw]).tensor_tensor(out=kraw[:], in0=kraw[:], in1=xfac[:].unsqueeze(2).to_broadcast([128, 4, 128]), op=ALU.mult)
        K.dve(R=[Bkraw, Bgxq], W=[Bknb]).tensor_tensor(out=knb[:], in0=kraw[:], in1=gxq[:].unsqueeze(1).to_broadcast([128, 4, 128]), op=ALU.mult)
        for h in range(4):
            K.pe(R=[Bknb, Bidb], W=[BpsXK]).transpose(psXK[:, h * 128:(h + 1) * 128], knb[:, h, :], identb[:])
        K.act(R=[BpsXK], W=[BxqT]).copy(xqT[:, :, i * 128:(i + 1) * 128], psXK[:, 0:512].rearrange("p (h t) -> p h t", h=4))
    xpc = 0
    for i in range(NS):
        tcol = slice(i * 128, (i + 1) * 128)
        for h in range(4):
            for mc in range(2):
                K.pe(R=[BkmT, BxqT], W=[BpsXL]).matmul(
                    psXL[:, mc * 128:(mc + 1) * 128], lhsT=kmT[:, h, mc * 128:(mc + 1) * 128], rhs=xqT[:, h, tcol], start=True, stop=True)
            p_t, Bp_t = xp[xpc % 2]
            xpc += 1
            K.act(R=[BpsXL], W=[Bp_t]).activation(out=p_t[:].rearrange("p c t -> p (c t)"), in_=psXL[:, 0:256], func=AF.Exp, scale=float(128 ** -0.5))
            for mc in range(2):
                K.pe(R=[Bp_t, Bvm], W=[BpsXO]).matmul(
                    psXO[:, 0:VW], lhsT=p_t[:, mc, :], rhs=vm[:, mc, h, :], start=(mc == 0), stop=(mc == 1))
            K.act(R=[BpsXO], W=[Bxo]).copy(xo[:, h, :], psXO[:, 0:VW])
        K.dve(R=[Bxo], W=[Bxrd]).reciprocal(xrd[:], xo[:, :, 128])
        K.dve(R=[Bxo, Bxrd], W=[Bcross_b]).tensor_tensor(
            out=cross_b[:], in0=xo[:, :, 0:128], in1=xrd[:].unsqueeze(2).to_broadcast([128, 4, 128]), op=ALU.mult)
        for h in range(4):
            K.pe(R=[Bcross_b, Bidb], W=[BpsXK]).transpose(psXK[:, h * 128:(h + 1) * 128], cross_b[:, h, :], identb[:])
        K.act(R=[BpsXK], W=[BcrossT]).copy(crossT[:, :, tcol], psXK[:, 0:512].rearrange("p (h t) -> p h t", h=4))
    K.barrier()
    stX.close()

    stP = ExitStack()
    Wup, BWup = sb(stP, "Wup", [128, KC, 768], BF16)
    Wpg, BWpg = sb(stP, "Wpg", [96, 4, 2, 192], BF16)
    pscale, Bpscale = sb(stP, "pscale", [96, 8])
    invcnt, Binvcnt = sb(stP, "invcnt", [96, 4, NOWN])
    U = [sb(stP, "U%d" % i, [96, NS, 144]) for i in range(2)]
    Wn = [sb(stP, "Wn%d" % i, [96, NS, 144]) for i in range(2)]
    pTt, BpTt = sb(stP, "pTt", [96, 8, NOWN], BF16)
    psU = [ps(stP, "psU%d" % i, [128, 512]) for i in range(3)]
    psG = [ps(stP, "psG%d" % i, [128, 512]) for i in range(2)]
    for c0 in (0, 512):
        w = min(512, 768 - c0)
        K.dma("pool", BWup, R=[Bw], W=[BWup]).dma_start(
            out=Wup[:, :, c0:c0 + w], in_=w_in[:, C_UP + c0:C_UP + c0 + w].rearrange("(k p) c -> p k c", p=128))
    K.dma("pool", BWpg, R=[Bw], W=[BWpg]).dma_start(out=Wpg[:], in_=wpg_d.rearrange("g (i p) o -> p g i o", p=96))
    K.dma("sp", Bpscale, W=[Bpscale]).dma_start(out=pscale[:], in_=pscale_d[:, :])
    K.dma("sp", Binvcnt, W=[Binvcnt]).dma_start(out=invcnt[:], in_=invcnt_d[:, :, :])
    tok_chunks = NTC + [(NOWN, 128)]
    for i in range(2):
        K.pool(W=[U[i][1]]).memset(U[i][0][:], 0.0)
        K.pool(W=[Wn[i][1]]).memset(Wn[i][0][:], 0.0)
    for cc in range(8):
        g = cc // 2
        for ti, (o, w) in enumerate(tok_chunks):
            pU, BpU = psU[ti % 3]
            for kc in range(KC):
                K.pe(R=[BWup, BhT_s], W=[BpU]).matmul(
                    pU[0:96, 0:w], lhsT=Wup[:, kc, cc * 96:(cc + 1) * 96], rhs=hT[:, kc, o:o + w], start=(kc == 0), stop=(kc == KC - 1))
            u_t, Bu_t = U[cc % 2]
            if o < NOWN:
                s0 = o // 128
                K.act(R=[BpU], W=[Bu_t]).copy(u_t[:, s0:s0 + w // 128, 16:144], pU[0:96, 0:w].rearrange("p (s t) -> p s t", t=128))
            else:
                K.act(R=[BpU], W=[Bu_t]).copy(u_t[:, :, 0:16], pU[0:96, 0:NS * 16].rearrange("p (s t) -> p s t", t=16))
        cur, Bcur = u_t, Bu_t
        d = 1
        wi_ = 0
        while d < (2 << g):
            nxt, Bnxt = Wn[wi_ % 2]
            wi_ += 1
            K.dve(R=[Bcur], W=[Bnxt]).tensor_tensor(out=nxt[:, :, d:144], in0=cur[:, :, d:144], in1=cur[:, :, 0:144 - d], op=ALU.add)
            cur, Bcur = nxt, Bnxt
            d *= 2
        fin, Bfin = Wn[wi_ % 2]
        K.dve(R=[Bcur, Binvcnt], W=[Bfin]).tensor_tensor(
            out=fin[:, :, 16:144], in0=cur[:, :, 16:144], in1=invcnt[:, g, :].rearrange("p (s t) -> p s t", t=128), op=ALU.mult)
        K.dve(R=[Bfin, Bu_t], W=[BpTt]).tensor_tensor(
            out=pTt[:, cc, :].rearrange("p (s t) -> p s t", t=128), in0=fin[:, :, 16:144], in1=u_t[:, :, 16:144], op=ALU.subtract)
    for co in range(8):
        g = co // 2
        for ti, (o, w) in enumerate(NTC):
            pG, BpG = psG[ti % 2]
            for ci in range(2):
                K.pe(R=[BWpg, BpTt], W=[BpG]).matmul(
                    pG[0:96, 0:w], lhsT=Wpg[:, g, ci, (co % 2) * 96:(co % 2) * 96 + 96], rhs=pTt[:, 2 * g + ci, o:o + w], start=(ci == 0), stop=(ci == 1))
            K.act(R=[BpG, Bpscale], W=[Bp2T]).activation(out=p2T[:, co, o:o + w], in_=pG[0:96, 0:w], func=AF.Copy, scale=pscale[:, co:co + 1])
    K.barrier()
    stP.close()

    stM = ExitStack()
    mergedT, BmergedT = sb(stM, "mergedT", [128, KC, NOWN], BF16)
    bgT, BbgT = sb(stM, "bgT", [128, 48])
    Wg_ = [sb(stM, "Wg%d" % i, [128, KC, 3, 128], BF16) for i in range(2)]
    Wpo_ = [sb(stM, "Wpo%d" % i, [96, 8, 128], BF16) for i in range(2)]
    Wao_ = [sb(stM, "Wao%d" % i, [128, 6, 128], BF16) for i in range(2)]
    Wco_ = [sb(stM, "Wco%d" % i, [128, 4, 128], BF16) for i in range(2)]
    sg = [sb(stM, "sg%d" % i, [128, 512]) for i in range(3)]
    macc = [sb(stM, "macc%d" % i, [128, 512]) for i in range(2)]
    mtmp = [sb(stM, "mtmp%d" % i, [128, 512]) for i in range(2)]
    psGa = [ps(stM, "psGa%d" % i, [128, 512]) for i in range(3)]
    psBr = [ps(stM, "psBr%d" % i, [128, 512]) for i in range(3)]
    K.dma("sp", BbgT, W=[BbgT]).dma_start(out=bgT[:], in_=bgT_d[:, :])
    mc_ = 0

    def merge_loads(j):
        wb = j % 2
        (Wg_t, BWg_t), (Wpo_t, BWpo_t), (Wao_t, BWao_t), (Wco_t, BWco_t) = Wg_[wb], Wpo_[wb], Wao_[wb], Wco_[wb]
        for br in range(3):
            c0 = C_G + br * D + j * 128
            K.dma("pool", BWg_t, R=[Bw], W=[BWg_t]).dma_start(
                out=Wg_t[:, :, br, :], in_=w_in[:, c0:c0 + 128].rearrange("(k p) c -> p k c", p=128))
        K.dma("pool", BWpo_t, R=[Bw], W=[BWpo_t]).dma_start(
            out=Wpo_t[:], in_=wpo_d[:, j * 128:(j + 1) * 128].rearrange("(k p) c -> p k c", p=96))
        K.dma("pool", BWao_t, R=[Bw], W=[BWao_t]).dma_start(
            out=Wao_t[:], in_=wao_d[:, j * 128:(j + 1) * 128].rearrange("(k p) c -> p k c", p=128))
        K.dma("pool", BWco_t, R=[Bw], W=[BWco_t]).dma_start(
            out=Wco_t[:], in_=wco_d[:, j * 128:(j + 1) * 128].rearrange("(k p) c -> p k c", p=128))

    merge_loads(0)
    for j in range(KC):
        if j + 1 < KC:
            merge_loads(j + 1)
        wb = j % 2
        (Wg_t, BWg_t), (Wpo_t, BWpo_t), (Wao_t, BWao_t), (Wco_t, BWco_t) = Wg_[wb], Wpo_[wb], Wao_[wb], Wco_[wb]
        for (o, w) in NTC:
            for br in range(3):
                pg_, Bpg_ = psGa[br]
                for kc in range(KC):
                    K.pe(R=[BWg_t, BhT_s], W=[Bpg_]).matmul(
                        pg_[:, 0:w], lhsT=Wg_t[:, kc, br, :], rhs=hT[:, kc, o:o + w], start=(kc == 0), stop=(kc == KC - 1))
                K.act(R=[Bpg_, BbgT], W=[sg[br][1]]).activation(
                    out=sg[br][0][:, 0:w], in_=pg_[:, 0:w], func=AF.Sigmoid, bias=bgT[:, br * 16 + j:br * 16 + j + 1], scale=1.0)
            pb0, Bpb0 = psBr[0]
            for kc in range(8):
                K.pe(R=[BWpo_t, Bp2T], W=[Bpb0]).matmul(pb0[:, 0:w], lhsT=Wpo_t[:, kc, :], rhs=p2T[:, kc, o:o + w], start=(kc == 0), stop=(kc == 7))
            pb1, Bpb1 = psBr[1]
            for kc in range(6):
                K.pe(R=[BWao_t, BattnT], W=[Bpb1]).matmul(pb1[:, 0:w], lhsT=Wao_t[:, kc, :], rhs=attnT[:, kc, o:o + w], start=(kc == 0), stop=(kc == 5))
            pb2, Bpb2 = psBr[2]
            for kc in range(4):
                K.pe(R=[BWco_t, BcrossT], W=[Bpb2]).matmul(pb2[:, 0:w], lhsT=Wco_t[:, kc, :], rhs=crossT[:, kc, o:o + w], start=(kc == 0), stop=(kc == 3))
            ma, Bma = macc[mc_ % 2]
            mt_, Bmt_ = mtmp[mc_ % 2]
            mc_ += 1
            K.dve(R=[sg[0][1], Bpb0], W=[Bma]).tensor_tensor(out=ma[:, 0:w], in0=sg[0][0][:, 0:w], in1=pb0[:, 0:w], op=ALU.mult)
            K.dve(R=[sg[1][1], Bpb1], W=[Bmt_]).tensor_tensor(out=mt_[:, 0:w], in0=sg[1][0][:, 0:w], in1=pb1[:, 0:w], op=ALU.mult)
            K.dve(R=[Bma, Bmt_], W=[Bma]).tensor_tensor(out=ma[:, 0:w], in0=ma[:, 0:w], in1=mt_[:, 0:w], op=ALU.add)
            K.dve(R=[sg[2][1], Bpb2], W=[Bmt_]).tensor_tensor(out=mt_[:, 0:w], in0=sg[2][0][:, 0:w], in1=pb2[:, 0:w], op=ALU.mult)
            K.dve(R=[Bma, Bmt_], W=[BmergedT]).tensor_tensor(out=mergedT[:, j, o:o + w], in0=ma[:, 0:w], in1=mt_[:, 0:w], op=ALU.add)
    K.dma("sp", BmergedT, R=[BmergedT], W=[BmT]).dma_start(out=mT_scr[:, :, :], in_=mergedT[:])
    K.barrier()
    stM.close()
    stD.close()

    stE = ExitStack()
    acc, Bacc = sb(stE, "acc", [128, NS, D])
    h2T, Bh2T = sb(stE, "h2T", [128, KC, NOWN], BF16)
    comb, Bcomb = sb(stE, "comb", [128, NS, 16])
    stO = ExitStack()
    mergedT, BmergedT = sb(stO, "mergedT2", [128, KC, NOWN], BF16)
    K.dma("sp", BmergedT, R=[BmT], W=[BmergedT]).dma_start(out=mergedT[:], in_=mT_scr[:, :, :])
    gffn, Bgffn = sb(stO, "gffn", [128, D])
    Wo_ = [sb(stO, "Wo%d" % i, [128, KC, 512], BF16) for i in range(2)]
    xres = [sb(stO, "xres%d" % i, [128, 512]) for i in range(2)]
    wr, Bwr = sb(stO, "wr", [128, KC, 20])
    x2n, Bx2n = sb(stO, "x2n", [128, D])
    x2b, Bx2b = sb(stO, "x2b", [128, D], BF16)
    x2nT, Bx2nT = sb(stO, "x2nT", [128, KC, 128])
    ojunk, Bojunk = sb(stO, "ojunk", [128, D], BF16)
    o_ss, Bo_ss = sb(stO, "o_ss", [128, 1])
    o_sd, Bo_sd = sb(stO, "o_sd", [128, 1])
    o_rs, Bo_rs = sb(stO, "o_rs", [128, 1])
    lg, Blg = sb(stO, "lg", [128, NS, 20])
    rt = {n: sb(stO, "rt_" + n, [128, NS] + sz) for n, sz in
          (("mg", []), ("ohg", [4]), ("eg", [4]), ("sg", []), ("pg", []), ("les", [4]), ("m1", []), ("oh1", [4]), ("le2", [4]),
           ("m2", []), ("oh2", [4]), ("dm", []), ("ex", []), ("den", []), ("w1", []), ("w2", []), ("cl", [4]), ("cl2", [4]), ("prod", [4, 4]))}
    psW = [ps(stO, "psW%d" % i, [128, 512]) for i in range(2)]
    psFT = [ps(stO, "psFT%d" % i, [128, 512]) for i in range(2)]
    psBT = [ps(stO, "psBT%d" % i, [128, 1024], BF16) for i in range(2)]
    psR, BpsR = ps(stO, "psR", [128, 512])
    K.dma("sp", Bgffn, W=[Bgffn]).dma_start(out=gffn[:], in_=gffn_d[:, :])
    K.dma("sp", Bwr, W=[Bwr]).dma_start(out=wr[:], in_=wr_d.rearrange("(k p) c -> p k c", p=128))
    xc = 0
    for cg in range(4):
        W_t, BW_t = Wo_[cg % 2]
        K.dma("pool", BW_t, R=[Bw], W=[BW_t]).dma_start(
            out=W_t[:], in_=wo_d[:, cg * 512:(cg + 1) * 512].rearrange("(k p) c -> p k c", p=128))
        for i in range(NS):
            pW, BpW = psW[i % 2]
            for kc in range(KC):
                K.pe(R=[BmergedT, BW_t], W=[BpW]).matmul(
                    pW[:, :], lhsT=mergedT[:, kc, i * 128:(i + 1) * 128], rhs=W_t[:, kc, :], start=(kc == 0), stop=(kc == KC - 1))
            xr, Bxr = xres[xc % 2]
            xc += 1
            K.dma("sp", Bxr, R=[Bx_own], W=[Bxr]).dma_start(out=xr[:], in_=x_own[i * 128:(i + 1) * 128, cg * 512:(cg + 1) * 512])
            K.dve(R=[BpW, Bxr], W=[Bacc]).tensor_tensor(out=acc[:, i, cg * 512:(cg + 1) * 512], in0=pW[:, :], in1=xr[:], op=ALU.add)
    R_ = lambda n: rt[n][0]
    B_ = lambda n: rt[n][1]
    for i in range(NS):
        x2 = acc[:, i, :]
        rms_stats(x2, Bacc, ojunk[:], Bojunk, o_ss[:], Bo_ss, o_sd[:], Bo_sd, o_rs[:], Bo_rs, D)
        K.dve(R=[Bacc, Bo_rs, Bgffn], W=[Bx2n]).scalar_tensor_tensor(
            out=x2n[:], in0=x2, scalar=o_rs[:, 0:1], in1=gffn[:], op0=ALU.mult, op1=ALU.mult)
        K.pool(R=[Bx2n], W=[Bx2b]).tensor_copy(x2b[:], x2n[:])
        for half in range(2):
            pT, BpT = psBT[half]
            for kk in range(8):
                kc = half * 8 + kk
                K.pe(R=[Bx2b, Bidb], W=[BpT]).transpose(pT[:, kk * 128:(kk + 1) * 128], x2b[:, kc * 128:(kc + 1) * 128], identb[:])
            K.act(R=[BpT], W=[Bh2T]).copy(h2T[:, half * 8:(half + 1) * 8, i * 128:(i + 1) * 128], pT[:, :].rearrange("p (k t) -> p k t", k=8))
        for q4 in range(4):
            pF, BpF = psFT[q4 % 2]
            for kk in range(4):
                kc = q4 * 4 + kk
                K.pe(R=[Bx2n, Bidf], W=[BpF]).transpose(pF[:, kk * 128:(kk + 1) * 128], x2n[:, kc * 128:(kc + 1) * 128], identf[:])
            K.dve(R=[BpF], W=[Bx2nT]).tensor_copy(x2nT[:, q4 * 4:(q4 + 1) * 4, :].rearrange("p k t -> p (k t)"), pF[:, :])
        for kc in range(KC):
            K.pe(R=[Bx2nT, Bwr], W=[BpsR]).matmul(psR[:, 0:20], lhsT=x2nT[:, kc, :], rhs=wr[:, kc, :], start=(kc == 0), stop=(kc == KC - 1))
        K.act(R=[BpsR], W=[Blg]).copy(lg[:, i, :], psR[:, 0:20])
    def bc(ap, shape):
        return ap.to_broadcast(shape)
    lgG = lg[:, :, 0:4]
    lgE = lg[:, :, 4:20].rearrange("p s (g j) -> p s g j", g=4)
    K.dve(R=[Blg], W=[B_("mg")]).tensor_reduce(out=R_("mg")[:], in_=lgG, axis=AX.X, op=ALU.max)
    K.dve(R=[Blg, B_("mg")], W=[B_("eg")]).tensor_tensor(out=R_("eg")[:], in0=lgG, in1=bc(R_("mg")[:].unsqueeze(2), [128, NS, 4]), op=ALU.subtract)
    K.dve(R=[B_("eg")], W=[B_("ohg")]).tensor_scalar(R_("ohg")[:], R_("eg")[:], 0.0, None, op0=ALU.is_ge)
    K.act(R=[B_("eg")], W=[B_("eg")]).activation(out=R_("eg")[:], in_=R_("eg")[:], func=AF.Exp)
    K.dve(R=[B_("eg")], W=[B_("sg")]).tensor_reduce(out=R_("sg")[:], in_=R_("eg")[:], axis=AX.X, op=ALU.add)
    K.dve(R=[B_("sg")], W=[B_("pg")]).reciprocal(R_("pg")[:], R_("sg")[:])
    K.dve(R=[Blg, B_("ohg")], W=[B_("prod")]).tensor_tensor(
        out=R_("prod")[:], in0=lgE, in1=bc(R_("ohg")[:].unsqueeze(3), [128, NS, 4, 4]), op=ALU.mult)
    K.dve(R=[B_("prod")], W=[B_("les")]).tensor_reduce(
        out=R_("les")[:], in_=R_("prod")[:].rearrange("p s g j -> p s j g"), axis=AX.X, op=ALU.add)
    K.dve(R=[B_("les")], W=[B_("m1")]).tensor_reduce(out=R_("m1")[:], in_=R_("les")[:], axis=AX.X, op=ALU.max)
    K.dve(R=[B_("les"), B_("m1")], W=[B_("oh1")]).tensor_tensor(out=R_("oh1")[:], in0=R_("les")[:], in1=bc(R_("m1")[:].unsqueeze(2), [128, NS, 4]), op=ALU.is_ge)
    K.dve(R=[B_("oh1"), B_("les")], W=[B_("le2")]).scalar_tensor_tensor(
        out=R_("le2")[:], in0=R_("oh1")[:], scalar=-1.0e30, in1=R_("les")[:], op0=ALU.mult, op1=ALU.add)
    K.dve(R=[B_("le2")], W=[B_("m2")]).tensor_reduce(out=R_("m2")[:], in_=R_("le2")[:], axis=AX.X, op=ALU.max)
    K.dve(R=[B_("le2"), B_("m2")], W=[B_("oh2")]).tensor_tensor(out=R_("oh2")[:], in0=R_("le2")[:], in1=bc(R_("m2")[:].unsqueeze(2), [128, NS, 4]), op=ALU.is_ge)
    K.dve(R=[B_("m2"), B_("m1")], W=[B_("dm")]).tensor_tensor(out=R_("dm")[:], in0=R_("m2")[:], in1=R_("m1")[:], op=ALU.subtract)
    K.act(R=[B_("dm")], W=[B_("ex")]).activation(out=R_("ex")[:], in_=R_("dm")[:], func=AF.Exp)
    K.dve(R=[B_("ex")], W=[B_("den")]).tensor_scalar(R_("den")[:], R_("ex")[:], 1.0, None, op0=ALU.add)
    K.dve(R=[B_("den")], W=[B_("w1")]).reciprocal(R_("w1")[:], R_("den")[:])
    K.dve(R=[B_("w1"), B_("pg")], W=[B_("w1")]).tensor_tensor(out=R_("w1")[:], in0=R_("w1")[:], in1=R_("pg")[:], op=ALU.mult)
    K.dve(R=[B_("w1"), B_("ex")], W=[B_("w2")]).tensor_tensor(out=R_("w2")[:], in0=R_("w1")[:], in1=R_("ex")[:], op=ALU.mult)
    K.dve(R=[B_("oh1"), B_("w1")], W=[B_("cl")]).tensor_tensor(out=R_("cl")[:], in0=R_("oh1")[:], in1=bc(R_("w1")[:].unsqueeze(2), [128, NS, 4]), op=ALU.mult)
    K.dve(R=[B_("oh2"), B_("w2")], W=[B_("cl2")]).tensor_tensor(out=R_("cl2")[:], in0=R_("oh2")[:], in1=bc(R_("w2")[:].unsqueeze(2), [128, NS, 4]), op=ALU.mult)
    K.dve(R=[B_("cl"), B_("cl2")], W=[B_("cl2")]).tensor_tensor(out=R_("cl2")[:], in0=R_("cl2")[:], in1=R_("cl")[:], op=ALU.add)
    K.dve(R=[B_("cl2"), B_("ohg")], W=[Bcomb]).tensor_tensor(
        out=comb[:].rearrange("p s (g j) -> p s g j", g=4), in0=bc(R_("cl2")[:].unsqueeze(2), [128, NS, 4, 4]),
        in1=bc(R_("ohg")[:].unsqueeze(3), [128, NS, 4, 4]), op=ALU.mult)
    K.barrier()
    stO.close()

    stF = ExitStack()
    Wgu = [sb(stF, "Wgu%d" % i, [128, KC, 2, 128], BF16) for i in range(4)]
    Wd = [sb(stF, "Wd%d" % i, [128, 4, D], BF16) for i in range(2)]
    actT = [sb(stF, "actT%d" % i, [128, 4, NOWN], BF16) for i in range(2)]
    sil = [sb(stF, "sil%d" % i, [128, 512]) for i in range(2)]
    psGU = [ps(stF, "psGU%d" % i, [128, 512]) for i in range(4)]
    psD = [ps(stF, "psD%d" % i, [128, 512]) for i in range(4)]
    guc = 0
    slc = 0
    pdc = 0
    for e in range(NE):
        Wd_t, BWd_t = Wd[e % 2]
        a_t, Ba_t = actT[e % 2]
        for fc in range(4):
            Wgu_t, BWgu_t = Wgu[guc % 4]
            guc += 1
            K.dma("pool", BWgu_t, R=[Bw], W=[BWgu_t]).dma_start(
                out=Wgu_t[:, :, 0, :], in_=weg_d[e, :, fc * 128:(fc + 1) * 128].rearrange("(k p) c -> p k c", p=128))
            K.dma("pool", BWgu_t, R=[Bw], W=[BWgu_t]).dma_start(
                out=Wgu_t[:, :, 1, :], in_=weu_d[e, :, fc * 128:(fc + 1) * 128].rearrange("(k p) c -> p k c", p=128))
            if fc == 0:
                K.dma("pool", BWd_t, R=[Bw], W=[BWd_t]).dma_start(
                    out=Wd_t[:], in_=wed_d[e, :, :].rearrange("(k p) c -> p k c", p=128))
            for (o, w) in NTC:
                pg_, Bpg_ = psGU[pdc % 2 * 2]
                pu_, Bpu_ = psGU[pdc % 2 * 2 + 1]
                pdc += 1
                for kc in range(KC):
                    K.pe(R=[BWgu_t, Bh2T], W=[Bpg_]).matmul(pg_[:, 0:w], lhsT=Wgu_t[:, kc, 0, :], rhs=h2T[:, kc, o:o + w], start=(kc == 0), stop=(kc == KC - 1))
                for kc in range(KC):
                    K.pe(R=[BWgu_t, Bh2T], W=[Bpu_]).matmul(pu_[:, 0:w], lhsT=Wgu_t[:, kc, 1, :], rhs=h2T[:, kc, o:o + w], start=(kc == 0), stop=(kc == KC - 1))
                s_t, Bs_t = sil[slc % 2]
                slc += 1
                K.act(R=[Bpg_], W=[Bs_t]).activation(out=s_t[:, 0:w], in_=pg_[:, 0:w], func=AF.Silu)
                K.dve(R=[Bs_t, Bpu_], W=[Ba_t]).tensor_tensor(out=a_t[:, fc, o:o + w], in0=s_t[:, 0:w], in1=pu_[:, 0:w], op=ALU.mult)
        for i in range(NS):
            for cg in range(4):
                pD, BpD = psD[cg]
                for fc in range(4):
                    K.pe(R=[Ba_t, BWd_t], W=[BpD]).matmul(
                        pD[:, :], lhsT=a_t[:, fc, i * 128:(i + 1) * 128], rhs=Wd_t[:, fc, cg * 512:(cg + 1) * 512], start=(fc == 0), stop=(fc == 3))
                dst = acc[:, i, cg * 512:(cg + 1) * 512]
                K.dve(R=[BpD, Bcomb, Bacc], W=[Bacc]).scalar_tensor_tensor(
                    out=dst, in0=pD[:, :], scalar=comb[:, i, e:e + 1], in1=dst, op0=ALU.mult, op1=ALU.add)
    for i in range(NS):
        K.dma("sp", By, R=[Bacc], W=[By]).dma_start(out=y_out[i * 128:(i + 1) * 128, :], in_=acc[:, i, :])
    K.finish()
    K.emit()
    stF.close()
    stE.close()
    es.close()
    return nc


def _prep_inputs(inp, NS):
    f32 = np.float32
    S = 1024 * NS
    NT = S // 128
    x = np.ascontiguousarray(np.asarray(inp["x"], dtype=f32)[0])
    pos = np.asarray(inp["positions"])[0].astype(np.int32)
    sq = lambda k: np.asarray(inp[k], dtype=f32)[0]
    rep = lambda v: np.ascontiguousarray(np.broadcast_to(np.asarray(v, dtype=f32)[None, :], (128, v.shape[0])))
    w_router = np.ascontiguousarray(np.concatenate([sq("w_router_group"), sq("w_router_expert")], axis=1))
    invf128 = (10000.0 ** (-np.arange(0, 128, 2, dtype=np.float32) / np.float32(128))).astype(f32)
    invf64 = (10000.0 ** (-np.arange(0, 64, 2, dtype=np.float32) / np.float32(64))).astype(f32)
    invfT = rep(np.concatenate([invf128, invf128, invf64, invf64]))
    hp = np.float32(np.pi / 2)
    offsT = rep(np.concatenate([np.zeros(64, f32), np.full(64, hp, f32), np.zeros(32, f32), np.full(32, hp, f32)]))
    shared = {
        "x_all": x,
        "posT_all": np.ascontiguousarray(pos.reshape(NT, 128).T),
        "mem": np.ascontiguousarray(np.asarray(inp["mem"], dtype=f32)[0]),
        "w_in": sq("w_in"),
        "gmix_rep": rep(sq("g_mix")), "gffn_rep": rep(sq("g_ffn")), "gmem_rep": rep(sq("g_mem")),
        "bgT": np.ascontiguousarray(sq("b_gate").reshape(48, 128).T),
        "w_pool_grp": sq("w_pool_grp"),
        "pscaleT": np.ascontiguousarray(sq("pool_scale").reshape(8, 96).T),
        "gq_rep": rep(sq("q_norm_g")), "gk_rep": rep(sq("k_norm_g")),
        "gxq_rep": rep(sq("xq_norm_g")), "gxk_rep": rep(sq("xk_norm_g")),
        "w_mem_kv": sq("w_mem_kv"), "w_pool_out": sq("w_pool_out"), "w_attn_out": sq("w_attn_out"),
        "w_cross_out": sq("w_cross_out"), "w_o": sq("w_o"), "w_router": w_router,
        "w_e_gate": sq("w_e_gate"), "w_e_up": sq("w_e_up"), "w_e_down": sq("w_e_down"),
        "ident": np.eye(128, dtype=f32), "invfT": invfT, "offsT": offsT,
    }
    maps = []
    for c in range(NCORES):
        rows = np.concatenate([np.arange((8 * s + c) * 128, (8 * s + c + 1) * 128) for s in range(NS)])
        x_own = np.zeros((NS * 128 + 128, D), f32)
        x_own[:NS * 128] = x[rows]
        for s in range(NS):
            st = (8 * s + c) * 128
            if st >= 16:
                x_own[NS * 128 + s * 16:NS * 128 + (s + 1) * 16] = x[st - 16:st]
        t_i = np.arange(128)[:, None]
        j_i = np.arange(1024)[None, :]
        tailmask = np.where(j_i > 128 * c + t_i, np.float32(NEG), np.float32(0)).astype(f32)
        invcnt = np.zeros((96, 4, NS * 128), f32)
        for g, w in enumerate((2, 4, 8, 16)):
            invcnt[:, g, :] = (1.0 / np.minimum(rows + 1, w).astype(np.float64)).astype(f32)[None, :]
        m = dict(shared)
        m.update({"x_own": x_own, "posT_own": np.ascontiguousarray(pos[rows].reshape(NS, 128).T),
                  "tailmask": tailmask, "invcnt": invcnt})
        maps.append(m)
    return maps


_NC_CACHE = {}


def run(inp, NS):
    if NS not in _NC_CACHE:
        _NC_CACHE[NS] = build(NS)
    nc = _NC_CACHE[NS]
    maps = _prep_inputs(inp, NS)
    res = run_bass_kernel_spmd(nc, maps, core_ids=list(range(NCORES)))
    S = 1024 * NS
    out = np.zeros((1, S, D), np.float32)
    for c in range(NCORES):
        y = res.results[c]["y_own"]
        for s in range(NS):
            st = (8 * s + c) * 128
            out[0, st:st + 128] = y[s * 128:(s + 1) * 128]
    return out


def kernel(**inputs):
    return run(inputs, 8)
```
